# Optimizing a Trainium2 kernel written in Bass

```python
import jax, jax.numpy as jnp
from jax import lax
import numpy as np

D_MODEL = 2048
BATCH = 4
SEQ = 8192
DEPTH = 1

GRID_W = 64
Q_BLOCK = 128
ROPE_THETA = 10000.0
EPS = 1e-6

A_HEADS = 8
A_KV_HEADS = 2
A_HEAD_DIM = 128
A_GROUP = A_HEADS // A_KV_HEADS
A_WIDTH = A_HEADS * A_HEAD_DIM
B_HEADS = 8
B_QK_DIM = 64
B_V_DIM = 2 * B_QK_DIM
B_WIDTH = B_HEADS * B_V_DIM
A_Q_COLS = A_HEADS * A_HEAD_DIM
A_KV_COLS = A_KV_HEADS * A_HEAD_DIM
B_QK_COLS = B_HEADS * 2 * B_QK_DIM
B_V_COLS = B_HEADS * B_V_DIM
IN_COLS = A_Q_COLS + 2 * A_KV_COLS + 2 * B_QK_COLS + B_V_COLS
N_BRANCHES = 2
N_GROUPS = 4
EXPERTS_PER_GROUP = 8
N_EXPERTS = N_GROUPS * EXPERTS_PER_GROUP
TOP_K = 2
EXPERT_FF = D_MODEL // 4
MOE_BLOCK = 128

kernel_name = "hybrid_gated_axialgqa_diffattn_hmoe"


def rmsnorm(x, g):
    xf = x.astype(jnp.float32)
    xf = xf * lax.rsqrt(jnp.mean(xf * xf, axis=-1, keepdims=True) + EPS)
    return xf.astype(x.dtype) * g


def rope_angles(pos, dim):
    inv = ROPE_THETA ** (-jnp.arange(0, dim, 2, dtype=jnp.float32) / dim)
    ang = pos.astype(jnp.float32)[:, None] * inv[None, :]
    return jnp.cos(ang), jnp.sin(ang)


def apply_rope(x, cos, sin):
    half = x.shape[-1] // 2
    xf = x.astype(jnp.float32)
    x1, x2 = xf[..., :half], xf[..., half:]
    out = jnp.concatenate([x1 * cos - x2 * sin, x2 * cos + x1 * sin], axis=-1)
    return out.astype(x.dtype)


def axial_gqa(q, k, v, q_g, k_g, rows, cols):
    q = rmsnorm(q, q_g)
    k = rmsnorm(k, k_g)
    half = A_HEAD_DIM // 2
    cr, sr = rope_angles(rows, half)
    cc, sc = rope_angles(cols, half)
    bc = lambda t: t[None, :, None, :]

    def axial(t):
        return jnp.concatenate([apply_rope(t[..., :half], bc(cr), bc(sr)),
                                apply_rope(t[..., half:], bc(cc), bc(sc))], axis=-1)

    q, k = axial(q), axial(k)
    b, s = q.shape[0], q.shape[1]
    nb = s // Q_BLOCK
    qb = q.reshape(b, nb, Q_BLOCK, A_KV_HEADS, A_GROUP, A_HEAD_DIM).transpose(1, 0, 2, 3, 4, 5)
    scale = A_HEAD_DIM ** -0.5

    def block(qi):
        sc_ = jnp.einsum('bqkgd,bskd->bkgqs', qi, k, preferred_element_type=jnp.float32) * scale
        p = jax.nn.softmax(sc_, axis=-1).astype(v.dtype)
        return jnp.einsum('bkgqs,bskd->bqkgd', p, v)

    o = lax.map(block, qb)
    return o.transpose(1, 0, 2, 3, 4, 5).reshape(b, s, A_WIDTH)


def diff_attention(q, k, v, lam, lam_init, subln_g, pos):
    c, s_ = rope_angles(pos, B_QK_DIM)
    bc = lambda t: t[None, :, None, None, :]
    q = apply_rope(q, bc(c), bc(s_))
    k = apply_rope(k, bc(c), bc(s_))
    b, s = q.shape[0], q.shape[1]
    nb = s // Q_BLOCK
    qb = q.reshape(b, nb, Q_BLOCK, B_HEADS, 2, B_QK_DIM).transpose(1, 0, 2, 3, 4, 5)
    scale = B_QK_DIM ** -0.5

    def block(qi):
        sc_ = jnp.einsum('bqhcd,bshcd->bhcqs', qi, k, preferred_element_type=jnp.float32) * scale
        p = jax.nn.softmax(sc_, axis=-1)
        a = (p[:, :, 0] - lam * p[:, :, 1]).astype(v.dtype)
        return jnp.einsum('bhqs,bshe->bqhe', a, v)

    o = lax.map(block, qb)
    o = o.transpose(1, 0, 2, 3, 4).reshape(b, s, B_HEADS, B_V_DIM)
    o = rmsnorm(o, subln_g) * (1.0 - lam_init)
    return o.reshape(b, s, B_WIDTH)


def hier_moe(h, w_rg, w_re, w1, w3, w2):
    b, s, d = h.shape
    t = b * s
    hf = h.reshape(t, d)
    g_logits = jnp.matmul(hf, w_rg).astype(jnp.float32)
    g_prob = jax.nn.softmax(g_logits, axis=-1)
    g_idx = jnp.argmax(g_logits, axis=-1).astype(jnp.int32)
    g_w = jnp.take_along_axis(g_prob, g_idx[:, None], axis=-1)
    e_logits = jnp.matmul(hf, w_re).astype(jnp.float32).reshape(t, N_GROUPS, EXPERTS_PER_GROUP)
    e_in_group = jnp.take_along_axis(e_logits, g_idx[:, None, None], axis=1)[:, 0]
    top_v, top_i = lax.top_k(e_in_group, TOP_K)
    e_w = jax.nn.softmax(top_v, axis=-1) * g_w
    expert = g_idx[:, None] * EXPERTS_PER_GROUP + top_i.astype(jnp.int32)

    n_assign = t * TOP_K
    e_flat = expert.reshape(n_assign)
    tok_flat = jnp.repeat(jnp.arange(t, dtype=jnp.int32), TOP_K)
    w_flat = e_w.reshape(n_assign)
    order = jnp.argsort(e_flat)
    e_sorted, tok_sorted, w_sorted = e_flat[order], tok_flat[order], w_flat[order]
    counts = jnp.bincount(e_flat, length=N_EXPERTS).astype(jnp.int32)
    offsets = jnp.cumsum(counts) - counts
    padded = ((counts + MOE_BLOCK - 1) // MOE_BLOCK) * MOE_BLOCK
    padded_ends = jnp.cumsum(padded)
    padded_offsets = padded_ends - padded
    rank = jnp.arange(n_assign, dtype=jnp.int32) - offsets[e_sorted]
    pos = padded_offsets[e_sorted] + rank
    n_blocks = -(-n_assign // MOE_BLOCK) + N_EXPERTS
    p_len = n_blocks * MOE_BLOCK
    buf_tok = jnp.full((p_len,), t, jnp.int32).at[pos].set(tok_sorted)
    buf_w = jnp.zeros((p_len,), jnp.float32).at[pos].set(w_sorted)
    block_start = jnp.arange(n_blocks, dtype=jnp.int32) * MOE_BLOCK
    block_expert = jnp.clip(jnp.searchsorted(padded_ends, block_start, side='right'),
                            0, N_EXPERTS - 1).astype(jnp.int32)
    x_pad = jnp.concatenate([hf, jnp.zeros((1, d), hf.dtype)], axis=0)

    def run(args):
        tok, wt, e = args
        xb = x_pad[tok]
        u = jax.nn.silu(xb @ w1[e]) * (xb @ w3[e])
        return (u @ w2[e]) * wt[:, None].astype(hf.dtype)

    out = lax.map(run, (buf_tok.reshape(n_blocks, MOE_BLOCK),
                        buf_w.reshape(n_blocks, MOE_BLOCK), block_expert))
    y = jnp.zeros((t + 1, d), hf.dtype).at[buf_tok].add(out.reshape(p_len, d))
    return y[:t].reshape(b, s, d)


def setup_inputs(seed: int = 0) -> dict:
    key = jax.random.key(seed)
    ks = jax.random.split(key, 24)
    f32 = jnp.float32
    nrm = lambda k, shape, scale: jax.random.normal(k, shape, f32) * scale
    gain = lambda k, shape: 1.0 + 0.01 * jax.random.normal(k, shape, f32)
    L, D = DEPTH, D_MODEL
    return {
        "x": jax.random.normal(ks[0], (BATCH, SEQ, D), f32),
        "g_mix": gain(ks[1], (L, D)),
        "w_in": nrm(ks[2], (L, D, IN_COLS), D ** -0.5),
        "q_norm_a": gain(ks[3], (L, A_HEAD_DIM)),
        "k_norm_a": gain(ks[4], (L, A_HEAD_DIM)),
        "lam_q1": nrm(ks[5], (L, B_QK_DIM), 0.1),
        "lam_k1": nrm(ks[6], (L, B_QK_DIM), 0.1),
        "lam_q2": nrm(ks[7], (L, B_QK_DIM), 0.1),
        "lam_k2": nrm(ks[8], (L, B_QK_DIM), 0.1),
        "subln_b": gain(ks[9], (L, B_V_DIM)),
        "w_branch_a": nrm(ks[10], (L, A_WIDTH, D), A_WIDTH ** -0.5),
        "w_branch_b": nrm(ks[11], (L, B_WIDTH, D), B_WIDTH ** -0.5),
        "w_gate": nrm(ks[12], (L, D, N_BRANCHES * D), D ** -0.5),
        "b_gate": nrm(ks[13], (L, N_BRANCHES * D), 0.01),
        "w_out": nrm(ks[14], (L, D, D), D ** -0.5),
        "g_ffn": gain(ks[15], (L, D)),
        "w_router_group": nrm(ks[16], (L, D, N_GROUPS), D ** -0.5),
        "w_router_expert": nrm(ks[17], (L, D, N_EXPERTS), D ** -0.5),
        "w_e_gate": nrm(ks[18], (L, N_EXPERTS, D, EXPERT_FF), D ** -0.5),
        "w_e_up": nrm(ks[19], (L, N_EXPERTS, D, EXPERT_FF), D ** -0.5),
        "w_e_down": nrm(ks[20], (L, N_EXPERTS, EXPERT_FF, D), EXPERT_FF ** -0.5),
        "g_final": gain(ks[21], (D,)),
    }


def reference(x, g_mix, w_in, q_norm_a, k_norm_a, lam_q1, lam_k1, lam_q2, lam_k2, subln_b,
              w_branch_a, w_branch_b, w_gate, b_gate, w_out, g_ffn, w_router_group,
              w_router_expert, w_e_gate, w_e_up, w_e_down, g_final):
    b, s, _ = x.shape
    ROWS = s // GRID_W
    pos = jnp.arange(s, dtype=jnp.int32)
    rows = jnp.repeat(jnp.arange(ROWS, dtype=jnp.int32), GRID_W)
    cols = jnp.tile(jnp.arange(GRID_W, dtype=jnp.int32), ROWS)
    split_at = [A_Q_COLS, A_Q_COLS + A_KV_COLS, A_Q_COLS + 2 * A_KV_COLS,
                A_Q_COLS + 2 * A_KV_COLS + B_QK_COLS, A_Q_COLS + 2 * A_KV_COLS + 2 * B_QK_COLS]
    for l in range(DEPTH):
        lam_init = 0.8 - 0.6 * float(np.exp(-0.3 * l))
        h = rmsnorm(x, g_mix[l])
        proj = h @ w_in[l]
        qa, ka, va, qb, kb, vb = jnp.split(proj, split_at, axis=-1)
        qa = qa.reshape(b, s, A_HEADS, A_HEAD_DIM)
        ka = ka.reshape(b, s, A_KV_HEADS, A_HEAD_DIM)
        va = va.reshape(b, s, A_KV_HEADS, A_HEAD_DIM)
        qb = qb.reshape(b, s, B_HEADS, 2, B_QK_DIM)
        kb = kb.reshape(b, s, B_HEADS, 2, B_QK_DIM)
        vb = vb.reshape(b, s, B_HEADS, B_V_DIM)
        o_a = axial_gqa(qa, ka, va, q_norm_a[l], k_norm_a[l], rows, cols)
        lam = (jnp.exp(jnp.sum(lam_q1[l].astype(jnp.float32) * lam_k1[l].astype(jnp.float32)))
               - jnp.exp(jnp.sum(lam_q2[l].astype(jnp.float32) * lam_k2[l].astype(jnp.float32)))
               + lam_init)
        o_b = diff_attention(qb, kb, vb, lam, lam_init, subln_b[l], pos)
        gates = jax.nn.sigmoid((h @ w_gate[l] + b_gate[l]).astype(jnp.float32)).astype(x.dtype)
        g_a, g_b = jnp.split(gates, N_BRANCHES, axis=-1)
        merged = g_a * (o_a @ w_branch_a[l]) + g_b * (o_b @ w_branch_b[l])
        x = x + merged @ w_out[l]
        x = x + hier_moe(rmsnorm(x, g_ffn[l]), w_router_group[l], w_router_expert[l],
                         w_e_gate[l], w_e_up[l], w_e_down[l])
    return rmsnorm(x, g_final)
```

```python
import contextlib
import numpy as np
import concourse.bass as bass
import concourse.mybir as mybir
from concourse.bass_utils import run_bass_kernel_spmd

F32 = mybir.dt.float32
BF16 = mybir.dt.bfloat16
I32 = mybir.dt.int32
AF = mybir.ActivationFunctionType
ALU = mybir.AluOpType
AX = mybir.AxisListType

D = 2048
S = 8192
OWN = 4096
NCORES = 8
EPS = 1e-6
NEXP = 32
FF = 512
NBLK = 96
NPOS = NBLK * 128
IN_COLS = 4608


class Buf:
    def __init__(self, name, t=None, parent=None):
        self.name, self.t, self.parent = name, t, parent
        self.w, self.r, self.kids = {}, {}, {}

    def reg(self, key):
        if key not in self.kids:
            self.kids[key] = Buf("%s.%s" % (self.name, key), self.t, self)
        return self.kids[key]


def _merge(d, s):
    for k, v in s.items():
        if d.get(k, 0) < v:
            d[k] = v


class Sched:
    def __init__(self, nc, es):
        self.nc, self.es = nc, es
        self.engs = {"pe": nc.tensor, "act": nc.scalar, "dve": nc.vector, "pool": nc.gpsimd, "sp": nc.sync}
        self.sems, self.cnt, self.cur = {}, {}, {}
        self.known = {e: {} for e in self.engs}
        self.nsem = 0
        self.phase = 0
        for e in self.engs:
            self._newkey(e)

    def _newkey(self, e):
        key = "%s#%d" % (e, self.phase)
        self._newsem(key)
        self.cur[e] = key

    def _newsem(self, key):
        h = self.es.enter_context(self.nc.semaphore("s%d" % self.nsem))
        self.nsem += 1
        self.sems[key] = h
        self.cnt[key] = 0

    def chan(self, name, persistent=False, sw=False):
        self.nchan = getattr(self, "nchan", 0) + 1
        key = "dma_%s_%d" % (name, self.nchan)
        if not hasattr(self, "free_chans"):
            self.free_chans, self.live_chans = {True: [], False: []}, []
        free = self.free_chans[sw]
        if free and not persistent:
            h, base = free.pop()
            self.sems[key] = h
            self.cnt[key] = base
        else:
            self._newsem(key)
        if not persistent:
            self.live_chans.append((key, sw))
        return key

    def _deps(self, reads, writes):
        d = {}
        for b in reads:
            _merge(d, b.w)
            if b.parent is not None:
                _merge(d, b.parent.w)
            for k in b.kids.values():
                _merge(d, k.w)
        for b in writes:
            xs = [b] + ([b.parent] if b.parent is not None else []) + list(b.kids.values())
            for x in xs:
                _merge(d, x.w)
                _merge(d, x.r)
        return d

    def _wait(self, e, deps):
        kn = self.known[e]
        for k, v in deps.items():
            if e == "pe" and k == self.cur["pe"]:
                continue
            if k == self.cur[e] and v <= self.cnt[k] - 3:
                continue
            if kn.get(k, 0) < v:
                self.engs[e].wait_ge(self.sems[k], v)
                kn[k] = v

    def _commit(self, tok, reads, writes):
        k, v = tok
        for b in reads:
            if b.r.get(k, 0) < v:
                b.r[k] = v
        for b in writes:
            b.w = {k: v}
            b.r = {}
            for kid in b.kids.values():
                kid.w, kid.r = {}, {}

    def op(self, e, fn, reads=(), writes=()):
        self._wait(e, self._deps(reads, writes))
        ins = fn(self.engs[e])
        key = self.cur[e]
        self.cnt[key] += 1
        assert self.cnt[key] < 60000, key
        ins.then_inc(self.sems[key], 1)
        self._commit((key, self.cnt[key]), reads, writes)

    def dma(self, q, ch, out, in_, reads=(), writes=(), **kw):
        self._wait(q, self._deps(reads, writes))
        ins = self.engs[q].dma_start(out=out, in_=in_, **kw)
        self.cnt[ch] += 16
        assert self.cnt[ch] < 60000, ch
        ins.then_inc(self.sems[ch], 16)
        self._commit((ch, self.cnt[ch]), reads, writes)

    def custom_dma(self, q, ch, fn, reads=(), writes=()):
        self._wait(q, self._deps(reads, writes))
        ins = fn(self.engs[q])
        self.cnt[ch] += 16
        ins.then_inc(self.sems[ch], 16)
        self._commit((ch, self.cnt[ch]), reads, writes)

    def fence(self, ch, buf, readers=False):
        v = self.cnt[ch]
        for b in [buf] + list(buf.kids.values()):
            d = b.r if readers else b.w
            if ch in d or b is buf:
                d[ch] = v

    def wait_all(self, e, keys=None):
        tot = {k: v for k, v in self.cnt.items() if v > 0 and (keys is None or k in keys)}
        self._wait(e, tot)

    def barrier(self):
        tot = {k: v for k, v in self.cnt.items() if v > 0}
        for e in self.engs:
            kn = self.known[e]
            for k, v in tot.items():
                if k == self.cur[e]:
                    continue
                if kn.get(k, 0) < v:
                    self.engs[e].wait_ge(self.sems[k], v)
                    kn[k] = v
        for e in self.engs:
            for k, v in tot.items():
                self.known[e][k] = v
        self.phase += 1
        for e in self.engs:
            self._newkey(e)
        for (key, sw) in getattr(self, "live_chans", []):
            self.free_chans[sw].append((self.sems[key], self.cnt[key]))
        self.live_chans = []


class Pool_:
    N = [0]

    def __init__(self, nc, es):
        self.nc, self.es = nc, es

    def sb(self, name, shape, dt):
        Pool_.N[0] += 1
        t = self.es.enter_context(self.nc.sbuf_tensor("%s_%d" % (name, Pool_.N[0]), list(shape), dt))
        return Buf(name, t)


LCOL_FLAG = True


def build(stop_after=None, debug=False):
    nc = bass.Bass("TRN2", target_bir_lowering=False)

    def din(name, shape, dt=F32):
        return nc.dram_tensor(name, list(shape), dt, kind="ExternalInput").ap()

    def dscr(name, shape, dt):
        kind = "ExternalOutput" if (debug and name in debug) else "Internal"
        return nc.dram_tensor(name, list(shape), dt, kind=kind).ap()

    xT = din("xT", [D, S])
    x_own = din("x_own", [OWN, D])
    g_mix = din("g_mix", [D])
    w_in = din("w_in", [D, IN_COLS])
    q_norm_a = din("q_norm_a", [128])
    k_norm_a = din("k_norm_a", [128])
    lam4 = din("lam4", [4, 64])
    subln_b = din("subln_b", [128])
    w_branch_a = din("w_branch_a", [1024, D])
    w_branch_b = din("w_branch_b", [1024, D])
    w_gate = din("w_gate", [D, 2 * D])
    b_gate = din("b_gate", [2 * D])
    w_out = din("w_out", [D, D])
    g_ffn = din("g_ffn", [D])
    w_router = din("w_router", [D, 36])
    w_e_gate = din("w_e_gate", [NEXP, D, FF])
    w_e_up = din("w_e_up", [NEXP, D, FF])
    w_e_down = din("w_e_down", [NEXP, FF, D])
    g_final = din("g_final", [D])
    cosA = din("cosA", [128, S])
    sinA = din("sinA", [128, S])
    cosB = din("cosB", [128, S])
    sinB = din("sinB", [128, S])
    rmat = din("rmat", [128, 128])
    ident = din("ident", [128, 128])
    consts = din("consts", [128, 64])
    ustrict = din("ustrict", [128, 128])
    thr = din("thr", [128, 64])

    y = nc.dram_tensor("y", [OWN, D], F32, kind="ExternalOutput").ap()

    KAT = dscr("KAT", [2, 128, S], BF16)
    VA = dscr("VA", [S, 256], BF16)
    QAT = dscr("QAT", [8, 128, OWN], BF16)
    KBT = dscr("KBT", [8, 128, S], BF16)
    QBT = dscr("QBT", [8, 128, OWN], BF16)
    VB = dscr("VB", [S, 1024], BF16)
    HT = dscr("HT", [D, OWN], BF16)
    OT = dscr("OT", [D, OWN], BF16)
    MT = dscr("MT", [D, OWN], BF16)
    X1 = dscr("X1", [OWN, D], F32)
    XBUF = dscr("XBUF", [NPOS, D], BF16)
    OBUF = dscr("OBUF", [NPOS, D], F32)
    WD1 = dscr("WD1", [8, 128, 12288], BF16)
    WO16 = dscr("WO16", [4, 128, 8192], BF16)
    WG16 = dscr("WG16", [NEXP * 128, 8192], BF16)
    WU16 = dscr("WU16", [NEXP * 128, 8192], BF16)
    WD16 = dscr("WD16", [NEXP * 128, 8192], BF16)

    es = contextlib.ExitStack()
    with es:
        sch = Sched(nc, es)
        P0 = Pool_(nc, es)

        ps_es = contextlib.ExitStack()
        es.enter_context(ps_es)
        PS = []
        for i in range(8):
            t = ps_es.enter_context(nc.psum_tensor("ps%d" % i, [128, 512], F32))
            PS.append(Buf("ps%d" % i, t))

        ones_bf = P0.sb("ones_bf", [128, 128], BF16)
        rmat_bf = P0.sb("rmat_bf", [128, 128], BF16)
        ident_bf = P0.sb("ident_bf", [128, 128], BF16)
        ident_f = P0.sb("ident_f", [128, 128], F32)
        gmix_sb = P0.sb("gmix_sb", [128, 16], F32)
        qn_sb = P0.sb("qn_sb", [128, 1], F32)
        kn_sb = P0.sb("kn_sb", [128, 1], F32)
        ch_c = sch.chan("const", persistent=True)
        ch_cp = sch.chan("constp", persistent=True, sw=True)
        sch.op("dve", lambda e: e.memset(ones_bf.t[:], 1.0), writes=[ones_bf])
        eps_sb = P0.sb("eps_sb", [128, 1], F32)
        sch.op("dve", lambda e: e.memset(eps_sb.t[:], EPS), writes=[eps_sb])
        sch.dma("pool", ch_cp, rmat_bf.t[:], rmat[:, :], writes=[rmat_bf])
        sch.dma("pool", ch_cp, ident_bf.t[:], ident[:, :], writes=[ident_bf])
        sch.dma("sp", ch_c, ident_f.t[:], ident[:, :], writes=[ident_f])
        sch.dma("sp", ch_c, gmix_sb.t[:], g_mix.rearrange("(c p) -> p c", p=128), writes=[gmix_sb],
                allow_slow_non_contiguous=True)
        sch.dma("sp", ch_c, qn_sb.t[:], q_norm_a.rearrange("(p o) -> p o", o=1), writes=[qn_sb])
        sch.dma("sp", ch_c, kn_sb.t[:], k_norm_a.rearrange("(p o) -> p o", o=1), writes=[kn_sb])


        zero_bf = P0.sb("zero_bf", [128, D], BF16)
        ch_z = sch.chan("zfill", persistent=True)
        sch.op("pool", lambda e: e.memset(zero_bf.t[:], 0.0), writes=[zero_bf])
        for b_ in range(NBLK):
            sch.dma("sp", ch_z, XBUF[b_ * 128:(b_ + 1) * 128, :], zero_bf.t[:], reads=[zero_bf])

        xT_v = xT.rearrange("(c p) t -> p c t", p=128)
        w_in_v = w_in.rearrange("(c p) n -> p c n", p=128)

        def phaseA(passno):
            pes = contextlib.ExitStack()
            with pes:
                P = Pool_(nc, pes)
                if passno == 1:
                    segs = [(1024, 512), (2560, 2048)]
                    ntiles = 16
                else:
                    segs = [(0, 1024), (1536, 1024)]
                    ntiles = 8
                ncols = sum(n for _, n in segs)
                wsb = P.sb("wsb", [128, 16, ncols], BF16)
                ch_w = sch.chan("wA", sw=True)
                off = 0
                segoff = {}
                for (c0, n) in segs:
                    segoff[c0] = off
                    for c in range(16):
                        for p0 in range(0, n, 512):
                            sch.dma("pool", ch_w, wsb.t[:, c, off + p0:off + p0 + 512], w_in_v[:, c, c0 + p0:c0 + p0 + 512],
                                    writes=[wsb.reg((c0, c, p0))])
                    off += n
                sch.fence(ch_w, wsb)
                xt = P.sb("xt", [128, 16, 512], F32)
                hts = [P.sb("ht%d" % i, [128, 16, 512], BF16) for i in range(2)]
                sqs = [P.sb("sq%d" % i, [128, 512], BF16) for i in range(8)]
                rstd = P.sb("rstd", [128, 512], F32)
                cs = [[P.sb("cs%d_%d" % (i, j), [128, 512], F32) for j in range(4)] for i in range(2)]
                xgs = [P.sb("xg%d" % i, [128, 512], BF16) for i in range(2)]
                sq2s = [P.sb("sq2_%d" % i, [128, 512], BF16) for i in range(2)]
                rs2 = [P.sb("rs2_%d" % i, [128, 512], F32) for i in range(2)]
                t1s = [P.sb("t1_%d" % i, [128, 512], F32) for i in range(2)]
                t2s = [P.sb("t2_%d" % i, [128, 512], F32) for i in range(2)]
                outs = [P.sb("out%d" % i, [128, 512], BF16) for i in range(3)]
                vts = [P.sb("vt%d" % i, [128, 512], BF16) for i in range(3)]
                ch_x = sch.chan("xA")
                ch_cs = [sch.chan("csA%d" % i) for i in range(2)]
                ch_out = [sch.chan("outA%d" % i) for i in range(3)]
                ch_vt = [sch.chan("vtA%d" % i) for i in range(3)]
                ch_ht = [sch.chan("htA%d" % i) for i in range(2)]
                ps_main = [PS[0], PS[1], PS[2]]
                ps_ss, ps_rot, ps_ss2 = PS[3], PS[4], PS[5]
                cnt = {"main": 0, "ep": 0, "out": 0, "vt": 0}

                def prep_load(t):
                    tk = slice(t * 512, (t + 1) * 512)
                    csb = cs[t % 2]
                    for c4 in range(4):
                        sch.dma("sp", ch_x, xt.t[:, 4 * c4:4 * c4 + 4, :], xT_v[:, 4 * c4:4 * c4 + 4, tk],
                                writes=[xt.reg(c4)])
                    sch.fence(ch_x, xt)
                    for j, tab in enumerate((cosA, sinA, cosB, sinB)):
                        sch.dma("sp", ch_cs[t % 2], csb[j].t[:], tab[:, tk], writes=[csb[j]])
                    for j in range(4):
                        csb[j].w = {ch_cs[t % 2]: sch.cnt[ch_cs[t % 2]]}

                def prep_sq(t, q):
                    for c in range(4 * q, 4 * q + 4):
                        sq = sqs[c % 8]
                        sch.op("act", lambda e, c=c, sq=sq: e.activation(out=sq.t[:], in_=xt.t[:, c, :], func=AF.Square),
                               reads=[xt.reg(c // 4)], writes=[sq])

                def prep_mm(t, q):
                    for c in range(4 * q, 4 * q + 4):
                        sq = sqs[c % 8]
                        sch.op("pe", lambda e, c=c, sq=sq: e.matmul(ps_ss.t[:], ones_bf.t[:], sq.t[:], start=(c == 0), stop=(c == 15)),
                               reads=[ones_bf, sq], writes=[ps_ss])

                def prep_fin(t):
                    tk = slice(t * 512, (t + 1) * 512)
                    ht = hts[t % 2]
                    sch.op("act", lambda e: e.activation(out=rstd.t[:], in_=ps_ss.t[:], func=AF.Ln, scale=1.0 / D, bias=eps_sb.t[:, 0:1]),
                           reads=[ps_ss, eps_sb], writes=[rstd])
                    sch.op("act", lambda e: e.activation(out=rstd.t[:], in_=rstd.t[:], func=AF.Exp, scale=-0.5),
                           reads=[rstd], writes=[rstd])
                    for c in range(16):
                        sch.op("dve", lambda e, c=c: e.scalar_tensor_tensor(out=ht.t[:, c, :], in0=xt.t[:, c, :],
                                                                            scalar=gmix_sb.t[:, c:c + 1], in1=rstd.t[:],
                                                                            op0=ALU.mult, op1=ALU.mult),
                               reads=[xt.reg(c // 4), rstd, gmix_sb], writes=[ht.reg(c)])
                    if passno == 2:
                        for c4 in range(4):
                            sch.dma("sp", ch_ht[t % 2], HT.rearrange("(c p) t -> p c t", p=128)[:, 4 * c4:4 * c4 + 4, tk],
                                    ht.t[:, 4 * c4:4 * c4 + 4, :], reads=[ht.reg(4 * c4 + i) for i in range(4)])
                        sch.fence(ch_ht[t % 2], ht, readers=True)

                def prep_step(t, n):
                    if n == 1:
                        prep_load(t)
                    if 3 <= n <= 6:
                        prep_sq(t, n - 3)
                    if 4 <= n <= 7:
                        prep_mm(t, n - 4)
                    if n == 7:
                        prep_fin(t)

                def tile_main(t):
                    tk = slice(t * 512, (t + 1) * 512)
                    ht = hts[t % 2]
                    csb = cs[t % 2]
                    if passno == 1:
                        fm = [("A", 1024 + 128 * i, kn_sb, KAT[i, :, tk]) for i in range(2)] + \
                             [("B", 2560 + 128 * i, None, KBT[i, :, tk]) for i in range(8)]
                    else:
                        fm = [("A", 128 * i, qn_sb, QAT[i, :, tk]) for i in range(8)] + \
                             [("B", 1536 + 128 * i, None, QBT[i, :, tk]) for i in range(8)]
                    nfm = [0]
                    pend = [None]
                    for (kind, c0, gsb, dst) in fm:
                        seg0 = max(s0 for (s0, n) in segs if s0 <= c0)
                        wo = segoff[seg0] + (c0 - seg0)
                        ps = ps_main[cnt["main"] % 3]
                        cnt["main"] += 1
                        for c in range(16):
                            sch.op("pe", lambda e, c=c, ps=ps, wo=wo: e.matmul(ps.t[:], wsb.t[:, c, wo:wo + 128], ht.t[:, c, :],
                                                                               start=(c == 0), stop=(c == 15)),
                                   reads=[wsb, ht.reg(c)], writes=[ps])
                        def _ep(kind=kind, gsb=gsb, dst=dst, ps=ps):
                            i2 = cnt["ep"] % 2
                            cnt["ep"] += 1
                            xg, sq2, r2, t1, t2 = xgs[i2], sq2s[i2], rs2[i2], t1s[i2], t2s[i2]
                            ob = outs[cnt["out"] % 3]
                            cho = ch_out[cnt["out"] % 3]
                            cnt["out"] += 1
                            if kind == "A":
                                cosb, sinb = csb[0], csb[1]
                                sch.op("act", lambda e, ps=ps, sq2=sq2: e.activation(out=sq2.t[:], in_=ps.t[:], func=AF.Square),
                                       reads=[ps], writes=[sq2])
                                sch.op("act", lambda e, ps=ps, xg=xg, gsb=gsb: e.activation(out=xg.t[:], in_=ps.t[:], func=AF.Copy,
                                                                                           scale=gsb.t[:, 0:1]),
                                       reads=[ps, gsb], writes=[xg])
                                sch.op("pe", lambda e, sq2=sq2: e.matmul(ps_ss2.t[:], ones_bf.t[:], sq2.t[:], start=True, stop=True),
                                       reads=[ones_bf, sq2], writes=[ps_ss2])
                            else:
                                cosb, sinb = csb[2], csb[3]
                                sch.op("act", lambda e, ps=ps, xg=xg: e.activation(out=xg.t[:], in_=ps.t[:], func=AF.Copy),
                                       reads=[ps], writes=[xg])
                            sch.op("pe", lambda e, xg=xg: e.matmul(ps_rot.t[:], rmat_bf.t[:], xg.t[:], start=True, stop=True),
                                   reads=[rmat_bf, xg], writes=[ps_rot])
                            sch.op("pool", lambda e, xg=xg, t1=t1, cosb=cosb: e.tensor_tensor(out=t1.t[:], in0=xg.t[:], in1=cosb.t[:], op=ALU.mult),
                                   reads=[xg, cosb], writes=[t1])
                            sch.op("dve", lambda e, t2=t2, sinb=sinb: e.tensor_tensor(out=t2.t[:], in0=ps_rot.t[:], in1=sinb.t[:], op=ALU.mult),
                                   reads=[ps_rot, sinb], writes=[t2])
                            if kind == "A":
                                sch.op("act", lambda e, r2=r2: e.activation(out=r2.t[:], in_=ps_ss2.t[:], func=AF.Ln, scale=1.0 / 128, bias=eps_sb.t[:, 0:1]),
                                       reads=[ps_ss2, eps_sb], writes=[r2])
                                sch.op("act", lambda e, r2=r2: e.activation(out=r2.t[:], in_=r2.t[:], func=AF.Exp, scale=-0.5),
                                       reads=[r2], writes=[r2])
                                sch.op("pool", lambda e, t1=t1, t2=t2: e.tensor_tensor(out=t1.t[:], in0=t1.t[:], in1=t2.t[:], op=ALU.add),
                                       reads=[t1, t2], writes=[t1])
                                sch.op("pool", lambda e, t1=t1, r2=r2, ob=ob: e.tensor_tensor(out=ob.t[:], in0=t1.t[:], in1=r2.t[:], op=ALU.mult),
                                       reads=[t1, r2], writes=[ob])
                            else:
                                sch.op("pool", lambda e, t1=t1, t2=t2, ob=ob: e.tensor_tensor(out=ob.t[:], in0=t1.t[:], in1=t2.t[:], op=ALU.add),
                                       reads=[t1, t2], writes=[ob])
                            sch.dma("sp", cho, dst, ob.t[:], reads=[ob])
                            nfm[0] += 1
                            if t + 1 < ntiles:
                                prep_step(t + 1, nfm[0])
                        if pend[0] is not None:
                            pend[0]()
                        pend[0] = _ep

                    pend[0]()
                    pend[0] = None
                    if passno == 1:
                        for j in range(4):
                            rows = slice(t * 512 + j * 128, t * 512 + (j + 1) * 128)
                            for (c0, n, dstt, d0) in [(1280, 256, VA, 0), (3584, 512, VB, 0), (4096, 512, VB, 512)]:
                                seg0 = max(s0 for (s0, nn) in segs if s0 <= c0)
                                wo = segoff[seg0] + (c0 - seg0)
                                ps = ps_main[cnt["main"] % 3]
                                cnt["main"] += 1
                                for c in range(16):
                                    sch.op("pe", lambda e, c=c, ps=ps, wo=wo, n=n, j=j: e.matmul(ps.t[:, 0:n], ht.t[:, c, j * 128:(j + 1) * 128],
                                                                                                 wsb.t[:, c, wo:wo + n], start=(c == 0), stop=(c == 15)),
                                           reads=[wsb, ht.reg(c)], writes=[ps])
                                vt = vts[cnt["vt"] % 3]
                                chv = ch_vt[cnt["vt"] % 3]
                                cnt["vt"] += 1
                                sch.op("act", lambda e, ps=ps, vt=vt, n=n: e.activation(out=vt.t[:, 0:n], in_=ps.t[:, 0:n], func=AF.Copy),
                                       reads=[ps], writes=[vt])
                                sch.dma("sp", chv, dstt[rows, d0:d0 + n], vt.t[:, 0:n], reads=[vt])
                for n_ in range(1, 8):
                    prep_step(0, n_)
                for t in range(ntiles):
                    tile_main(t)
                sch.barrier()

        phaseA(1)
        if stop_after == "A1":
            return nc
        phaseA(2)
        if stop_after == "A2":
            return nc

        lam_init = 0.8 - 0.6 * float(np.exp(-0.3 * 0))
        neglam = P0.sb("neglam", [128, 1], F32)
        ones_f = P0.sb("ones_f", [1, 128], F32)
        lam_sb = P0.sb("lam_sb", [1, 256], F32)
        lam_t = P0.sb("lam_t", [1, 8], F32)
        sch.op("dve", lambda e: e.memset(ones_f.t[:], 1.0), writes=[ones_f])
        sch.dma("sp", ch_c, lam_sb.t[:], lam4.rearrange("(o a) d -> o (a d)", o=1), writes=[lam_sb])
        sch.op("dve", lambda e: e.tensor_tensor(out=lam_sb.t[0:1, 0:64], in0=lam_sb.t[0:1, 0:64], in1=lam_sb.t[0:1, 64:128], op=ALU.mult),
               reads=[lam_sb], writes=[lam_sb])
        sch.op("dve", lambda e: e.tensor_tensor(out=lam_sb.t[0:1, 128:192], in0=lam_sb.t[0:1, 128:192], in1=lam_sb.t[0:1, 192:256], op=ALU.mult),
               reads=[lam_sb], writes=[lam_sb])
        sch.op("dve", lambda e: e.reduce_sum(out=lam_t.t[0:1, 0:1], in_=lam_sb.t[0:1, 0:64], axis=AX.X), reads=[lam_sb], writes=[lam_t])
        sch.op("dve", lambda e: e.reduce_sum(out=lam_t.t[0:1, 1:2], in_=lam_sb.t[0:1, 128:192], axis=AX.X), reads=[lam_sb], writes=[lam_t])
        sch.op("act", lambda e: e.activation(out=lam_t.t[0:1, 2:4], in_=lam_t.t[0:1, 0:2], func=AF.Exp), reads=[lam_t], writes=[lam_t])
        sch.op("dve", lambda e: e.tensor_tensor(out=lam_t.t[0:1, 4:5], in0=lam_t.t[0:1, 3:4], in1=lam_t.t[0:1, 2:3], op=ALU.subtract),
               reads=[lam_t], writes=[lam_t])
        sch.op("dve", lambda e: e.tensor_scalar(out=lam_t.t[0:1, 5:6], in0=lam_t.t[0:1, 4:5], scalar1=-lam_init, scalar2=None, op0=ALU.add),
               reads=[lam_t], writes=[lam_t])
        sch.op("pe", lambda e: e.matmul(PS[7].t[:, 0:1], ones_f.t[0:1, :], lam_t.t[0:1, 5:6], start=True, stop=True),
               reads=[ones_f, lam_t], writes=[PS[7]])
        sch.op("dve", lambda e: e.tensor_copy(out=neglam.t[:], in_=PS[7].t[:, 0:1]), reads=[PS[7]], writes=[neglam])

        OT_v = OT.rearrange("(c p) t -> c p t", p=128)
        SCALE_A = 128.0 ** -0.5
        SCALE_B = 64.0 ** -0.5

        def attention(kind):
            pes = contextlib.ExitStack()
            with pes:
                P = Pool_(nc, pes)
                psS = []
                for i in range(2):
                    t = pes.enter_context(nc.psum_tensor("psS%s%d" % (kind, i), [128, 1024], F32))
                    psS.append(Buf("psS%d" % i, t))
                psO = []
                for i in range(2):
                    t = pes.enter_context(nc.psum_tensor("psO%s%d" % (kind, i), [128, 512], F32))
                    psO.append(Buf("psO%d" % i, t))
                LCOL = LCOL_FLAG
                t = pes.enter_context(nc.psum_tensor("psL%s" % kind, [128, 512], F32))
                psL = Buf("psL", t)
                t = pes.enter_context(nc.psum_tensor("psRr%s" % kind, [128, 512], F32))
                psR = Buf("psR", t)
                psL2 = [psL, psR]
                sel64 = P.sb("sel64", [64, 128], F32)
                sch.op("pool", lambda e: e.memset(sel64.t[:], 1.0 / 32.0), writes=[sel64])
                lsbs = [P.sb("lsb%d" % i, [64, 512], F32) for i in range(2)]
                accs = [[P.sb("acc%d_%d" % (i, j), [128, 1024], F32) for j in range(2)] for i in range(2)]
                onesF_ = P.sb("onesFa", [128, 32], F32)
                sch.op("pool", lambda e: e.memset(onesF_.t[:], 1.0), writes=[onesF_])
                KTs = [P.sb("KT%d" % i, [128, S], BF16) for i in range(2)]
                Vs = [P.sb("V%d" % i, [128, 64, 128], BF16) for i in range(2)]
                QTs = [P.sb("QT%d" % i, [128, OWN], BF16) for i in range(2)]
                pTs = [P.sb("pT%d" % i, [128, 1024], BF16) for i in range(3)]
                rls = [P.sb("rl%d" % i, [128, 512], F32) for i in range(2)]
                d0s = [P.sb("d0%d" % i, [128, 512], F32) for i in range(2)]
                obs = [P.sb("ob%d" % i, [128, 512], BF16) for i in range(2)]
                ch_k = [sch.chan("k%d" % i) for i in range(2)]
                ch_v = [sch.chan("v%d" % i) for i in range(2)]
                ch_q = [sch.chan("q%d" % i) for i in range(2)]
                ch_o = [sch.chan("o%d" % i) for i in range(2)]
                NST = 32 if kind == "A" else 64
                LAG = 2

                def load_kv(g):
                    kb, vb = KTs[g % 2], Vs[g % 2]
                    if kind == "A":
                        src_k = KAT[g, :, :]
                        src_v = VA.rearrange("(kc p) c -> p kc c", p=128)[:, :, g * 128:(g + 1) * 128]
                    else:
                        src_k = KBT[g, :, :]
                        src_v = VB.rearrange("(kc p) c -> p kc c", p=128)[:, :, g * 128:(g + 1) * 128]
                    sch.dma("sp", ch_k[g % 2], kb.t[:], src_k, writes=[kb])
                    sch.dma("sp", ch_v[g % 2], vb.t[:], src_v, writes=[vb])

                def load_q(h):
                    src = QAT[h, :, :] if kind == "A" else QBT[h, :, :]
                    sch.dma("sp", ch_q[h % 2], QTs[h % 2].t[:], src, writes=[QTs[h % 2]])

                def kvidx(h):
                    return h // 4 if kind == "A" else h

                steps = [(h, qt, st) for h in range(8) for qt in range(8) for st in range(NST)]
                N = len(steps)
                load_kv(0)
                load_q(0)
                if kind == "B":
                    emit_conversions()
                epi_n = [0]

                def emit_S(i):
                    h, qt, st = steps[i]
                    g = kvidx(h)
                    kb, qb = KTs[g % 2], QTs[h % 2]
                    ps = psS[i % 2]
                    pT = pTs[i % 3]
                    for m in range(2):
                        if kind == "A":
                            kc = 2 * st + m
                            lhsT = kb.t[:, kc * 128:(kc + 1) * 128]
                            rhs = qb.t[:, qt * 512:(qt + 1) * 512]
                        else:
                            kc = st
                            lhsT = kb.t[m * 64:(m + 1) * 64, kc * 128:(kc + 1) * 128]
                            rhs = qb.t[m * 64:(m + 1) * 64, qt * 512:(qt + 1) * 512]
                        sch.op("pe", lambda e, ps=ps, lhsT=lhsT, rhs=rhs, m=m: e.matmul(ps.t[:, m * 512:(m + 1) * 512], lhsT, rhs, start=True, stop=True),
                               reads=[kb, qb], writes=[ps])
                    sc = SCALE_A if kind == "A" else SCALE_B
                    sch.op("act", lambda e, ps=ps, pT=pT, sc=sc: e.activation(out=pT.t[:], in_=ps.t[:], func=AF.Exp, scale=sc),
                           reads=[ps], writes=[pT])

                def emit_PV(i):
                    h, qt, st = steps[i]
                    g = kvidx(h)
                    if qt == 0 and st == 0 and h + 1 < 8:
                        if kvidx(h + 1) != g:
                            load_kv(kvidx(h + 1))
                        load_q(h + 1)
                    vb = Vs[g % 2]
                    pT = pTs[i % 3]
                    for m in range(2):
                        if kind == "A":
                            kc = 2 * st + m
                            po = psO[qt % 2]
                            first, last = (st == 0 and m == 0), (st == NST - 1 and m == 1)
                        else:
                            kc = st
                            po = psO[m]
                            first, last = (st == 0), (st == NST - 1)
                        sch.op("pe", lambda e, po=po, pT=pT, kc=kc, m=m, first=first, last=last: e.matmul(po.t[:], vb.t[:, kc, :], pT.t[:, m * 512:(m + 1) * 512],
                                                                                                          start=first, stop=last),
                               reads=[vb, pT], writes=[po])
                    accD, accP = accs[qt % 2]
                    if st % 2 == 0:
                        for m in range(2):
                            sch.op("pe", lambda e, pT=pT, m=m: e.matmul(psL.t[32 * m:32 * m + 32, :], ones_bf.t[:, 0:32], pT.t[:, m * 512:(m + 1) * 512],
                                                                        start=(st == 0), stop=False, tile_position=(0, 32 * m)),
                                   reads=[ones_bf, pT], writes=[psL])
                    else:
                        en, acc = ("dve", accD) if st % 4 == 1 else ("pool", accP)
                        if st < 4:
                            sch.op(en, lambda e, acc=acc, pT=pT: e.tensor_copy(out=acc.t[:], in_=pT.t[:]), reads=[pT], writes=[acc])
                        else:
                            sch.op(en, lambda e, acc=acc, pT=pT: e.tensor_tensor(out=acc.t[:], in0=acc.t[:], in1=pT.t[:], op=ALU.add),
                                   reads=[acc, pT], writes=[acc])
                    if st == NST - 1:
                        for m in range(2):
                            for ai, acc in enumerate((accD, accP)):
                                sch.op("pe", lambda e, acc=acc, m=m, ai=ai: e.matmul(psL.t[32 * m:32 * m + 32, :], onesF_.t[:, 0:32], acc.t[:, m * 512:(m + 1) * 512],
                                                                                      start=False, stop=(ai == 1), tile_position=(0, 32 * m)),
                                       reads=[onesF_, acc], writes=[psL])
                    if st == NST - 1:
                        k2 = epi_n[0] % 2
                        epi_n[0] += 1
                        ob, rl, d0, lsb = obs[k2], rls[k2], d0s[k2], lsbs[k2]
                        if LCOL:
                            sch.op("dve", lambda e, lsb=lsb: e.tensor_copy(out=lsb.t[0:64, :], in_=psL.t[0:64, :]), reads=[psL], writes=[lsb])
                        if kind == "A":
                            po = psO[qt % 2]
                            if LCOL:
                                sch.op("pe", lambda e, lsb=lsb: e.matmul(psR.t[:], sel64.t[0:64, :], lsb.t[0:64, :], start=True, stop=True),
                                       reads=[sel64, lsb], writes=[psR])
                                sch.op("dve", lambda e, rl=rl: e.reciprocal(out=rl.t[:], in_=psR.t[:]), reads=[psR], writes=[rl])
                            else:
                                sch.op("dve", lambda e, d0=d0: e.tensor_copy(out=d0.t[:], in_=psL2[0].t[:]), reads=[psL2[0]], writes=[d0])
                                sch.op("dve", lambda e, d0=d0, rl=rl: e.tensor_tensor(out=rl.t[:], in0=psL2[1].t[:], in1=d0.t[:], op=ALU.add), reads=[psL2[1], d0], writes=[rl])
                                sch.op("dve", lambda e, rl=rl: e.reciprocal(out=rl.t[:], in_=rl.t[:]), reads=[rl], writes=[rl])
                            sch.op("dve", lambda e, ob=ob, po=po, rl=rl: e.tensor_tensor(out=ob.t[:], in0=po.t[:], in1=rl.t[:], op=ALU.mult),
                                   reads=[po, rl], writes=[ob])
                            row = h
                        else:
                            for m in range(2):
                                tgt = d0 if m == 0 else rl
                                if LCOL:
                                    sch.op("pe", lambda e, lsb=lsb, m=m: e.matmul(psR.t[:], sel64.t[32 * m:32 * m + 32, :], lsb.t[32 * m:32 * m + 32, :], start=True, stop=True),
                                           reads=[sel64, lsb], writes=[psR])
                                    sch.op("dve", lambda e, rl=rl: e.reciprocal(out=rl.t[:], in_=psR.t[:]), reads=[psR], writes=[rl])
                                else:
                                    sch.op("dve", lambda e, rl=rl, m=m: e.reciprocal(out=rl.t[:], in_=psL2[m].t[:]), reads=[psL2[m]], writes=[rl])
                                sch.op("dve", lambda e, tgt=tgt, rl=rl, m=m: e.tensor_tensor(out=tgt.t[:], in0=psO[m].t[:], in1=rl.t[:], op=ALU.mult),
                                       reads=[psO[m], rl], writes=[tgt])
                            sch.op("dve", lambda e, ob=ob, rl=rl, d0=d0: e.scalar_tensor_tensor(out=ob.t[:], in0=rl.t[:], scalar=neglam.t[:, 0:1],
                                                                                                  in1=d0.t[:], op0=ALU.mult, op1=ALU.add),
                                   reads=[rl, d0, neglam], writes=[ob])
                            row = 8 + h
                        sch.dma("sp", ch_o[k2], OT_v[row, :, qt * 512:(qt + 1) * 512], ob.t[:], reads=[ob])

                for i in range(N + LAG):
                    if i < N:
                        emit_S(i)
                    if i >= LAG:
                        emit_PV(i - LAG)
                sch.barrier()


        def emit_conversions():
            ch = sch.chan("conv", sw=True)
            wa_v = w_branch_a.rearrange("(c p) n -> p c n", p=128)
            wb_v = w_branch_b.rearrange("(c p) n -> p c n", p=128)
            wg_v = w_gate.rearrange("(c p) n -> p c n", p=128)
            wo_v = w_out.rearrange("(c p) n -> p c n", p=128)
            for grp in range(8):
                cs_ = slice(grp * 256, (grp + 1) * 256)
                cs2 = slice(D + grp * 256, D + (grp + 1) * 256)
                for (off, nk, src) in ((0, 8, wa_v[:, :, cs_]), (2048, 8, wb_v[:, :, cs_]), (4096, 16, wg_v[:, :, cs_]), (8192, 16, wg_v[:, :, cs2])):
                    sch.dma("pool", ch, WD1[grp, :, off:off + nk * 256].rearrange("p (k n) -> p k n", n=256), src)
            for n in range(4):
                for c4 in range(4):
                    sch.dma("pool", ch, WO16[n, :, c4 * 2048:(c4 + 1) * 2048].rearrange("p (k n) -> p k n", n=512),
                            wo_v[:, 4 * c4:4 * c4 + 4, n * 512:(n + 1) * 512])
            for e_ in range(NEXP):
                rows = slice(e_ * 128, (e_ + 1) * 128)
                for (dstw, srcw, cc) in ((WG16, w_e_gate, 16), (WU16, w_e_up, 16), (WD16, w_e_down, 4)):
                    srcv = srcw[e_].rearrange("(p c) n -> p (c n)", c=cc)
                    for a_ in range(4):
                        sch.dma("pool", ch, dstw[rows, a_ * 2048:(a_ + 1) * 2048], srcv[:, a_ * 2048:(a_ + 1) * 2048])

        sch.barrier()
        ps_es.close()
        attention("A")
        if stop_after == "BA":
            return nc
        attention("B")
        if stop_after == "BB":
            return nc
        ps_es = contextlib.ExitStack()
        es.enter_context(ps_es)
        del PS[:]
        for i in range(8):
            t = ps_es.enter_context(nc.psum_tensor("psd%d" % i, [128, 512], F32))
            PS.append(Buf("psd%d" % i, t))

        sg_sb = P0.sb("sg_sb", [128, 1], F32)
        bg_sb = P0.sb("bg_sb", [128, 32], F32)
        sch.dma("sp", ch_c, sg_sb.t[:], subln_b.rearrange("(p o) -> p o", o=1), writes=[sg_sb])
        sch.dma("sp", ch_c, bg_sb.t[:], b_gate.rearrange("(c p) -> p c", p=128), writes=[bg_sb], allow_slow_non_contiguous=True)
        sch.op("dve", lambda e: e.tensor_scalar(out=sg_sb.t[:], in0=sg_sb.t[:], scalar1=(1.0 - lam_init), scalar2=0.0,
                                                op0=ALU.mult, op1=ALU.add), reads=[sg_sb], writes=[sg_sb])

        def phaseD1():
            pes = contextlib.ExitStack()
            with pes:
                P = Pool_(nc, pes)
                TT = 1024
                ht = P.sb("d1ht", [128, 16, TT], BF16)
                ot = P.sb("d1ot", [128, 16, TT], BF16)
                mt = P.sb("d1mt", [128, 16, TT], BF16)
                wgrp = []
                for i in range(2):
                    wgrp.append(dict(wa=P.sb("wa%d" % i, [128, 8, 256], BF16), wb=P.sb("wb%d" % i, [128, 8, 256], BF16),
                                     wga=P.sb("wga%d" % i, [128, 16, 256], BF16), wgb=P.sb("wgb%d" % i, [128, 16, 256], BF16)))
                ch_wg = [sch.chan("d1w%d" % i) for i in range(2)]
                ch_in = sch.chan("d1in")
                ch_mt = sch.chan("d1mt")
                sqs = [P.sb("d1sq%d" % i, [128, 512], BF16) for i in range(2)]
                rs = [P.sb("d1rs%d" % i, [128, 512], F32) for i in range(2)]
                sga = [P.sb("d1sga%d" % i, [128, 512], F32) for i in range(2)]
                sgb = [P.sb("d1sgb%d" % i, [128, 512], F32) for i in range(2)]
                m1s = [P.sb("d1m1%d" % i, [128, 512], F32) for i in range(2)]
                m2s = [P.sb("d1m2%d" % i, [128, 512], F32) for i in range(2)]
                HT_v = HT.rearrange("(c p) t -> p c t", p=128)
                OTv = OT.rearrange("(c p) t -> p c t", p=128)
                MT_v = MT.rearrange("(c p) t -> p c t", p=128)
                wa_v = w_branch_a.rearrange("(c p) n -> p c n", p=128)
                wb_v = w_branch_b.rearrange("(c p) n -> p c n", p=128)
                wg_v = w_gate.rearrange("(c p) n -> p c n", p=128)
                gcount = 0
                ep = 0
                for tt in range(OWN // TT):
                    tk = slice(tt * TT, (tt + 1) * TT)
                    for c4 in range(4):
                        sch.dma("sp", ch_in, ht.t[:, 4 * c4:4 * c4 + 4, :], HT_v[:, 4 * c4:4 * c4 + 4, tk], writes=[ht.reg(c4)])
                        sch.dma("sp", ch_in, ot.t[:, 4 * c4:4 * c4 + 4, :], OTv[:, 4 * c4:4 * c4 + 4, tk], writes=[ot.reg(c4)])
                    sch.fence(ch_in, ht)
                    sch.fence(ch_in, ot)
                    for c in range(8, 16):
                        for hh in range(TT // 512):
                            i2 = ep % 2
                            ep += 1
                            sl = slice(hh * 512, (hh + 1) * 512)
                            sq, r_ = sqs[i2], rs[i2]
                            pss = PS[i2]
                            sch.op("act", lambda e, sq=sq, c=c, sl=sl: e.activation(out=sq.t[:], in_=ot.t[:, c, sl], func=AF.Square),
                                   reads=[ot.reg(c // 4)], writes=[sq])
                            sch.op("pe", lambda e, pss=pss, sq=sq: e.matmul(pss.t[:], ones_bf.t[:], sq.t[:], start=True, stop=True),
                                   reads=[ones_bf, sq], writes=[pss])
                            sch.op("act", lambda e, r_=r_, pss=pss: e.activation(out=r_.t[:], in_=pss.t[:], func=AF.Ln, scale=1.0 / 128, bias=eps_sb.t[:, 0:1]),
                                   reads=[pss, eps_sb], writes=[r_])
                            sch.op("act", lambda e, r_=r_: e.activation(out=r_.t[:], in_=r_.t[:], func=AF.Exp, scale=-0.5), reads=[r_], writes=[r_])
                            sch.op("dve", lambda e, r_=r_, c=c, sl=sl: e.scalar_tensor_tensor(out=ot.t[:, c, sl], in0=ot.t[:, c, sl], scalar=sg_sb.t[:, 0:1],
                                                                                                in1=r_.t[:], op0=ALU.mult, op1=ALU.mult),
                                   reads=[ot.reg(c // 4), sg_sb, r_], writes=[ot.reg(c // 4)])
                    for grp in range(8):
                        wg = wgrp[gcount % 2]
                        chw = ch_wg[gcount % 2]
                        gcount += 1
                        cs_ = slice(grp * 256, (grp + 1) * 256)
                        cs2 = slice(D + grp * 256, D + (grp + 1) * 256)
                        sch.dma("sp", chw, wg["wa"].t[:], WD1[grp, :, 0:2048].rearrange("p (k n) -> p k n", n=256), writes=[wg["wa"]])
                        sch.dma("sp", chw, wg["wb"].t[:], WD1[grp, :, 2048:4096].rearrange("p (k n) -> p k n", n=256), writes=[wg["wb"]])
                        sch.dma("sp", chw, wg["wga"].t[:], WD1[grp, :, 4096:8192].rearrange("p (k n) -> p k n", n=256), writes=[wg["wga"]])
                        sch.dma("sp", chw, wg["wgb"].t[:], WD1[grp, :, 8192:12288].rearrange("p (k n) -> p k n", n=256), writes=[wg["wgb"]])
                        for nm in ("wa", "wb", "wga", "wgb"):
                            wg[nm].w = {chw: sch.cnt[chw]}
                        for jj in range(2):
                            j = grp * 2 + jj
                            wsl = slice(jj * 128, (jj + 1) * 128)
                            for hh in range(TT // 512):
                                sl = slice(hh * 512, (hh + 1) * 512)
                                i2 = ep % 2
                                ep += 1
                                pa, pb, ga, gb = PS[4 * i2], PS[1 + 4 * i2], PS[2 + 4 * i2], PS[3 + 4 * i2]
                                for k in range(8):
                                    sch.op("pe", lambda e, k=k, pa=pa, wsl=wsl, sl=sl: e.matmul(pa.t[:], wg["wa"].t[:, k, wsl], ot.t[:, k, sl], start=(k == 0), stop=(k == 7)),
                                           reads=[wg["wa"], ot.reg(k // 4)], writes=[pa])
                                for k in range(8):
                                    sch.op("pe", lambda e, k=k, pb=pb, wsl=wsl, sl=sl: e.matmul(pb.t[:], wg["wb"].t[:, k, wsl], ot.t[:, 8 + k, sl], start=(k == 0), stop=(k == 7)),
                                           reads=[wg["wb"], ot.reg(2 + k // 4)], writes=[pb])
                                for k in range(16):
                                    sch.op("pe", lambda e, k=k, ga=ga, wsl=wsl, sl=sl: e.matmul(ga.t[:], wg["wga"].t[:, k, wsl], ht.t[:, k, sl], start=(k == 0), stop=(k == 15)),
                                           reads=[wg["wga"], ht.reg(k // 4)], writes=[ga])
                                for k in range(16):
                                    sch.op("pe", lambda e, k=k, gb=gb, wsl=wsl, sl=sl: e.matmul(gb.t[:], wg["wgb"].t[:, k, wsl], ht.t[:, k, sl], start=(k == 0), stop=(k == 15)),
                                           reads=[wg["wgb"], ht.reg(k // 4)], writes=[gb])
                                sa, sb_, m1, m2 = sga[i2], sgb[i2], m1s[i2], m2s[i2]
                                sch.op("act", lambda e, sa=sa, ga=ga, j=j: e.activation(out=sa.t[:], in_=ga.t[:], func=AF.Sigmoid, bias=bg_sb.t[:, j:j + 1]),
                                       reads=[ga, bg_sb], writes=[sa])
                                sch.op("act", lambda e, sb_=sb_, gb=gb, j=j: e.activation(out=sb_.t[:], in_=gb.t[:], func=AF.Sigmoid, bias=bg_sb.t[:, 16 + j:17 + j]),
                                       reads=[gb, bg_sb], writes=[sb_])
                                sch.op("dve", lambda e, m1=m1, sa=sa, pa=pa: e.tensor_tensor(out=m1.t[:], in0=pa.t[:], in1=sa.t[:], op=ALU.mult),
                                       reads=[pa, sa], writes=[m1])
                                sch.op("dve", lambda e, m2=m2, sb_=sb_, pb=pb: e.tensor_tensor(out=m2.t[:], in0=pb.t[:], in1=sb_.t[:], op=ALU.mult),
                                       reads=[pb, sb_], writes=[m2])
                                sch.op("pool", lambda e, m1=m1, m2=m2, j=j, sl=sl: e.tensor_tensor(out=mt.t[:, j, sl], in0=m1.t[:], in1=m2.t[:], op=ALU.add),
                                       reads=[m1, m2], writes=[mt.reg(j // 4)])
                    for c4 in range(4):
                        sch.dma("sp", ch_mt, MT_v[:, 4 * c4:4 * c4 + 4, tk], mt.t[:, 4 * c4:4 * c4 + 4, :], reads=[mt.reg(c4)])
                    sch.fence(ch_mt, mt, readers=True)
                sch.barrier()

        phaseD1()
        if stop_after == "D1":
            return nc

        def phaseD2():
            pes = contextlib.ExitStack()
            with pes:
                P = Pool_(nc, pes)
                mt = P.sb("d2mt", [128, 16, OWN], BF16)
                wos = [P.sb("d2wo%d" % i, [128, 16, 512], BF16) for i in range(2)]
                xr = [P.sb("d2xr%d" % i, [128, 512], F32) for i in range(3)]
                ch_m = sch.chan("d2m")
                ch_w = [sch.chan("d2w%d" % i) for i in range(2)]
                ch_x = [sch.chan("d2x%d" % i) for i in range(3)]
                ch_s = [sch.chan("d2s%d" % i) for i in range(3)]
                MT_v = MT.rearrange("(c p) t -> p c t", p=128)
                wo_v = w_out.rearrange("(c p) n -> p c n", p=128)
                for c in range(16):
                    sch.dma("sp", ch_m, mt.t[:, c, :], MT_v[:, c, :], writes=[mt.reg(c)])
                sch.fence(ch_m, mt)
                it = 0
                for n in range(4):
                    wo = wos[n % 2]
                    ns = slice(n * 512, (n + 1) * 512)
                    sch.dma("sp", ch_w[n % 2], wo.t[:], WO16[n].rearrange("p (k n) -> p k n", n=512), writes=[wo])
                    for ts in range(32):
                        rows = slice(ts * 128, (ts + 1) * 128)
                        ps = PS[it % 4]
                        x_ = xr[it % 3]
                        chx, chs = ch_x[it % 3], ch_s[it % 3]
                        it += 1
                        sch.dma("sp", chx, x_.t[:], x_own[rows, ns], writes=[x_])
                        for k in range(16):
                            sch.op("pe", lambda e, k=k, ps=ps, rows=rows, wo=wo: e.matmul(ps.t[:], mt.t[:, k, rows], wo.t[:, k, :], start=(k == 0), stop=(k == 15)),
                                   reads=[mt, wo], writes=[ps])
                        sch.op("dve", lambda e, x_=x_, ps=ps: e.tensor_tensor(out=x_.t[:], in0=ps.t[:], in1=x_.t[:], op=ALU.add),
                               reads=[ps, x_], writes=[x_])
                        sch.dma("sp", chs, X1[rows, ns], x_.t[:], reads=[x_])
                sch.barrier()

        phaseD2()
        if stop_after == "D2":
            return nc

        ps_es.close()
        ps_es2 = contextlib.ExitStack()
        es.enter_context(ps_es2)
        PF = []
        for i in range(6):
            t = ps_es2.enter_context(nc.psum_tensor("pf%d" % i, [128, 512], F32))
            PF.append(Buf("pf%d" % i, t))
        PB = []
        for i in range(2):
            t = ps_es2.enter_context(nc.psum_tensor("pb%d" % i, [128, 1024], BF16))
            PB.append(Buf("pb%d" % i, t))

        bc_pos = nc.gpsimd.to_reg(NPOS - 1)
        bc_w = nc.gpsimd.to_reg(NEXP * 128 - 1)
        posi = P0.sb("posi", [128, 64], I32)
        wts = P0.sb("wts", [128, 64], F32)
        widx = P0.sb("widx", [128, 128], I32)
        XB_ROWS = XBUF

        def phaseE():
            pes = contextlib.ExitStack()
            with pes:
                P = Pool_(nc, pes)
                H2 = P.sb("H2", [128, 32, D], BF16)
                gffn_b = P.sb("gffn_b", [128, D], F32)
                wr_sb = P.sb("wr_sb", [128, 16, 36], F32)
                ustr = P.sb("ustr", [128, 128], F32)
                onesF = P.sb("onesF", [128, 128], F32)
                thr_sb = P.sb("thr_sb", [128, 64], F32)
                cst_sb = P.sb("cst_sb", [128, 64], F32)
                x1b = P.sb("x1b", [128, D], F32)
                h2f = P.sb("h2f", [128, D], F32)
                h2T = P.sb("h2T", [128, D], F32)
                M1all = P.sb("M1all", [128, 32, 32], F32)
                M2all = P.sb("M2all", [128, 32, 32], F32)
                rank_all = P.sb("rank_all", [128, 32, 32], F32)
                Msum = P.sb("Msum", [128, 32], F32)
                Mt = P.sb("Mt", [128, 32], F32)
                lgt = P.sb("lgt", [128, 36], F32)
                sm = P.sb("sm", [128, 64], F32)
                esel = P.sb("esel", [128, 8], F32)
                esel2 = P.sb("esel2", [128, 8], F32)
                oh1 = P.sb("oh1", [128, 8], F32)
                oh2 = P.sb("oh2", [128, 8], F32)
                goh = P.sb("goh", [128, 4], F32)
                junk4 = P.sb("junk4", [128, 4], F32)
                posf = P.sb("posf", [128, 64], F32)
                tmp32 = P.sb("tmp32", [128, 32], F32)
                tmp32b = P.sb("tmp32b", [128, 32], F32)
                offs_sb = P.sb("offs_sb", [128, 32], F32)
                pend_sb = P.sb("pend_sb", [128, 32], F32)
                padrep = P.sb("padrep", [32, 128], F32)
                cmp64 = P.sb("cmp64", [32, 64], F32)
                ch_e = sch.chan("e_c")
                ch_x1 = sch.chan("e_x1")
                ch_sc = sch.chan("e_sc", sw=True)
                sch.dma("sp", ch_e, gffn_b.t[:], g_ffn.rearrange("(o n) -> o n", o=1).partition_broadcast(128), writes=[gffn_b])
                sch.dma("sp", ch_e, wr_sb.t[:], w_router.rearrange("(c p) n -> p c n", p=128), writes=[wr_sb])
                sch.dma("sp", ch_e, ustr.t[:], ustrict[:, :], writes=[ustr])
                sch.dma("sp", ch_e, thr_sb.t[:], thr[:, :], writes=[thr_sb])
                sch.dma("sp", ch_e, cst_sb.t[:], consts[:, :], writes=[cst_sb])
                for b_ in (gffn_b, wr_sb, ustr, thr_sb, cst_sb):
                    b_.w = {ch_e: sch.cnt[ch_e]}
                sch.op("dve", lambda e: e.memset(onesF.t[:], 1.0), writes=[onesF])
                sch.op("dve", lambda e: e.memset(Msum.t[:], 0.0), writes=[Msum])

                def dv(fn, reads, writes, eng="dve"):
                    sch.op(eng, fn, reads=reads, writes=writes)

                def col(i):
                    return sm.t[:, i:i + 1]

                for t in range(32):
                    rows = slice(t * 128, (t + 1) * 128)
                    sch.dma("sp", ch_x1, x1b.t[:], X1[rows, :], writes=[x1b])
                    dv(lambda e: e.memset(sm.t[:, 0:16], 0.0), [], [sm])
                    dv(lambda e: e.activation(out=h2f.t[:], in_=x1b.t[:], func=AF.Square, accum_out=col(0)), [x1b, sm], [h2f, sm], "act")
                    dv(lambda e: e.activation(out=col(1), in_=col(0), func=AF.Ln, scale=1.0 / D, bias=eps_sb.t[:, 0:1]), [sm, eps_sb], [sm], "act")
                    dv(lambda e: e.activation(out=col(2), in_=col(1), func=AF.Exp, scale=-0.5), [sm], [sm], "act")
                    dv(lambda e: e.scalar_tensor_tensor(out=h2f.t[:], in0=x1b.t[:], scalar=col(2), in1=gffn_b.t[:], op0=ALU.mult, op1=ALU.mult),
                       [x1b, sm, gffn_b], [h2f])
                    dv(lambda e, t=t: e.tensor_copy(out=H2.t[:, t, :], in_=h2f.t[:]), [h2f], [H2.reg(t)], "pool")
                    for g4 in range(4):
                        pst = PF[g4 % 2]
                        for j in range(4):
                            c = g4 * 4 + j
                            dv(lambda e, pst=pst, j=j, c=c: e.transpose(out=pst.t[:, j * 128:(j + 1) * 128], in_=h2f.t[:, c * 128:(c + 1) * 128], identity=ident_f.t[:]),
                               [h2f, ident_f], [pst], "pe")
                        dv(lambda e, pst=pst, g4=g4: e.activation(out=h2T.t[:, g4 * 512:(g4 + 1) * 512], in_=pst.t[:], func=AF.Copy), [pst], [h2T.reg(g4)], "act")
                    psl = PF[2]
                    for c in range(16):
                        dv(lambda e, c=c: e.matmul(psl.t[:, 0:36], h2T.t[:, c * 128:(c + 1) * 128], wr_sb.t[:, c, :], start=(c == 0), stop=(c == 15)),
                           [h2T.reg(c // 4), wr_sb], [psl], "pe")
                    dv(lambda e: e.tensor_copy(out=lgt.t[:], in_=psl.t[:, 0:36]), [psl], [lgt])
                    dv(lambda e: e.reduce_max(out=col(3), in_=lgt.t[:, 0:4], axis=AX.X), [lgt], [sm])
                    dv(lambda e: e.tensor_scalar(out=col(4), in0=col(3), scalar1=-1.0, scalar2=0.0, op0=ALU.mult, op1=ALU.add), [sm], [sm])
                    dv(lambda e: e.tensor_scalar(out=goh.t[:], in0=lgt.t[:, 0:4], scalar1=col(3), scalar2=1.0, op0=ALU.is_equal, op1=ALU.mult), [lgt, sm], [goh])
                    dv(lambda e: e.activation(out=junk4.t[:], in_=lgt.t[:, 0:4], func=AF.Exp, bias=col(4), accum_out=col(5)), [lgt, sm], [junk4, sm], "act")
                    dv(lambda e: e.reciprocal(out=col(6), in_=col(5)), [sm], [sm])
                    dv(lambda e: e.tensor_scalar(out=esel.t[:], in0=lgt.t[:, 4:12], scalar1=goh.t[:, 0:1], scalar2=0.0, op0=ALU.mult, op1=ALU.add), [lgt, goh], [esel])
                    for g_ in range(1, 4):
                        dv(lambda e, g_=g_: e.scalar_tensor_tensor(out=esel.t[:], in0=lgt.t[:, 4 + 8 * g_:12 + 8 * g_], scalar=goh.t[:, g_:g_ + 1], in1=esel.t[:],
                                                                   op0=ALU.mult, op1=ALU.add), [lgt, goh, esel], [esel])
                    dv(lambda e: e.reduce_max(out=col(7), in_=esel.t[:], axis=AX.X), [esel], [sm])
                    dv(lambda e: e.tensor_scalar(out=oh1.t[:], in0=esel.t[:], scalar1=col(7), scalar2=1.0, op0=ALU.is_equal, op1=ALU.mult), [esel, sm], [oh1])
                    dv(lambda e: e.scalar_tensor_tensor(out=esel2.t[:], in0=oh1.t[:], scalar=-1.0e30, in1=esel.t[:], op0=ALU.mult, op1=ALU.add), [oh1, esel], [esel2])
                    dv(lambda e: e.reduce_max(out=col(8), in_=esel2.t[:], axis=AX.X), [esel2], [sm])
                    dv(lambda e: e.tensor_scalar(out=oh2.t[:], in0=esel2.t[:], scalar1=col(8), scalar2=1.0, op0=ALU.is_equal, op1=ALU.mult), [esel2, sm], [oh2])
                    dv(lambda e: e.tensor_tensor(out=col(9), in0=col(8), in1=col(7), op=ALU.subtract), [sm], [sm])
                    dv(lambda e: e.activation(out=col(10), in_=col(9), func=AF.Exp), [sm], [sm], "act")
                    dv(lambda e: e.tensor_scalar(out=col(11), in0=col(10), scalar1=1.0, scalar2=0.0, op0=ALU.add, op1=ALU.add), [sm], [sm])
                    dv(lambda e: e.reciprocal(out=col(12), in_=col(11)), [sm], [sm])
                    dv(lambda e, t=t: e.tensor_tensor(out=wts.t[:, t:t + 1], in0=col(12), in1=col(6), op=ALU.mult), [sm], [wts])
                    dv(lambda e, t=t: e.tensor_tensor(out=wts.t[:, 32 + t:33 + t], in0=col(6), in1=wts.t[:, t:t + 1], op=ALU.subtract), [sm, wts], [wts])
                    for g_ in range(4):
                        dv(lambda e, g_=g_, t=t: e.tensor_scalar(out=M1all.t[:, t, 8 * g_:8 * g_ + 8], in0=oh1.t[:], scalar1=goh.t[:, g_:g_ + 1], scalar2=0.0,
                                                                 op0=ALU.mult, op1=ALU.add), [oh1, goh], [M1all.reg(t)])
                        dv(lambda e, g_=g_, t=t: e.tensor_scalar(out=M2all.t[:, t, 8 * g_:8 * g_ + 8], in0=oh2.t[:], scalar1=goh.t[:, g_:g_ + 1], scalar2=0.0,
                                                                 op0=ALU.mult, op1=ALU.add), [oh2, goh], [M2all.reg(t)])
                    dv(lambda e, t=t: e.tensor_tensor(out=Mt.t[:], in0=M1all.t[:, t, :], in1=M2all.t[:, t, :], op=ALU.add), [M1all.reg(t), M2all.reg(t)], [Mt])
                    psr = PF[3]
                    dv(lambda e: e.matmul(psr.t[:, 0:32], ustr.t[:], Mt.t[:], start=True, stop=False), [ustr, Mt], [psr], "pe")
                    dv(lambda e: e.matmul(psr.t[:, 0:32], onesF.t[:], Msum.t[:], start=False, stop=True), [onesF, Msum], [psr], "pe")
                    dv(lambda e, t=t: e.tensor_copy(out=rank_all.t[:, t, :], in_=psr.t[:, 0:32]), [psr], [rank_all.reg(t)])
                    dv(lambda e: e.tensor_tensor(out=Msum.t[:], in0=Msum.t[:], in1=Mt.t[:], op=ALU.add), [Msum, Mt], [Msum])

                psc = PF[4]
                dv(lambda e: e.matmul(psc.t[0:32, 0:1], Msum.t[:], onesF.t[:, 0:1], start=True, stop=True), [Msum, onesF], [psc], "pe")
                dv(lambda e: e.tensor_copy(out=sm.t[0:32, 20:21], in_=psc.t[0:32, 0:1]), [psc], [sm])
                dv(lambda e: e.tensor_scalar(out=cmp64.t[:], in0=thr_sb.t[0:32, :], scalar1=sm.t[0:32, 20:21], scalar2=1.0, op0=ALU.is_lt, op1=ALU.mult), [thr_sb, sm], [cmp64])
                dv(lambda e: e.reduce_sum(out=sm.t[0:32, 21:22], in_=cmp64.t[:], axis=AX.X), [cmp64], [sm])
                dv(lambda e: e.tensor_scalar(out=sm.t[0:32, 22:23], in0=sm.t[0:32, 21:22], scalar1=128.0, scalar2=0.0, op0=ALU.mult, op1=ALU.add), [sm], [sm])
                dv(lambda e: e.tensor_scalar(out=padrep.t[:], in0=onesF.t[0:32, :], scalar1=sm.t[0:32, 22:23], scalar2=0.0, op0=ALU.mult, op1=ALU.add), [onesF, sm], [padrep])
                pso, pso2 = PF[5], PF[4]
                dv(lambda e: e.matmul(pso.t[:, 0:32], padrep.t[:], ustr.t[0:32, 0:32], start=True, stop=True), [padrep, ustr], [pso], "pe")
                dv(lambda e: e.matmul(pso2.t[:, 0:32], padrep.t[:], ident_f.t[0:32, 0:32], start=True, stop=True), [padrep, ident_f], [pso2], "pe")
                dv(lambda e: e.tensor_copy(out=offs_sb.t[:], in_=pso.t[:, 0:32]), [pso], [offs_sb])
                dv(lambda e: e.tensor_tensor(out=pend_sb.t[:], in0=pso2.t[:, 0:32], in1=offs_sb.t[:], op=ALU.add), [pso2, offs_sb], [pend_sb])
                for t in range(32):
                    dv(lambda e, t=t: e.tensor_tensor(out=tmp32.t[:], in0=rank_all.t[:, t, :], in1=offs_sb.t[:], op=ALU.add), [rank_all.reg(t), offs_sb], [tmp32])
                    dv(lambda e, t=t: e.tensor_tensor(out=tmp32b.t[:], in0=tmp32.t[:], in1=M1all.t[:, t, :], op=ALU.mult), [tmp32, M1all.reg(t)], [tmp32b])
                    dv(lambda e, t=t: e.reduce_sum(out=posf.t[:, t:t + 1], in_=tmp32b.t[:], axis=AX.X), [tmp32b], [posf])
                    dv(lambda e, t=t: e.tensor_tensor(out=tmp32b.t[:], in0=tmp32.t[:], in1=M2all.t[:, t, :], op=ALU.mult), [tmp32, M2all.reg(t)], [tmp32b])
                    dv(lambda e, t=t: e.reduce_sum(out=posf.t[:, 32 + t:33 + t], in_=tmp32b.t[:], axis=AX.X), [tmp32b], [posf])
                dv(lambda e: e.tensor_copy(out=posi.t[:], in_=posf.t[:]), [posf], [posi])
                dv(lambda e: e.tensor_scalar(out=tmp32.t[:], in0=pend_sb.t[:], scalar1=cst_sb.t[:, 0:1], scalar2=1.0, op0=ALU.is_le, op1=ALU.mult), [pend_sb, cst_sb], [tmp32])
                dv(lambda e: e.reduce_sum(out=sm.t[:, 24:25], in_=tmp32.t[:], axis=AX.X), [tmp32], [sm])
                dv(lambda e: e.tensor_scalar(out=sm.t[:, 25:26], in0=sm.t[:, 24:25], scalar1=float(NEXP - 1), scalar2=0.0, op0=ALU.min, op1=ALU.add), [sm], [sm])
                beb = P.sb("beb", [128, 128], F32)
                widf = P.sb("widf", [128, 128], F32)
                dv(lambda e: e.tensor_scalar(out=beb.t[:], in0=onesF.t[:], scalar1=sm.t[:, 25:26], scalar2=0.0, op0=ALU.mult, op1=ALU.add), [onesF, sm], [beb])
                psb = PF[5]
                dv(lambda e: e.matmul(psb.t[:, 0:128], beb.t[:], ident_f.t[:], start=True, stop=True), [beb, ident_f], [psb], "pe")
                dv(lambda e: e.tensor_scalar(out=widf.t[:], in0=psb.t[:, 0:128], scalar1=128.0, scalar2=cst_sb.t[:, 1:2], op0=ALU.mult, op1=ALU.add),
                   [psb, cst_sb], [widf])
                berep = P.sb("berep", [128, 128], F32)
                eqs = P.sb("eqs", [128, 128], F32)
                dv(lambda e: e.tensor_copy(out=berep.t[:], in_=psb.t[:, 0:128]), [psb], [berep])
                dv(lambda e: e.tensor_tensor(out=eqs.t[:, 2:128], in0=berep.t[:, 2:128], in1=berep.t[:, 0:126], op=ALU.is_equal), [berep], [eqs])
                dv(lambda e: e.scalar_tensor_tensor(out=widf.t[:, 2:128], in0=eqs.t[:, 2:128], scalar=1.0e6, in1=widf.t[:, 2:128], op0=ALU.mult, op1=ALU.add),
                   [eqs, widf], [widf])
                dv(lambda e: e.tensor_copy(out=widx.t[:], in_=widf.t[:]), [widf], [widx])
                for t in range(32):
                    for k in range(2):
                        ci = k * 32 + t
                        sch.custom_dma("pool", ch_sc, lambda e, t=t, ci=ci: e.indirect_dma_start(
                            out=XBUF[:, :], out_offset=bass.IndirectOffsetOnAxis(ap=posi.t[:, ci:ci + 1], axis=0),
                            in_=H2.t[:, t, :], in_offset=None, bounds_check=bc_pos, oob_is_err=False), reads=[H2.reg(t), posi])
                sch.barrier()

        phaseE()
        if debug and "posi" in debug:
            d_posi = nc.dram_tensor("posi", [128, 64], I32, kind="ExternalOutput").ap()
            d_widx = nc.dram_tensor("widx", [128, 128], I32, kind="ExternalOutput").ap()
            d_wts = nc.dram_tensor("wts", [128, 64], F32, kind="ExternalOutput").ap()
            ch_d = sch.chan("dbg")
            sch.dma("sp", ch_d, d_posi[:, :], posi.t[:], reads=[posi])
            sch.dma("sp", ch_d, d_widx[:, :], widx.t[:], reads=[widx])
            sch.dma("sp", ch_d, d_wts[:, :], wts.t[:], reads=[wts])
            sch.barrier()
        if stop_after == "E":
            return nc

        def phaseF():
            pes = contextlib.ExitStack()
            with pes:
                P = Pool_(nc, pes)
                xbs = [P.sb("xb%d" % i, [128, D], BF16) for i in range(2)]
                xbT = [P.sb("xbT%d" % i, [128, D], BF16) for i in range(2)]
                w1s = [P.sb("w1_%d" % i, [128, 16, FF], BF16) for i in range(2)]
                w3s = [P.sb("w3_%d" % i, [128, 16, FF], BF16) for i in range(2)]
                w2s = [P.sb("w2_%d" % i, [128, 4, D], BF16) for i in range(2)]
                s1s = [P.sb("s1_%d" % i, [128, FF], F32) for i in range(2)]
                ubs = [P.sb("ub%d" % i, [128, FF], BF16) for i in range(2)]
                uTs = [P.sb("uT%d" % i, [128, FF], BF16) for i in range(2)]
                ybs = [P.sb("yb%d" % i, [128, D], F32) for i in range(2)]
                ch_xb = [sch.chan("f_xb%d" % i) for i in range(2)]
                ch_w = [sch.chan("f_w%d" % i, sw=True) for i in range(2)]
                ch_y = [sch.chan("f_y%d" % i) for i in range(2)]

                def dv(fn, reads, writes, eng="dve"):
                    sch.op(eng, fn, reads=reads, writes=writes)

                wg_rows = w_e_gate.rearrange("e (p c) n -> (e p) (c n)", c=16).rearrange("r (a m) -> (r a) m", m=2048)
                wu_rows = w_e_up.rearrange("e (p c) n -> (e p) (c n)", c=16).rearrange("r (a m) -> (r a) m", m=2048)
                wd_rows = w_e_down.rearrange("e (p c) n -> (e p) (c n)", c=4).rearrange("r (a m) -> (r a) m", m=2048)

                def load_block(b):
                    i = b % 2
                    sch.dma("sp", ch_xb[i], xbs[i].t[:], XBUF[b * 128:(b + 1) * 128, :], writes=[xbs[i]])
                    w1, w3, w2 = w1s[i], w3s[i], w2s[i]
                    for (dst, srcv) in ((w1, WG16), (w3, WU16), (w2, WD16)):
                        sch.custom_dma("pool", ch_w[i], lambda e, dst=dst, srcv=srcv, b=b: e.indirect_dma_start(
                            out=dst.t[:].rearrange("p c n -> p (c n)"), out_offset=None, in_=srcv[:, :],
                            in_offset=bass.IndirectOffsetOnAxis(ap=widx.t[:, b:b + 1], axis=0),
                            bounds_check=bc_w, oob_is_err=False), reads=[widx], writes=[dst])
                    for bb in (w1, w3, w2):
                        sch.fence(ch_w[i], bb)

                def transpose_x(b):
                    xb, xT_ = xbs[b % 2], xbT[b % 2]
                    for half in range(2):
                        pb = PB[half]
                        for j in range(8):
                            c = half * 8 + j
                            dv(lambda e, pb=pb, j=j, c=c: e.transpose(out=pb.t[:, j * 128:(j + 1) * 128], in_=xb.t[:].rearrange("q (p c) -> q c p", c=16)[:, c, :], identity=ident_bf.t[:]),
                               [xb, ident_bf], [pb], "pe")
                        if half == 0:
                            dv(lambda e, pb=pb: e.activation(out=xT_.t[:, 0:1024], in_=pb.t[:], func=AF.Copy), [pb], [xT_.reg(0)], "act")
                        else:
                            dv(lambda e, pb=pb: e.tensor_copy(out=xT_.t[:, 1024:2048], in_=pb.t[:]), [pb], [xT_.reg(1)])

                load_block(0)
                transpose_x(0)
                for b in range(NBLK):
                    i = b % 2
                    if b + 1 < NBLK:
                        load_block(b + 1)
                    xb, xT_, w1, w3, w2 = xbs[i], xbT[i], w1s[i], w3s[i], w2s[i]
                    ps1, ps3 = PF[0], PF[1]
                    for c in range(16):
                        dv(lambda e, c=c: e.matmul(ps1.t[:], xT_.t[:, c * 128:(c + 1) * 128], w1.t[:, c, :], start=(c == 0), stop=(c == 15)),
                           [xT_.reg(c // 8), w1], [ps1], "pe")
                    for c in range(16):
                        dv(lambda e, c=c: e.matmul(ps3.t[:], xT_.t[:, c * 128:(c + 1) * 128], w3.t[:, c, :], start=(c == 0), stop=(c == 15)),
                           [xT_.reg(c // 8), w3], [ps3], "pe")
                    s1, ub, uT, yb = s1s[i], ubs[i], uTs[i], ybs[i]
                    dv(lambda e: e.activation(out=s1.t[:], in_=ps1.t[:], func=AF.Silu), [ps1], [s1], "act")
                    dv(lambda e: e.tensor_tensor(out=ub.t[:], in0=ps3.t[:], in1=s1.t[:], op=ALU.mult), [ps3, s1], [ub])
                    if b + 1 < NBLK:
                        transpose_x(b + 1)
                    pbu = PB[0]
                    for k in range(4):
                        dv(lambda e, k=k: e.transpose(out=pbu.t[:, k * 128:(k + 1) * 128], in_=ub.t[:].rearrange("q (p c) -> q c p", c=4)[:, k, :], identity=ident_bf.t[:]),
                           [ub, ident_bf], [pbu], "pe")
                    dv(lambda e: e.tensor_copy(out=uT.t[:], in_=pbu.t[:, 0:512]), [pbu], [uT])
                    for n in range(4):
                        py = PF[2 + n]
                        for k in range(4):
                            dv(lambda e, k=k, n=n, py=py: e.matmul(py.t[:], uT.t[:, k * 128:(k + 1) * 128], w2.t[:, k, n * 512:(n + 1) * 512], start=(k == 0), stop=(k == 3)),
                               [uT, w2], [py], "pe")
                        if n % 2 == 0:
                            dv(lambda e, n=n, py=py: e.activation(out=yb.t[:, n * 512:(n + 1) * 512], in_=py.t[:], func=AF.Copy), [py], [yb.reg(n)], "act")
                        else:
                            dv(lambda e, n=n, py=py: e.tensor_copy(out=yb.t[:, n * 512:(n + 1) * 512], in_=py.t[:]), [py], [yb.reg(n)])
                    sch.dma("sp", ch_y[i], OBUF[b * 128:(b + 1) * 128, :], yb.t[:], reads=[yb])
                sch.barrier()

        phaseF()
        if stop_after == "F":
            return nc

        def phaseG():
            pes = contextlib.ExitStack()
            with pes:
                P = Pool_(nc, pes)
                gfin_b = P.sb("gfin_b", [128, D], F32)
                x1s = [P.sb("gx1_%d" % i, [128, D], F32) for i in range(2)]
                o1s = [P.sb("go1_%d" % i, [128, D], F32) for i in range(2)]
                o2s = [P.sb("go2_%d" % i, [128, D], F32) for i in range(2)]
                sm = P.sb("gsm", [128, 8], F32)
                ch_g = sch.chan("g_c")
                ch_x = [sch.chan("g_x%d" % i) for i in range(2)]
                ch_o1 = [sch.chan("g_o1%d" % i, sw=True) for i in range(2)]
                ch_o2 = [sch.chan("g_o2%d" % i, sw=True) for i in range(2)]
                ch_y = [sch.chan("g_y%d" % i) for i in range(2)]
                sch.dma("sp", ch_g, gfin_b.t[:], g_final.rearrange("(o n) -> o n", o=1).partition_broadcast(128), writes=[gfin_b])

                def dv(fn, reads, writes, eng="dve"):
                    sch.op(eng, fn, reads=reads, writes=writes)

                def col(i):
                    return sm.t[:, i:i + 1]

                for t in range(32):
                    i = t % 2
                    rows = slice(t * 128, (t + 1) * 128)
                    x1b, o1, o2 = x1s[i], o1s[i], o2s[i]
                    sch.dma("sp", ch_x[i], x1b.t[:], X1[rows, :], writes=[x1b])
                    sch.custom_dma("pool", ch_o1[i], lambda e, o1=o1, t=t: e.indirect_dma_start(
                        out=o1.t[:, :], out_offset=None, in_=OBUF[:, :],
                        in_offset=bass.IndirectOffsetOnAxis(ap=posi.t[:, t:t + 1], axis=0), bounds_check=bc_pos, oob_is_err=False),
                        reads=[posi], writes=[o1])
                    sch.custom_dma("pool", ch_o2[i], lambda e, o2=o2, t=t: e.indirect_dma_start(
                        out=o2.t[:, :], out_offset=None, in_=OBUF[:, :],
                        in_offset=bass.IndirectOffsetOnAxis(ap=posi.t[:, 32 + t:33 + t], axis=0), bounds_check=bc_pos, oob_is_err=False),
                        reads=[posi], writes=[o2])
                    dv(lambda e, t=t, o1=o1, x1b=x1b: e.scalar_tensor_tensor(out=x1b.t[:], in0=o1.t[:], scalar=wts.t[:, t:t + 1], in1=x1b.t[:], op0=ALU.mult, op1=ALU.add),
                       [o1, wts, x1b], [x1b])
                    dv(lambda e, t=t, o2=o2, x1b=x1b: e.scalar_tensor_tensor(out=x1b.t[:], in0=o2.t[:], scalar=wts.t[:, 32 + t:33 + t], in1=x1b.t[:], op0=ALU.mult, op1=ALU.add),
                       [o2, wts, x1b], [x1b])
                    dv(lambda e: e.memset(sm.t[:, 0:4], 0.0), [], [sm])
                    dv(lambda e, o1=o1, x1b=x1b: e.activation(out=o1.t[:], in_=x1b.t[:], func=AF.Square, accum_out=col(0)), [x1b, sm], [o1, sm], "act")
                    dv(lambda e: e.activation(out=col(1), in_=col(0), func=AF.Ln, scale=1.0 / D, bias=eps_sb.t[:, 0:1]), [sm, eps_sb], [sm], "act")
                    dv(lambda e: e.activation(out=col(2), in_=col(1), func=AF.Exp, scale=-0.5), [sm], [sm], "act")
                    dv(lambda e, o2=o2, x1b=x1b: e.scalar_tensor_tensor(out=o2.t[:], in0=x1b.t[:], scalar=col(2), in1=gfin_b.t[:], op0=ALU.mult, op1=ALU.mult),
                       [x1b, sm, gfin_b], [o2])
                    sch.dma("sp", ch_y[i], y[rows, :], o2.t[:], reads=[o2])
                sch.barrier()

        phaseG()

    return nc


def host_inputs(inputs, cores=None):
    x = np.asarray(inputs["x"], dtype=np.float32)
    f32 = np.float32
    inv = (10000.0 ** (-np.arange(0, 64, 2, dtype=np.float32) / np.float32(64))).astype(f32)
    rmat = np.zeros((128, 128), f32)
    for blk in range(2):
        for i in range(32):
            rmat[blk * 64 + i + 32, blk * 64 + i] = -1.0
            rmat[blk * 64 + i, blk * 64 + i + 32] = 1.0
    ident = np.eye(128, dtype=f32)
    consts = np.zeros((128, 64), f32)
    consts[:, 0] = 128.0 * np.arange(128)
    consts[:, 1] = np.arange(128)
    thr = np.ascontiguousarray(np.broadcast_to(128.0 * np.arange(64, dtype=f32)[None, :], (128, 64)), f32)
    shared = {
        "g_mix": np.ascontiguousarray(inputs["g_mix"][0], f32),
        "w_in": np.ascontiguousarray(inputs["w_in"][0], f32),
        "q_norm_a": np.ascontiguousarray(inputs["q_norm_a"][0], f32),
        "k_norm_a": np.ascontiguousarray(inputs["k_norm_a"][0], f32),
        "lam4": np.ascontiguousarray(np.stack([inputs["lam_q1"][0], inputs["lam_k1"][0],
                                               inputs["lam_q2"][0], inputs["lam_k2"][0]]), f32),
        "subln_b": np.ascontiguousarray(inputs["subln_b"][0], f32),
        "w_branch_a": np.ascontiguousarray(inputs["w_branch_a"][0], f32),
        "w_branch_b": np.ascontiguousarray(inputs["w_branch_b"][0], f32),
        "w_gate": np.ascontiguousarray(inputs["w_gate"][0], f32),
        "b_gate": np.ascontiguousarray(inputs["b_gate"][0], f32),
        "w_out": np.ascontiguousarray(inputs["w_out"][0], f32),
        "g_ffn": np.ascontiguousarray(inputs["g_ffn"][0], f32),
        "w_router": np.ascontiguousarray(np.concatenate([inputs["w_router_group"][0],
                                                         inputs["w_router_expert"][0]], axis=1), f32),
        "w_e_gate": np.ascontiguousarray(inputs["w_e_gate"][0], f32),
        "w_e_up": np.ascontiguousarray(inputs["w_e_up"][0], f32),
        "w_e_down": np.ascontiguousarray(inputs["w_e_down"][0], f32),
        "g_final": np.ascontiguousarray(inputs["g_final"], f32),
        "rmat": rmat, "ident": ident, "consts": consts,
        "ustrict": np.triu(np.ones((128, 128), f32), 1), "thr": thr,
    }
    in_maps = []
    for core in (range(NCORES) if cores is None else cores):
        b, hf = core // 2, core % 2
        own = np.arange(hf * OWN, (hf + 1) * OWN)
        oth = np.arange((1 - hf) * OWN, (2 - hf) * OWN)
        perm = np.concatenate([own, oth])
        xb = x[b]
        m = dict(shared)
        m["xT"] = np.ascontiguousarray(xb[perm].T)
        m["x_own"] = np.ascontiguousarray(xb[own])
        pos = perm.astype(f32)
        rows = (perm // 64).astype(f32)
        cols = (perm % 64).astype(f32)
        angR = rows[None, :] * inv[:, None]
        angC = cols[None, :] * inv[:, None]
        angP = pos[None, :] * inv[:, None]
        m["cosA"] = np.ascontiguousarray(np.concatenate([np.cos(angR)] * 2 + [np.cos(angC)] * 2, 0), f32)
        m["sinA"] = np.ascontiguousarray(np.concatenate([np.sin(angR)] * 2 + [np.sin(angC)] * 2, 0), f32)
        m["cosB"] = np.ascontiguousarray(np.concatenate([np.cos(angP)] * 4, 0), f32)
        m["sinB"] = np.ascontiguousarray(np.concatenate([np.sin(angP)] * 4, 0), f32)
        in_maps.append(m)
    return in_maps


def kernel(**inputs):
    in_maps = host_inputs(inputs)
    nc = build()
    res = run_bass_kernel_spmd(nc, in_maps, core_ids=list(range(NCORES)))
    out = np.zeros((4, S, D), np.float32)
    for core in range(NCORES):
        b, hf = core // 2, core % 2
        out[b, hf * OWN:(hf + 1) * OWN] = res.results[core]["y"]
    return out
```

```python
import contextlib
import numpy as np
import concourse.bass as bass
import concourse.mybir as mybir
from concourse.bass_utils import run_bass_kernel_spmd

F32 = mybir.dt.float32
BF16 = mybir.dt.bfloat16
I32 = mybir.dt.int32
AF = mybir.ActivationFunctionType
ALU = mybir.AluOpType
AX = mybir.AxisListType

D = 2048
S = 8192
OWN = 4096
NCORES = 8
EPS = 1e-6
NEXP = 32
FF = 512
NBLK = 96
NPOS = NBLK * 128
IN_COLS = 4608


class Buf:
    def __init__(self, name, t=None, parent=None):
        self.name, self.t, self.parent = name, t, parent
        self.w, self.r, self.kids = {}, {}, {}

    def reg(self, key):
        if key not in self.kids:
            self.kids[key] = Buf("%s.%s" % (self.name, key), self.t, self)
        return self.kids[key]


def _merge(d, s):
    for k, v in s.items():
        if d.get(k, 0) < v:
            d[k] = v


class Sched:
    def __init__(self, nc, es):
        self.nc, self.es = nc, es
        self.engs = {"pe": nc.tensor, "act": nc.scalar, "dve": nc.vector, "pool": nc.gpsimd, "sp": nc.sync}
        self.sems, self.cnt, self.cur = {}, {}, {}
        self.known = {e: {} for e in self.engs}
        self.nsem = 0
        self.phase = 0
        for e in self.engs:
            self._newkey(e)

    def _newkey(self, e):
        key = "%s#%d" % (e, self.phase)
        self._newsem(key)
        self.cur[e] = key

    def _newsem(self, key):
        h = self.es.enter_context(self.nc.semaphore("s%d" % self.nsem))
        self.nsem += 1
        self.sems[key] = h
        self.cnt[key] = 0

    def chan(self, name, persistent=False, sw=False):
        self.nchan = getattr(self, "nchan", 0) + 1
        key = "dma_%s_%d" % (name, self.nchan)
        if not hasattr(self, "free_chans"):
            self.free_chans, self.live_chans = {True: [], False: []}, []
        free = self.free_chans[sw]
        if free and not persistent:
            h, base = free.pop()
            self.sems[key] = h
            self.cnt[key] = base
        else:
            self._newsem(key)
        if not persistent:
            self.live_chans.append((key, sw))
        return key

    def _deps(self, reads, writes):
        d = {}
        for b in reads:
            _merge(d, b.w)
            if b.parent is not None:
                _merge(d, b.parent.w)
            for k in b.kids.values():
                _merge(d, k.w)
        for b in writes:
            xs = [b] + ([b.parent] if b.parent is not None else []) + list(b.kids.values())
            for x in xs:
                _merge(d, x.w)
                _merge(d, x.r)
        return d

    def _wait(self, e, deps):
        kn = self.known[e]
        for k, v in deps.items():
            if e == "pe" and k == self.cur["pe"]:
                continue
            if k == self.cur[e] and v <= self.cnt[k] - 3:
                continue
            if kn.get(k, 0) < v:
                self.engs[e].wait_ge(self.sems[k], v)
                kn[k] = v

    def _commit(self, tok, reads, writes):
        k, v = tok
        for b in reads:
            if b.r.get(k, 0) < v:
                b.r[k] = v
        for b in writes:
            b.w = {k: v}
            b.r = {}
            for kid in b.kids.values():
                kid.w, kid.r = {}, {}

    def op(self, e, fn, reads=(), writes=()):
        self._wait(e, self._deps(reads, writes))
        ins = fn(self.engs[e])
        key = self.cur[e]
        self.cnt[key] += 1
        assert self.cnt[key] < 60000, key
        ins.then_inc(self.sems[key], 1)
        self._commit((key, self.cnt[key]), reads, writes)

    def dma(self, q, ch, out, in_, reads=(), writes=(), **kw):
        self._wait(q, self._deps(reads, writes))
        ins = self.engs[q].dma_start(out=out, in_=in_, **kw)
        self.cnt[ch] += 16
        assert self.cnt[ch] < 60000, ch
        ins.then_inc(self.sems[ch], 16)
        self._commit((ch, self.cnt[ch]), reads, writes)

    def custom_dma(self, q, ch, fn, reads=(), writes=()):
        self._wait(q, self._deps(reads, writes))
        ins = fn(self.engs[q])
        self.cnt[ch] += 16
        ins.then_inc(self.sems[ch], 16)
        self._commit((ch, self.cnt[ch]), reads, writes)

    def fence(self, ch, buf, readers=False):
        v = self.cnt[ch]
        for b in [buf] + list(buf.kids.values()):
            d = b.r if readers else b.w
            if ch in d or b is buf:
                d[ch] = v

    def wait_all(self, e, keys=None):
        tot = {k: v for k, v in self.cnt.items() if v > 0 and (keys is None or k in keys)}
        self._wait(e, tot)

    def barrier(self):
        tot = {k: v for k, v in self.cnt.items() if v > 0}
        for e in self.engs:
            kn = self.known[e]
            for k, v in tot.items():
                if k == self.cur[e]:
                    continue
                if kn.get(k, 0) < v:
                    self.engs[e].wait_ge(self.sems[k], v)
                    kn[k] = v
        for e in self.engs:
            for k, v in tot.items():
                self.known[e][k] = v
        self.phase += 1
        for e in self.engs:
            self._newkey(e)
        for (key, sw) in getattr(self, "live_chans", []):
            self.free_chans[sw].append((self.sems[key], self.cnt[key]))
        self.live_chans = []


class Pool_:
    N = [0]

    def __init__(self, nc, es):
        self.nc, self.es = nc, es

    def sb(self, name, shape, dt):
        Pool_.N[0] += 1
        t = self.es.enter_context(self.nc.sbuf_tensor("%s_%d" % (name, Pool_.N[0]), list(shape), dt))
        return Buf(name, t)


LCOL_FLAG = True


def build(stop_after=None, debug=False):
    nc = bass.Bass("TRN2", target_bir_lowering=False)

    def din(name, shape, dt=F32):
        return nc.dram_tensor(name, list(shape), dt, kind="ExternalInput").ap()

    def dscr(name, shape, dt):
        kind = "ExternalOutput" if (debug and name in debug) else "Internal"
        return nc.dram_tensor(name, list(shape), dt, kind=kind).ap()

    xT = din("xT", [D, S])
    x_own = din("x_own", [OWN, D])
    g_mix = din("g_mix", [D])
    w_in = din("w_in", [D, IN_COLS])
    q_norm_a = din("q_norm_a", [128])
    k_norm_a = din("k_norm_a", [128])
    lam4 = din("lam4", [4, 64])
    subln_b = din("subln_b", [128])
    w_branch_a = din("w_branch_a", [1024, D])
    w_branch_b = din("w_branch_b", [1024, D])
    w_gate = din("w_gate", [D, 2 * D])
    b_gate = din("b_gate", [2 * D])
    w_out = din("w_out", [D, D])
    g_ffn = din("g_ffn", [D])
    w_router = din("w_router", [D, 36])
    w_e_gate = din("w_e_gate", [NEXP, D, FF])
    w_e_up = din("w_e_up", [NEXP, D, FF])
    w_e_down = din("w_e_down", [NEXP, FF, D])
    g_final = din("g_final", [D])
    cosA = din("cosA", [128, S])
    sinA = din("sinA", [128, S])
    cosB = din("cosB", [128, S])
    sinB = din("sinB", [128, S])
    rmat = din("rmat", [128, 128])
    ident = din("ident", [128, 128])
    consts = din("consts", [128, 64])
    ustrict = din("ustrict", [128, 128])
    thr = din("thr", [128, 64])

    y = nc.dram_tensor("y", [OWN, D], F32, kind="ExternalOutput").ap()

    KAT = dscr("KAT", [2, 128, S], BF16)
    VA = dscr("VA", [S, 256], BF16)
    QAT = dscr("QAT", [8, 128, OWN], BF16)
    KBT = dscr("KBT", [8, 128, S], BF16)
    QBT = dscr("QBT", [8, 128, OWN], BF16)
    VB = dscr("VB", [S, 1024], BF16)
    HT = dscr("HT", [D, OWN], BF16)
    OT = dscr("OT", [D, OWN], BF16)
    MT = dscr("MT", [D, OWN], BF16)
    X1 = dscr("X1", [OWN, D], F32)
    XBUF = dscr("XBUF", [NPOS, D], BF16)
    OBUF = dscr("OBUF", [NPOS, D], F32)
    WD1 = dscr("WD1", [8, 128, 12288], BF16)
    WO16 = dscr("WO16", [4, 128, 8192], BF16)
    WG16 = dscr("WG16", [NEXP * 128, 8192], BF16)
    WU16 = dscr("WU16", [NEXP * 128, 8192], BF16)
    WD16 = dscr("WD16", [NEXP * 128, 8192], BF16)

    es = contextlib.ExitStack()
    with es:
        sch = Sched(nc, es)
        P0 = Pool_(nc, es)

        ps_es = contextlib.ExitStack()
        es.enter_context(ps_es)
        PS = []
        for i in range(8):
            t = ps_es.enter_context(nc.psum_tensor("ps%d" % i, [128, 512], F32))
            PS.append(Buf("ps%d" % i, t))

        ones_bf = P0.sb("ones_bf", [128, 128], BF16)
        rmat_bf = P0.sb("rmat_bf", [128, 128], BF16)
        ident_bf = P0.sb("ident_bf", [128, 128], BF16)
        ident_f = P0.sb("ident_f", [128, 128], F32)
        gmix_sb = P0.sb("gmix_sb", [128, 16], F32)
        qn_sb = P0.sb("qn_sb", [128, 1], F32)
        kn_sb = P0.sb("kn_sb", [128, 1], F32)
        ch_c = sch.chan("const", persistent=True)
        ch_cp = sch.chan("constp", persistent=True, sw=True)
        sch.op("dve", lambda e: e.memset(ones_bf.t[:], 1.0), writes=[ones_bf])
        eps_sb = P0.sb("eps_sb", [128, 1], F32)
        sch.op("dve", lambda e: e.memset(eps_sb.t[:], EPS), writes=[eps_sb])
        sch.dma("pool", ch_cp, rmat_bf.t[:], rmat[:, :], writes=[rmat_bf])
        sch.dma("pool", ch_cp, ident_bf.t[:], ident[:, :], writes=[ident_bf])
        sch.dma("sp", ch_c, ident_f.t[:], ident[:, :], writes=[ident_f])
        sch.dma("sp", ch_c, gmix_sb.t[:], g_mix.rearrange("(c p) -> p c", p=128), writes=[gmix_sb],
                allow_slow_non_contiguous=True)
        sch.dma("sp", ch_c, qn_sb.t[:], q_norm_a.rearrange("(p o) -> p o", o=1), writes=[qn_sb])
        sch.dma("sp", ch_c, kn_sb.t[:], k_norm_a.rearrange("(p o) -> p o", o=1), writes=[kn_sb])


        zero_bf = P0.sb("zero_bf", [128, D], BF16)
        ch_z = sch.chan("zfill", persistent=True)
        sch.op("pool", lambda e: e.memset(zero_bf.t[:], 0.0), writes=[zero_bf])
        for b_ in range(NBLK):
            sch.dma("sp", ch_z, XBUF[b_ * 128:(b_ + 1) * 128, :], zero_bf.t[:], reads=[zero_bf])

        xT_v = xT.rearrange("(c p) t -> p c t", p=128)
        w_in_v = w_in.rearrange("(c p) n -> p c n", p=128)

        def phaseA(passno):
            pes = contextlib.ExitStack()
            with pes:
                P = Pool_(nc, pes)
                if passno == 1:
                    segs = [(1024, 512), (2560, 2048)]
                    ntiles = 16
                else:
                    segs = [(0, 1024), (1536, 1024)]
                    ntiles = 8
                ncols = sum(n for _, n in segs)
                wsb = P.sb("wsb", [128, 16, ncols], BF16)
                ch_w = sch.chan("wA", sw=True)
                off = 0
                segoff = {}
                for (c0, n) in segs:
                    segoff[c0] = off
                    for c in range(16):
                        for p0 in range(0, n, 512):
                            sch.dma("pool", ch_w, wsb.t[:, c, off + p0:off + p0 + 512], w_in_v[:, c, c0 + p0:c0 + p0 + 512],
                                    writes=[wsb.reg((c0, c, p0))])
                    off += n
                sch.fence(ch_w, wsb)
                xt = P.sb("xt", [128, 16, 512], F32)
                hts = [P.sb("ht%d" % i, [128, 16, 512], BF16) for i in range(2)]
                sqs = [P.sb("sq%d" % i, [128, 512], BF16) for i in range(8)]
                rstd = P.sb("rstd", [128, 512], F32)
                cs = [[P.sb("cs%d_%d" % (i, j), [128, 512], F32) for j in range(4)] for i in range(2)]
                xgs = [P.sb("xg%d" % i, [128, 512], BF16) for i in range(2)]
                sq2s = [P.sb("sq2_%d" % i, [128, 512], BF16) for i in range(2)]
                rs2 = [P.sb("rs2_%d" % i, [128, 512], F32) for i in range(2)]
                t1s = [P.sb("t1_%d" % i, [128, 512], F32) for i in range(2)]
                t2s = [P.sb("t2_%d" % i, [128, 512], F32) for i in range(2)]
                outs = [P.sb("out%d" % i, [128, 512], BF16) for i in range(3)]
                vts = [P.sb("vt%d" % i, [128, 512], BF16) for i in range(3)]
                ch_x = sch.chan("xA")
                ch_cs = [sch.chan("csA%d" % i) for i in range(2)]
                ch_out = [sch.chan("outA%d" % i) for i in range(3)]
                ch_vt = [sch.chan("vtA%d" % i) for i in range(3)]
                ch_ht = [sch.chan("htA%d" % i) for i in range(2)]
                ps_main = [PS[0], PS[1], PS[2]]
                ps_ss, ps_rot, ps_ss2 = PS[3], PS[4], PS[5]
                cnt = {"main": 0, "ep": 0, "out": 0, "vt": 0}

                def prep_load(t):
                    tk = slice(t * 512, (t + 1) * 512)
                    csb = cs[t % 2]
                    for c4 in range(4):
                        sch.dma("sp", ch_x, xt.t[:, 4 * c4:4 * c4 + 4, :], xT_v[:, 4 * c4:4 * c4 + 4, tk],
                                writes=[xt.reg(c4)])
                    sch.fence(ch_x, xt)
                    for j, tab in enumerate((cosA, sinA, cosB, sinB)):
                        sch.dma("sp", ch_cs[t % 2], csb[j].t[:], tab[:, tk], writes=[csb[j]])
                    for j in range(4):
                        csb[j].w = {ch_cs[t % 2]: sch.cnt[ch_cs[t % 2]]}

                def prep_sq(t, q):
                    for c in range(4 * q, 4 * q + 4):
                        sq = sqs[c % 8]
                        sch.op("act", lambda e, c=c, sq=sq: e.activation(out=sq.t[:], in_=xt.t[:, c, :], func=AF.Square),
                               reads=[xt.reg(c // 4)], writes=[sq])

                def prep_mm(t, q):
                    for c in range(4 * q, 4 * q + 4):
                        sq = sqs[c % 8]
                        sch.op("pe", lambda e, c=c, sq=sq: e.matmul(ps_ss.t[:], ones_bf.t[:], sq.t[:], start=(c == 0), stop=(c == 15)),
                               reads=[ones_bf, sq], writes=[ps_ss])

                def prep_fin(t):
                    tk = slice(t * 512, (t + 1) * 512)
                    ht = hts[t % 2]
                    sch.op("act", lambda e: e.activation(out=rstd.t[:], in_=ps_ss.t[:], func=AF.Ln, scale=1.0 / D, bias=eps_sb.t[:, 0:1]),
                           reads=[ps_ss, eps_sb], writes=[rstd])
                    sch.op("act", lambda e: e.activation(out=rstd.t[:], in_=rstd.t[:], func=AF.Exp, scale=-0.5),
                           reads=[rstd], writes=[rstd])
                    for c in range(16):
                        sch.op("dve", lambda e, c=c: e.scalar_tensor_tensor(out=ht.t[:, c, :], in0=xt.t[:, c, :],
                                                                            scalar=gmix_sb.t[:, c:c + 1], in1=rstd.t[:],
                                                                            op0=ALU.mult, op1=ALU.mult),
                               reads=[xt.reg(c // 4), rstd, gmix_sb], writes=[ht.reg(c)])
                    if passno == 2:
                        for c4 in range(4):
                            sch.dma("sp", ch_ht[t % 2], HT.rearrange("(c p) t -> p c t", p=128)[:, 4 * c4:4 * c4 + 4, tk],
                                    ht.t[:, 4 * c4:4 * c4 + 4, :], reads=[ht.reg(4 * c4 + i) for i in range(4)])
                        sch.fence(ch_ht[t % 2], ht, readers=True)

                def prep_step(t, n):
                    if n == 1:
                        prep_load(t)
                    if 3 <= n <= 6:
                        prep_sq(t, n - 3)
                    if 4 <= n <= 7:
                        prep_mm(t, n - 4)
                    if n == 7:
                        prep_fin(t)

                def tile_main(t):
                    tk = slice(t * 512, (t + 1) * 512)
                    ht = hts[t % 2]
                    csb = cs[t % 2]
                    if passno == 1:
                        fm = [("A", 1024 + 128 * i, kn_sb, KAT[i, :, tk]) for i in range(2)] + \
                             [("B", 2560 + 128 * i, None, KBT[i, :, tk]) for i in range(8)]
                    else:
                        fm = [("A", 128 * i, qn_sb, QAT[i, :, tk]) for i in range(8)] + \
                             [("B", 1536 + 128 * i, None, QBT[i, :, tk]) for i in range(8)]
                    nfm = [0]
                    pend = [None]
                    for (kind, c0, gsb, dst) in fm:
                        seg0 = max(s0 for (s0, n) in segs if s0 <= c0)
                        wo = segoff[seg0] + (c0 - seg0)
                        ps = ps_main[cnt["main"] % 3]
                        cnt["main"] += 1
                        for c in range(16):
                            sch.op("pe", lambda e, c=c, ps=ps, wo=wo: e.matmul(ps.t[:], wsb.t[:, c, wo:wo + 128], ht.t[:, c, :],
                                                                               start=(c == 0), stop=(c == 15)),
                                   reads=[wsb, ht.reg(c)], writes=[ps])
                        def _ep(kind=kind, gsb=gsb, dst=dst, ps=ps):
                            i2 = cnt["ep"] % 2
                            cnt["ep"] += 1
                            xg, sq2, r2, t1, t2 = xgs[i2], sq2s[i2], rs2[i2], t1s[i2], t2s[i2]
                            ob = outs[cnt["out"] % 3]
                            cho = ch_out[cnt["out"] % 3]
                            cnt["out"] += 1
                            if kind == "A":
                                cosb, sinb = csb[0], csb[1]
                                sch.op("act", lambda e, ps=ps, sq2=sq2: e.activation(out=sq2.t[:], in_=ps.t[:], func=AF.Square),
                                       reads=[ps], writes=[sq2])
                                sch.op("act", lambda e, ps=ps, xg=xg, gsb=gsb: e.activation(out=xg.t[:], in_=ps.t[:], func=AF.Copy,
                                                                                           scale=gsb.t[:, 0:1]),
                                       reads=[ps, gsb], writes=[xg])
                                sch.op("pe", lambda e, sq2=sq2: e.matmul(ps_ss2.t[:], ones_bf.t[:], sq2.t[:], start=True, stop=True),
                                       reads=[ones_bf, sq2], writes=[ps_ss2])
                            else:
                                cosb, sinb = csb[2], csb[3]
                                sch.op("act", lambda e, ps=ps, xg=xg: e.activation(out=xg.t[:], in_=ps.t[:], func=AF.Copy),
                                       reads=[ps], writes=[xg])
                            sch.op("pe", lambda e, xg=xg: e.matmul(ps_rot.t[:], rmat_bf.t[:], xg.t[:], start=True, stop=True),
                                   reads=[rmat_bf, xg], writes=[ps_rot])
                            sch.op("pool", lambda e, xg=xg, t1=t1, cosb=cosb: e.tensor_tensor(out=t1.t[:], in0=xg.t[:], in1=cosb.t[:], op=ALU.mult),
                                   reads=[xg, cosb], writes=[t1])
                            sch.op("dve", lambda e, t2=t2, sinb=sinb: e.tensor_tensor(out=t2.t[:], in0=ps_rot.t[:], in1=sinb.t[:], op=ALU.mult),
                                   reads=[ps_rot, sinb], writes=[t2])
                            if kind == "A":
                                sch.op("act", lambda e, r2=r2: e.activation(out=r2.t[:], in_=ps_ss2.t[:], func=AF.Ln, scale=1.0 / 128, bias=eps_sb.t[:, 0:1]),
                                       reads=[ps_ss2, eps_sb], writes=[r2])
                                sch.op("act", lambda e, r2=r2: e.activation(out=r2.t[:], in_=r2.t[:], func=AF.Exp, scale=-0.5),
                                       reads=[r2], writes=[r2])
                                sch.op("pool", lambda e, t1=t1, t2=t2: e.tensor_tensor(out=t1.t[:], in0=t1.t[:], in1=t2.t[:], op=ALU.add),
                                       reads=[t1, t2], writes=[t1])
                                sch.op("pool", lambda e, t1=t1, r2=r2, ob=ob: e.tensor_tensor(out=ob.t[:], in0=t1.t[:], in1=r2.t[:], op=ALU.mult),
                                       reads=[t1, r2], writes=[ob])
                            else:
                                sch.op("pool", lambda e, t1=t1, t2=t2, ob=ob: e.tensor_tensor(out=ob.t[:], in0=t1.t[:], in1=t2.t[:], op=ALU.add),
                                       reads=[t1, t2], writes=[ob])
                            sch.dma("sp", cho, dst, ob.t[:], reads=[ob])
                            nfm[0] += 1
                            if t + 1 < ntiles:
                                prep_step(t + 1, nfm[0])
                        if pend[0] is not None:
                            pend[0]()
                        pend[0] = _ep

                    pend[0]()
                    pend[0] = None
                    if passno == 1:
                        for j in range(4):
                            rows = slice(t * 512 + j * 128, t * 512 + (j + 1) * 128)
                            for (c0, n, dstt, d0) in [(1280, 256, VA, 0), (3584, 512, VB, 0), (4096, 512, VB, 512)]:
                                seg0 = max(s0 for (s0, nn) in segs if s0 <= c0)
                                wo = segoff[seg0] + (c0 - seg0)
                                ps = ps_main[cnt["main"] % 3]
                                cnt["main"] += 1
                                for c in range(16):
                                    sch.op("pe", lambda e, c=c, ps=ps, wo=wo, n=n, j=j: e.matmul(ps.t[:, 0:n], ht.t[:, c, j * 128:(j + 1) * 128],
                                                                                                 wsb.t[:, c, wo:wo + n], start=(c == 0), stop=(c == 15)),
                                           reads=[wsb, ht.reg(c)], writes=[ps])
                                vt = vts[cnt["vt"] % 3]
                                chv = ch_vt[cnt["vt"] % 3]
                                cnt["vt"] += 1
                                sch.op("act", lambda e, ps=ps, vt=vt, n=n: e.activation(out=vt.t[:, 0:n], in_=ps.t[:, 0:n], func=AF.Copy),
                                       reads=[ps], writes=[vt])
                                sch.dma("sp", chv, dstt[rows, d0:d0 + n], vt.t[:, 0:n], reads=[vt])
                for n_ in range(1, 8):
                    prep_step(0, n_)
                for t in range(ntiles):
                    tile_main(t)
                sch.barrier()

        phaseA(1)
        if stop_after == "A1":
            return nc
        phaseA(2)
        if stop_after == "A2":
            return nc

        lam_init = 0.8 - 0.6 * float(np.exp(-0.3 * 0))
        neglam = P0.sb("neglam", [128, 1], F32)
        ones_f = P0.sb("ones_f", [1, 128], F32)
        lam_sb = P0.sb("lam_sb", [1, 256], F32)
        lam_t = P0.sb("lam_t", [1, 8], F32)
        sch.op("dve", lambda e: e.memset(ones_f.t[:], 1.0), writes=[ones_f])
        sch.dma("sp", ch_c, lam_sb.t[:], lam4.rearrange("(o a) d -> o (a d)", o=1), writes=[lam_sb])
        sch.op("dve", lambda e: e.tensor_tensor(out=lam_sb.t[0:1, 0:64], in0=lam_sb.t[0:1, 0:64], in1=lam_sb.t[0:1, 64:128], op=ALU.mult),
               reads=[lam_sb], writes=[lam_sb])
        sch.op("dve", lambda e: e.tensor_tensor(out=lam_sb.t[0:1, 128:192], in0=lam_sb.t[0:1, 128:192], in1=lam_sb.t[0:1, 192:256], op=ALU.mult),
               reads=[lam_sb], writes=[lam_sb])
        sch.op("dve", lambda e: e.reduce_sum(out=lam_t.t[0:1, 0:1], in_=lam_sb.t[0:1, 0:64], axis=AX.X), reads=[lam_sb], writes=[lam_t])
        sch.op("dve", lambda e: e.reduce_sum(out=lam_t.t[0:1, 1:2], in_=lam_sb.t[0:1, 128:192], axis=AX.X), reads=[lam_sb], writes=[lam_t])
        sch.op("act", lambda e: e.activation(out=lam_t.t[0:1, 2:4], in_=lam_t.t[0:1, 0:2], func=AF.Exp), reads=[lam_t], writes=[lam_t])
        sch.op("dve", lambda e: e.tensor_tensor(out=lam_t.t[0:1, 4:5], in0=lam_t.t[0:1, 3:4], in1=lam_t.t[0:1, 2:3], op=ALU.subtract),
               reads=[lam_t], writes=[lam_t])
        sch.op("dve", lambda e: e.tensor_scalar(out=lam_t.t[0:1, 5:6], in0=lam_t.t[0:1, 4:5], scalar1=-lam_init, scalar2=None, op0=ALU.add),
               reads=[lam_t], writes=[lam_t])
        sch.op("pe", lambda e: e.matmul(PS[7].t[:, 0:1], ones_f.t[0:1, :], lam_t.t[0:1, 5:6], start=True, stop=True),
               reads=[ones_f, lam_t], writes=[PS[7]])
        sch.op("dve", lambda e: e.tensor_copy(out=neglam.t[:], in_=PS[7].t[:, 0:1]), reads=[PS[7]], writes=[neglam])

        OT_v = OT.rearrange("(c p) t -> c p t", p=128)
        SCALE_A = 128.0 ** -0.5
        SCALE_B = 64.0 ** -0.5

        def attention(kind):
            pes = contextlib.ExitStack()
            with pes:
                P = Pool_(nc, pes)
                psS = []
                for i in range(2):
                    t = pes.enter_context(nc.psum_tensor("psS%s%d" % (kind, i), [128, 1024], F32))
                    psS.append(Buf("psS%d" % i, t))
                psO = []
                for i in range(2):
                    t = pes.enter_context(nc.psum_tensor("psO%s%d" % (kind, i), [128, 512], F32))
                    psO.append(Buf("psO%d" % i, t))
                LCOL = LCOL_FLAG
                t = pes.enter_context(nc.psum_tensor("psL%s" % kind, [128, 512], F32))
                psL = Buf("psL", t)
                t = pes.enter_context(nc.psum_tensor("psRr%s" % kind, [128, 512], F32))
                psR = Buf("psR", t)
                psL2 = [psL, psR]
                sel64 = P.sb("sel64", [64, 128], F32)
                sch.op("pool", lambda e: e.memset(sel64.t[:], 1.0 / 32.0), writes=[sel64])
                lsbs = [P.sb("lsb%d" % i, [64, 512], F32) for i in range(2)]
                KTs = [P.sb("KT%d" % i, [128, S], BF16) for i in range(2)]
                Vs = [P.sb("V%d" % i, [128, 64, 128], BF16) for i in range(2)]
                QTs = [P.sb("QT%d" % i, [128, OWN], BF16) for i in range(2)]
                pTs = [P.sb("pT%d" % i, [128, 1024], BF16) for i in range(3)]
                rls = [P.sb("rl%d" % i, [128, 512], F32) for i in range(2)]
                d0s = [P.sb("d0%d" % i, [128, 512], F32) for i in range(2)]
                obs = [P.sb("ob%d" % i, [128, 512], BF16) for i in range(2)]
                ch_k = [sch.chan("k%d" % i) for i in range(2)]
                ch_v = [sch.chan("v%d" % i) for i in range(2)]
                ch_q = [sch.chan("q%d" % i) for i in range(2)]
                ch_o = [sch.chan("o%d" % i) for i in range(2)]
                NST = 32 if kind == "A" else 64
                LAG = 2

                def load_kv(g):
                    kb, vb = KTs[g % 2], Vs[g % 2]
                    if kind == "A":
                        src_k = KAT[g, :, :]
                        src_v = VA.rearrange("(kc p) c -> p kc c", p=128)[:, :, g * 128:(g + 1) * 128]
                    else:
                        src_k = KBT[g, :, :]
                        src_v = VB.rearrange("(kc p) c -> p kc c", p=128)[:, :, g * 128:(g + 1) * 128]
                    sch.dma("sp", ch_k[g % 2], kb.t[:], src_k, writes=[kb])
                    sch.dma("sp", ch_v[g % 2], vb.t[:], src_v, writes=[vb])

                def load_q(h):
                    src = QAT[h, :, :] if kind == "A" else QBT[h, :, :]
                    sch.dma("sp", ch_q[h % 2], QTs[h % 2].t[:], src, writes=[QTs[h % 2]])

                def kvidx(h):
                    return h // 4 if kind == "A" else h

                steps = [(h, qt, st) for h in range(8) for qt in range(8) for st in range(NST)]
                N = len(steps)
                load_kv(0)
                load_q(0)
                if kind == "B":
                    emit_conversions()
                epi_n = [0]

                def emit_S(i):
                    h, qt, st = steps[i]
                    g = kvidx(h)
                    kb, qb = KTs[g % 2], QTs[h % 2]
                    ps = psS[i % 2]
                    pT = pTs[i % 3]
                    for m in range(2):
                        if kind == "A":
                            kc = 2 * st + m
                            lhsT = kb.t[:, kc * 128:(kc + 1) * 128]
                            rhs = qb.t[:, qt * 512:(qt + 1) * 512]
                        else:
                            kc = st
                            lhsT = kb.t[m * 64:(m + 1) * 64, kc * 128:(kc + 1) * 128]
                            rhs = qb.t[m * 64:(m + 1) * 64, qt * 512:(qt + 1) * 512]
                        sch.op("pe", lambda e, ps=ps, lhsT=lhsT, rhs=rhs, m=m: e.matmul(ps.t[:, m * 512:(m + 1) * 512], lhsT, rhs, start=True, stop=True),
                               reads=[kb, qb], writes=[ps])
                    sc = SCALE_A if kind == "A" else SCALE_B
                    sch.op("act", lambda e, ps=ps, pT=pT, sc=sc: e.activation(out=pT.t[:], in_=ps.t[:], func=AF.Exp, scale=sc),
                           reads=[ps], writes=[pT])

                def emit_PV(i):
                    h, qt, st = steps[i]
                    g = kvidx(h)
                    if qt == 0 and st == 0 and h + 1 < 8:
                        if kvidx(h + 1) != g:
                            load_kv(kvidx(h + 1))
                        load_q(h + 1)
                    vb = Vs[g % 2]
                    pT = pTs[i % 3]
                    for m in range(2):
                        if kind == "A":
                            kc = 2 * st + m
                            po = psO[qt % 2]
                            first, last = (st == 0 and m == 0), (st == NST - 1 and m == 1)
                        else:
                            kc = st
                            po = psO[m]
                            first, last = (st == 0), (st == NST - 1)
                        sch.op("pe", lambda e, po=po, pT=pT, kc=kc, m=m, first=first, last=last: e.matmul(po.t[:], vb.t[:, kc, :], pT.t[:, m * 512:(m + 1) * 512],
                                                                                                          start=first, stop=last),
                               reads=[vb, pT], writes=[po])
                    for m in range(2):
                        if LCOL:
                            sch.op("pe", lambda e, pT=pT, m=m: e.matmul(psL.t[32 * m:32 * m + 32, :], ones_bf.t[:, 0:32], pT.t[:, m * 512:(m + 1) * 512],
                                                                        start=(st == 0), stop=(st == NST - 1), tile_position=(0, 32 * m)),
                                   reads=[ones_bf, pT], writes=[psL])
                        else:
                            sch.op("pe", lambda e, pT=pT, m=m: e.matmul(psL2[m].t[:], ones_bf.t[:], pT.t[:, m * 512:(m + 1) * 512],
                                                                        start=(st == 0), stop=(st == NST - 1)),
                                   reads=[ones_bf, pT], writes=[psL2[m]])
                    if st == NST - 1:
                        k2 = epi_n[0] % 2
                        epi_n[0] += 1
                        ob, rl, d0, lsb = obs[k2], rls[k2], d0s[k2], lsbs[k2]
                        if LCOL:
                            sch.op("dve", lambda e, lsb=lsb: e.tensor_copy(out=lsb.t[0:64, :], in_=psL.t[0:64, :]), reads=[psL], writes=[lsb])
                        if kind == "A":
                            po = psO[qt % 2]
                            if LCOL:
                                sch.op("pe", lambda e, lsb=lsb: e.matmul(psR.t[:], sel64.t[0:64, :], lsb.t[0:64, :], start=True, stop=True),
                                       reads=[sel64, lsb], writes=[psR])
                                sch.op("dve", lambda e, rl=rl: e.reciprocal(out=rl.t[:], in_=psR.t[:]), reads=[psR], writes=[rl])
                            else:
                                sch.op("dve", lambda e, d0=d0: e.tensor_copy(out=d0.t[:], in_=psL2[0].t[:]), reads=[psL2[0]], writes=[d0])
                                sch.op("dve", lambda e, d0=d0, rl=rl: e.tensor_tensor(out=rl.t[:], in0=psL2[1].t[:], in1=d0.t[:], op=ALU.add), reads=[psL2[1], d0], writes=[rl])
                                sch.op("dve", lambda e, rl=rl: e.reciprocal(out=rl.t[:], in_=rl.t[:]), reads=[rl], writes=[rl])
                            sch.op("dve", lambda e, ob=ob, po=po, rl=rl: e.tensor_tensor(out=ob.t[:], in0=po.t[:], in1=rl.t[:], op=ALU.mult),
                                   reads=[po, rl], writes=[ob])
                            row = h
                        else:
                            for m in range(2):
                                tgt = d0 if m == 0 else rl
                                if LCOL:
                                    sch.op("pe", lambda e, lsb=lsb, m=m: e.matmul(psR.t[:], sel64.t[32 * m:32 * m + 32, :], lsb.t[32 * m:32 * m + 32, :], start=True, stop=True),
                                           reads=[sel64, lsb], writes=[psR])
                                    sch.op("dve", lambda e, rl=rl: e.reciprocal(out=rl.t[:], in_=psR.t[:]), reads=[psR], writes=[rl])
                                else:
                                    sch.op("dve", lambda e, rl=rl, m=m: e.reciprocal(out=rl.t[:], in_=psL2[m].t[:]), reads=[psL2[m]], writes=[rl])
                                sch.op("dve", lambda e, tgt=tgt, rl=rl, m=m: e.tensor_tensor(out=tgt.t[:], in0=psO[m].t[:], in1=rl.t[:], op=ALU.mult),
                                       reads=[psO[m], rl], writes=[tgt])
                            sch.op("dve", lambda e, ob=ob, rl=rl, d0=d0: e.scalar_tensor_tensor(out=ob.t[:], in0=rl.t[:], scalar=neglam.t[:, 0:1],
                                                                                                  in1=d0.t[:], op0=ALU.mult, op1=ALU.add),
                                   reads=[rl, d0, neglam], writes=[ob])
                            row = 8 + h
                        sch.dma("sp", ch_o[k2], OT_v[row, :, qt * 512:(qt + 1) * 512], ob.t[:], reads=[ob])

                for i in range(N + LAG):
                    if i < N:
                        emit_S(i)
                    if i >= LAG:
                        emit_PV(i - LAG)
                sch.barrier()


        def emit_conversions():
            ch = sch.chan("conv", sw=True)
            wa_v = w_branch_a.rearrange("(c p) n -> p c n", p=128)
            wb_v = w_branch_b.rearrange("(c p) n -> p c n", p=128)
            wg_v = w_gate.rearrange("(c p) n -> p c n", p=128)
            wo_v = w_out.rearrange("(c p) n -> p c n", p=128)
            for grp in range(8):
                cs_ = slice(grp * 256, (grp + 1) * 256)
                cs2 = slice(D + grp * 256, D + (grp + 1) * 256)
                for (off, nk, src) in ((0, 8, wa_v[:, :, cs_]), (2048, 8, wb_v[:, :, cs_]), (4096, 16, wg_v[:, :, cs_]), (8192, 16, wg_v[:, :, cs2])):
                    sch.dma("pool", ch, WD1[grp, :, off:off + nk * 256].rearrange("p (k n) -> p k n", n=256), src)
            for n in range(4):
                for c4 in range(4):
                    sch.dma("pool", ch, WO16[n, :, c4 * 2048:(c4 + 1) * 2048].rearrange("p (k n) -> p k n", n=512),
                            wo_v[:, 4 * c4:4 * c4 + 4, n * 512:(n + 1) * 512])
            for e_ in range(NEXP):
                rows = slice(e_ * 128, (e_ + 1) * 128)
                for (dstw, srcw, cc) in ((WG16, w_e_gate, 16), (WU16, w_e_up, 16), (WD16, w_e_down, 4)):
                    srcv = srcw[e_].rearrange("(p c) n -> p (c n)", c=cc)
                    for a_ in range(4):
                        sch.dma("pool", ch, dstw[rows, a_ * 2048:(a_ + 1) * 2048], srcv[:, a_ * 2048:(a_ + 1) * 2048])

        sch.barrier()
        ps_es.close()
        attention("A")
        if stop_after == "BA":
            return nc
        attention("B")
        if stop_after == "BB":
            return nc
        ps_es = contextlib.ExitStack()
        es.enter_context(ps_es)
        del PS[:]
        for i in range(8):
            t = ps_es.enter_context(nc.psum_tensor("psd%d" % i, [128, 512], F32))
            PS.append(Buf("psd%d" % i, t))

        sg_sb = P0.sb("sg_sb", [128, 1], F32)
        bg_sb = P0.sb("bg_sb", [128, 32], F32)
        sch.dma("sp", ch_c, sg_sb.t[:], subln_b.rearrange("(p o) -> p o", o=1), writes=[sg_sb])
        sch.dma("sp", ch_c, bg_sb.t[:], b_gate.rearrange("(c p) -> p c", p=128), writes=[bg_sb], allow_slow_non_contiguous=True)
        sch.op("dve", lambda e: e.tensor_scalar(out=sg_sb.t[:], in0=sg_sb.t[:], scalar1=(1.0 - lam_init), scalar2=0.0,
                                                op0=ALU.mult, op1=ALU.add), reads=[sg_sb], writes=[sg_sb])

        def phaseD1():
            pes = contextlib.ExitStack()
            with pes:
                P = Pool_(nc, pes)
                TT = 1024
                ht = P.sb("d1ht", [128, 16, TT], BF16)
                ot = P.sb("d1ot", [128, 16, TT], BF16)
                mt = P.sb("d1mt", [128, 16, TT], BF16)
                wgrp = []
                for i in range(2):
                    wgrp.append(dict(wa=P.sb("wa%d" % i, [128, 8, 256], BF16), wb=P.sb("wb%d" % i, [128, 8, 256], BF16),
                                     wga=P.sb("wga%d" % i, [128, 16, 256], BF16), wgb=P.sb("wgb%d" % i, [128, 16, 256], BF16)))
                ch_wg = [sch.chan("d1w%d" % i) for i in range(2)]
                ch_in = sch.chan("d1in")
                ch_mt = sch.chan("d1mt")
                sqs = [P.sb("d1sq%d" % i, [128, 512], BF16) for i in range(2)]
                rs = [P.sb("d1rs%d" % i, [128, 512], F32) for i in range(2)]
                sga = [P.sb("d1sga%d" % i, [128, 512], F32) for i in range(2)]
                sgb = [P.sb("d1sgb%d" % i, [128, 512], F32) for i in range(2)]
                m1s = [P.sb("d1m1%d" % i, [128, 512], F32) for i in range(2)]
                m2s = [P.sb("d1m2%d" % i, [128, 512], F32) for i in range(2)]
                HT_v = HT.rearrange("(c p) t -> p c t", p=128)
                OTv = OT.rearrange("(c p) t -> p c t", p=128)
                MT_v = MT.rearrange("(c p) t -> p c t", p=128)
                wa_v = w_branch_a.rearrange("(c p) n -> p c n", p=128)
                wb_v = w_branch_b.rearrange("(c p) n -> p c n", p=128)
                wg_v = w_gate.rearrange("(c p) n -> p c n", p=128)
                gcount = 0
                ep = 0
                for tt in range(OWN // TT):
                    tk = slice(tt * TT, (tt + 1) * TT)
                    for c4 in range(4):
                        sch.dma("sp", ch_in, ht.t[:, 4 * c4:4 * c4 + 4, :], HT_v[:, 4 * c4:4 * c4 + 4, tk], writes=[ht.reg(c4)])
                        sch.dma("sp", ch_in, ot.t[:, 4 * c4:4 * c4 + 4, :], OTv[:, 4 * c4:4 * c4 + 4, tk], writes=[ot.reg(c4)])
                    sch.fence(ch_in, ht)
                    sch.fence(ch_in, ot)
                    for c in range(8, 16):
                        for hh in range(TT // 512):
                            i2 = ep % 2
                            ep += 1
                            sl = slice(hh * 512, (hh + 1) * 512)
                            sq, r_ = sqs[i2], rs[i2]
                            pss = PS[i2]
                            sch.op("act", lambda e, sq=sq, c=c, sl=sl: e.activation(out=sq.t[:], in_=ot.t[:, c, sl], func=AF.Square),
                                   reads=[ot.reg(c // 4)], writes=[sq])
                            sch.op("pe", lambda e, pss=pss, sq=sq: e.matmul(pss.t[:], ones_bf.t[:], sq.t[:], start=True, stop=True),
                                   reads=[ones_bf, sq], writes=[pss])
                            sch.op("act", lambda e, r_=r_, pss=pss: e.activation(out=r_.t[:], in_=pss.t[:], func=AF.Ln, scale=1.0 / 128, bias=eps_sb.t[:, 0:1]),
                                   reads=[pss, eps_sb], writes=[r_])
                            sch.op("act", lambda e, r_=r_: e.activation(out=r_.t[:], in_=r_.t[:], func=AF.Exp, scale=-0.5), reads=[r_], writes=[r_])
                            sch.op("dve", lambda e, r_=r_, c=c, sl=sl: e.scalar_tensor_tensor(out=ot.t[:, c, sl], in0=ot.t[:, c, sl], scalar=sg_sb.t[:, 0:1],
                                                                                                in1=r_.t[:], op0=ALU.mult, op1=ALU.mult),
                                   reads=[ot.reg(c // 4), sg_sb, r_], writes=[ot.reg(c // 4)])
                    for grp in range(8):
                        wg = wgrp[gcount % 2]
                        chw = ch_wg[gcount % 2]
                        gcount += 1
                        cs_ = slice(grp * 256, (grp + 1) * 256)
                        cs2 = slice(D + grp * 256, D + (grp + 1) * 256)
                        sch.dma("sp", chw, wg["wa"].t[:], WD1[grp, :, 0:2048].rearrange("p (k n) -> p k n", n=256), writes=[wg["wa"]])
                        sch.dma("sp", chw, wg["wb"].t[:], WD1[grp, :, 2048:4096].rearrange("p (k n) -> p k n", n=256), writes=[wg["wb"]])
                        sch.dma("sp", chw, wg["wga"].t[:], WD1[grp, :, 4096:8192].rearrange("p (k n) -> p k n", n=256), writes=[wg["wga"]])
                        sch.dma("sp", chw, wg["wgb"].t[:], WD1[grp, :, 8192:12288].rearrange("p (k n) -> p k n", n=256), writes=[wg["wgb"]])
                        for nm in ("wa", "wb", "wga", "wgb"):
                            wg[nm].w = {chw: sch.cnt[chw]}
                        for jj in range(2):
                            j = grp * 2 + jj
                            wsl = slice(jj * 128, (jj + 1) * 128)
                            for hh in range(TT // 512):
                                sl = slice(hh * 512, (hh + 1) * 512)
                                i2 = ep % 2
                                ep += 1
                                pa, pb, ga, gb = PS[4 * i2], PS[1 + 4 * i2], PS[2 + 4 * i2], PS[3 + 4 * i2]
                                for k in range(8):
                                    sch.op("pe", lambda e, k=k, pa=pa, wsl=wsl, sl=sl: e.matmul(pa.t[:], wg["wa"].t[:, k, wsl], ot.t[:, k, sl], start=(k == 0), stop=(k == 7)),
                                           reads=[wg["wa"], ot.reg(k // 4)], writes=[pa])
                                for k in range(8):
                                    sch.op("pe", lambda e, k=k, pb=pb, wsl=wsl, sl=sl: e.matmul(pb.t[:], wg["wb"].t[:, k, wsl], ot.t[:, 8 + k, sl], start=(k == 0), stop=(k == 7)),
                                           reads=[wg["wb"], ot.reg(2 + k // 4)], writes=[pb])
                                for k in range(16):
                                    sch.op("pe", lambda e, k=k, ga=ga, wsl=wsl, sl=sl: e.matmul(ga.t[:], wg["wga"].t[:, k, wsl], ht.t[:, k, sl], start=(k == 0), stop=(k == 15)),
                                           reads=[wg["wga"], ht.reg(k // 4)], writes=[ga])
                                for k in range(16):
                                    sch.op("pe", lambda e, k=k, gb=gb, wsl=wsl, sl=sl: e.matmul(gb.t[:], wg["wgb"].t[:, k, wsl], ht.t[:, k, sl], start=(k == 0), stop=(k == 15)),
                                           reads=[wg["wgb"], ht.reg(k // 4)], writes=[gb])
                                sa, sb_, m1, m2 = sga[i2], sgb[i2], m1s[i2], m2s[i2]
                                sch.op("act", lambda e, sa=sa, ga=ga, j=j: e.activation(out=sa.t[:], in_=ga.t[:], func=AF.Sigmoid, bias=bg_sb.t[:, j:j + 1]),
                                       reads=[ga, bg_sb], writes=[sa])
                                sch.op("act", lambda e, sb_=sb_, gb=gb, j=j: e.activation(out=sb_.t[:], in_=gb.t[:], func=AF.Sigmoid, bias=bg_sb.t[:, 16 + j:17 + j]),
                                       reads=[gb, bg_sb], writes=[sb_])
                                sch.op("dve", lambda e, m1=m1, sa=sa, pa=pa: e.tensor_tensor(out=m1.t[:], in0=pa.t[:], in1=sa.t[:], op=ALU.mult),
                                       reads=[pa, sa], writes=[m1])
                                sch.op("dve", lambda e, m2=m2, sb_=sb_, pb=pb: e.tensor_tensor(out=m2.t[:], in0=pb.t[:], in1=sb_.t[:], op=ALU.mult),
                                       reads=[pb, sb_], writes=[m2])
                                sch.op("pool", lambda e, m1=m1, m2=m2, j=j, sl=sl: e.tensor_tensor(out=mt.t[:, j, sl], in0=m1.t[:], in1=m2.t[:], op=ALU.add),
                                       reads=[m1, m2], writes=[mt.reg(j // 4)])
                    for c4 in range(4):
                        sch.dma("sp", ch_mt, MT_v[:, 4 * c4:4 * c4 + 4, tk], mt.t[:, 4 * c4:4 * c4 + 4, :], reads=[mt.reg(c4)])
                    sch.fence(ch_mt, mt, readers=True)
                sch.barrier()

        phaseD1()
        if stop_after == "D1":
            return nc

        def phaseD2():
            pes = contextlib.ExitStack()
            with pes:
                P = Pool_(nc, pes)
                mt = P.sb("d2mt", [128, 16, OWN], BF16)
                wos = [P.sb("d2wo%d" % i, [128, 16, 512], BF16) for i in range(2)]
                xr = [P.sb("d2xr%d" % i, [128, 512], F32) for i in range(3)]
                ch_m = sch.chan("d2m")
                ch_w = [sch.chan("d2w%d" % i) for i in range(2)]
                ch_x = [sch.chan("d2x%d" % i) for i in range(3)]
                ch_s = [sch.chan("d2s%d" % i) for i in range(3)]
                MT_v = MT.rearrange("(c p) t -> p c t", p=128)
                wo_v = w_out.rearrange("(c p) n -> p c n", p=128)
                for c in range(16):
                    sch.dma("sp", ch_m, mt.t[:, c, :], MT_v[:, c, :], writes=[mt.reg(c)])
                sch.fence(ch_m, mt)
                it = 0
                for n in range(4):
                    wo = wos[n % 2]
                    ns = slice(n * 512, (n + 1) * 512)
                    sch.dma("sp", ch_w[n % 2], wo.t[:], WO16[n].rearrange("p (k n) -> p k n", n=512), writes=[wo])
                    for ts in range(32):
                        rows = slice(ts * 128, (ts + 1) * 128)
                        ps = PS[it % 4]
                        x_ = xr[it % 3]
                        chx, chs = ch_x[it % 3], ch_s[it % 3]
                        it += 1
                        sch.dma("sp", chx, x_.t[:], x_own[rows, ns], writes=[x_])
                        for k in range(16):
                            sch.op("pe", lambda e, k=k, ps=ps, rows=rows, wo=wo: e.matmul(ps.t[:], mt.t[:, k, rows], wo.t[:, k, :], start=(k == 0), stop=(k == 15)),
                                   reads=[mt, wo], writes=[ps])
                        sch.op("dve", lambda e, x_=x_, ps=ps: e.tensor_tensor(out=x_.t[:], in0=ps.t[:], in1=x_.t[:], op=ALU.add),
                               reads=[ps, x_], writes=[x_])
                        sch.dma("sp", chs, X1[rows, ns], x_.t[:], reads=[x_])
                sch.barrier()

        phaseD2()
        if stop_after == "D2":
            return nc

        ps_es.close()
        ps_es2 = contextlib.ExitStack()
        es.enter_context(ps_es2)
        PF = []
        for i in range(6):
            t = ps_es2.enter_context(nc.psum_tensor("pf%d" % i, [128, 512], F32))
            PF.append(Buf("pf%d" % i, t))
        PB = []
        for i in range(2):
            t = ps_es2.enter_context(nc.psum_tensor("pb%d" % i, [128, 1024], BF16))
            PB.append(Buf("pb%d" % i, t))

        bc_pos = nc.gpsimd.to_reg(NPOS - 1)
        bc_w = nc.gpsimd.to_reg(NEXP * 128 - 1)
        posi = P0.sb("posi", [128, 64], I32)
        wts = P0.sb("wts", [128, 64], F32)
        widx = P0.sb("widx", [128, 128], I32)
        XB_ROWS = XBUF

        def phaseE():
            pes = contextlib.ExitStack()
            with pes:
                P = Pool_(nc, pes)
                H2 = P.sb("H2", [128, 32, D], BF16)
                gffn_b = P.sb("gffn_b", [128, D], F32)
                wr_sb = P.sb("wr_sb", [128, 16, 36], F32)
                ustr = P.sb("ustr", [128, 128], F32)
                onesF = P.sb("onesF", [128, 128], F32)
                thr_sb = P.sb("thr_sb", [128, 64], F32)
                cst_sb = P.sb("cst_sb", [128, 64], F32)
                x1b = P.sb("x1b", [128, D], F32)
                h2f = P.sb("h2f", [128, D], F32)
                h2T = P.sb("h2T", [128, D], F32)
                M1all = P.sb("M1all", [128, 32, 32], F32)
                M2all = P.sb("M2all", [128, 32, 32], F32)
                rank_all = P.sb("rank_all", [128, 32, 32], F32)
                Msum = P.sb("Msum", [128, 32], F32)
                Mt = P.sb("Mt", [128, 32], F32)
                lgt = P.sb("lgt", [128, 36], F32)
                sm = P.sb("sm", [128, 64], F32)
                esel = P.sb("esel", [128, 8], F32)
                esel2 = P.sb("esel2", [128, 8], F32)
                oh1 = P.sb("oh1", [128, 8], F32)
                oh2 = P.sb("oh2", [128, 8], F32)
                goh = P.sb("goh", [128, 4], F32)
                junk4 = P.sb("junk4", [128, 4], F32)
                posf = P.sb("posf", [128, 64], F32)
                tmp32 = P.sb("tmp32", [128, 32], F32)
                tmp32b = P.sb("tmp32b", [128, 32], F32)
                offs_sb = P.sb("offs_sb", [128, 32], F32)
                pend_sb = P.sb("pend_sb", [128, 32], F32)
                padrep = P.sb("padrep", [32, 128], F32)
                cmp64 = P.sb("cmp64", [32, 64], F32)
                ch_e = sch.chan("e_c")
                ch_x1 = sch.chan("e_x1")
                ch_sc = sch.chan("e_sc", sw=True)
                sch.dma("sp", ch_e, gffn_b.t[:], g_ffn.rearrange("(o n) -> o n", o=1).partition_broadcast(128), writes=[gffn_b])
                sch.dma("sp", ch_e, wr_sb.t[:], w_router.rearrange("(c p) n -> p c n", p=128), writes=[wr_sb])
                sch.dma("sp", ch_e, ustr.t[:], ustrict[:, :], writes=[ustr])
                sch.dma("sp", ch_e, thr_sb.t[:], thr[:, :], writes=[thr_sb])
                sch.dma("sp", ch_e, cst_sb.t[:], consts[:, :], writes=[cst_sb])
                for b_ in (gffn_b, wr_sb, ustr, thr_sb, cst_sb):
                    b_.w = {ch_e: sch.cnt[ch_e]}
                sch.op("dve", lambda e: e.memset(onesF.t[:], 1.0), writes=[onesF])
                sch.op("dve", lambda e: e.memset(Msum.t[:], 0.0), writes=[Msum])

                def dv(fn, reads, writes, eng="dve"):
                    sch.op(eng, fn, reads=reads, writes=writes)

                def col(i):
                    return sm.t[:, i:i + 1]

                for t in range(32):
                    rows = slice(t * 128, (t + 1) * 128)
                    sch.dma("sp", ch_x1, x1b.t[:], X1[rows, :], writes=[x1b])
                    dv(lambda e: e.memset(sm.t[:, 0:16], 0.0), [], [sm])
                    dv(lambda e: e.activation(out=h2f.t[:], in_=x1b.t[:], func=AF.Square, accum_out=col(0)), [x1b, sm], [h2f, sm], "act")
                    dv(lambda e: e.activation(out=col(1), in_=col(0), func=AF.Ln, scale=1.0 / D, bias=eps_sb.t[:, 0:1]), [sm, eps_sb], [sm], "act")
                    dv(lambda e: e.activation(out=col(2), in_=col(1), func=AF.Exp, scale=-0.5), [sm], [sm], "act")
                    dv(lambda e: e.scalar_tensor_tensor(out=h2f.t[:], in0=x1b.t[:], scalar=col(2), in1=gffn_b.t[:], op0=ALU.mult, op1=ALU.mult),
                       [x1b, sm, gffn_b], [h2f])
                    dv(lambda e, t=t: e.tensor_copy(out=H2.t[:, t, :], in_=h2f.t[:]), [h2f], [H2.reg(t)], "pool")
                    for g4 in range(4):
                        pst = PF[g4 % 2]
                        for j in range(4):
                            c = g4 * 4 + j
                            dv(lambda e, pst=pst, j=j, c=c: e.transpose(out=pst.t[:, j * 128:(j + 1) * 128], in_=h2f.t[:, c * 128:(c + 1) * 128], identity=ident_f.t[:]),
                               [h2f, ident_f], [pst], "pe")
                        dv(lambda e, pst=pst, g4=g4: e.activation(out=h2T.t[:, g4 * 512:(g4 + 1) * 512], in_=pst.t[:], func=AF.Copy), [pst], [h2T.reg(g4)], "act")
                    psl = PF[2]
                    for c in range(16):
                        dv(lambda e, c=c: e.matmul(psl.t[:, 0:36], h2T.t[:, c * 128:(c + 1) * 128], wr_sb.t[:, c, :], start=(c == 0), stop=(c == 15)),
                           [h2T.reg(c // 4), wr_sb], [psl], "pe")
                    dv(lambda e: e.tensor_copy(out=lgt.t[:], in_=psl.t[:, 0:36]), [psl], [lgt])
                    dv(lambda e: e.reduce_max(out=col(3), in_=lgt.t[:, 0:4], axis=AX.X), [lgt], [sm])
                    dv(lambda e: e.tensor_scalar(out=col(4), in0=col(3), scalar1=-1.0, scalar2=0.0, op0=ALU.mult, op1=ALU.add), [sm], [sm])
                    dv(lambda e: e.tensor_scalar(out=goh.t[:], in0=lgt.t[:, 0:4], scalar1=col(3), scalar2=1.0, op0=ALU.is_equal, op1=ALU.mult), [lgt, sm], [goh])
                    dv(lambda e: e.activation(out=junk4.t[:], in_=lgt.t[:, 0:4], func=AF.Exp, bias=col(4), accum_out=col(5)), [lgt, sm], [junk4, sm], "act")
                    dv(lambda e: e.reciprocal(out=col(6), in_=col(5)), [sm], [sm])
                    dv(lambda e: e.tensor_scalar(out=esel.t[:], in0=lgt.t[:, 4:12], scalar1=goh.t[:, 0:1], scalar2=0.0, op0=ALU.mult, op1=ALU.add), [lgt, goh], [esel])
                    for g_ in range(1, 4):
                        dv(lambda e, g_=g_: e.scalar_tensor_tensor(out=esel.t[:], in0=lgt.t[:, 4 + 8 * g_:12 + 8 * g_], scalar=goh.t[:, g_:g_ + 1], in1=esel.t[:],
                                                                   op0=ALU.mult, op1=ALU.add), [lgt, goh, esel], [esel])
                    dv(lambda e: e.reduce_max(out=col(7), in_=esel.t[:], axis=AX.X), [esel], [sm])
                    dv(lambda e: e.tensor_scalar(out=oh1.t[:], in0=esel.t[:], scalar1=col(7), scalar2=1.0, op0=ALU.is_equal, op1=ALU.mult), [esel, sm], [oh1])
                    dv(lambda e: e.scalar_tensor_tensor(out=esel2.t[:], in0=oh1.t[:], scalar=-1.0e30, in1=esel.t[:], op0=ALU.mult, op1=ALU.add), [oh1, esel], [esel2])
                    dv(lambda e: e.reduce_max(out=col(8), in_=esel2.t[:], axis=AX.X), [esel2], [sm])
                    dv(lambda e: e.tensor_scalar(out=oh2.t[:], in0=esel2.t[:], scalar1=col(8), scalar2=1.0, op0=ALU.is_equal, op1=ALU.mult), [esel2, sm], [oh2])
                    dv(lambda e: e.tensor_tensor(out=col(9), in0=col(8), in1=col(7), op=ALU.subtract), [sm], [sm])
                    dv(lambda e: e.activation(out=col(10), in_=col(9), func=AF.Exp), [sm], [sm], "act")
                    dv(lambda e: e.tensor_scalar(out=col(11), in0=col(10), scalar1=1.0, scalar2=0.0, op0=ALU.add, op1=ALU.add), [sm], [sm])
                    dv(lambda e: e.reciprocal(out=col(12), in_=col(11)), [sm], [sm])
                    dv(lambda e, t=t: e.tensor_tensor(out=wts.t[:, t:t + 1], in0=col(12), in1=col(6), op=ALU.mult), [sm], [wts])
                    dv(lambda e, t=t: e.tensor_tensor(out=wts.t[:, 32 + t:33 + t], in0=col(6), in1=wts.t[:, t:t + 1], op=ALU.subtract), [sm, wts], [wts])
                    for g_ in range(4):
                        dv(lambda e, g_=g_, t=t: e.tensor_scalar(out=M1all.t[:, t, 8 * g_:8 * g_ + 8], in0=oh1.t[:], scalar1=goh.t[:, g_:g_ + 1], scalar2=0.0,
                                                                 op0=ALU.mult, op1=ALU.add), [oh1, goh], [M1all.reg(t)])
                        dv(lambda e, g_=g_, t=t: e.tensor_scalar(out=M2all.t[:, t, 8 * g_:8 * g_ + 8], in0=oh2.t[:], scalar1=goh.t[:, g_:g_ + 1], scalar2=0.0,
                                                                 op0=ALU.mult, op1=ALU.add), [oh2, goh], [M2all.reg(t)])
                    dv(lambda e, t=t: e.tensor_tensor(out=Mt.t[:], in0=M1all.t[:, t, :], in1=M2all.t[:, t, :], op=ALU.add), [M1all.reg(t), M2all.reg(t)], [Mt])
                    psr = PF[3]
                    dv(lambda e: e.matmul(psr.t[:, 0:32], ustr.t[:], Mt.t[:], start=True, stop=False), [ustr, Mt], [psr], "pe")
                    dv(lambda e: e.matmul(psr.t[:, 0:32], onesF.t[:], Msum.t[:], start=False, stop=True), [onesF, Msum], [psr], "pe")
                    dv(lambda e, t=t: e.tensor_copy(out=rank_all.t[:, t, :], in_=psr.t[:, 0:32]), [psr], [rank_all.reg(t)])
                    dv(lambda e: e.tensor_tensor(out=Msum.t[:], in0=Msum.t[:], in1=Mt.t[:], op=ALU.add), [Msum, Mt], [Msum])

                psc = PF[4]
                dv(lambda e: e.matmul(psc.t[0:32, 0:1], Msum.t[:], onesF.t[:, 0:1], start=True, stop=True), [Msum, onesF], [psc], "pe")
                dv(lambda e: e.tensor_copy(out=sm.t[0:32, 20:21], in_=psc.t[0:32, 0:1]), [psc], [sm])
                dv(lambda e: e.tensor_scalar(out=cmp64.t[:], in0=thr_sb.t[0:32, :], scalar1=sm.t[0:32, 20:21], scalar2=1.0, op0=ALU.is_lt, op1=ALU.mult), [thr_sb, sm], [cmp64])
                dv(lambda e: e.reduce_sum(out=sm.t[0:32, 21:22], in_=cmp64.t[:], axis=AX.X), [cmp64], [sm])
                dv(lambda e: e.tensor_scalar(out=sm.t[0:32, 22:23], in0=sm.t[0:32, 21:22], scalar1=128.0, scalar2=0.0, op0=ALU.mult, op1=ALU.add), [sm], [sm])
                dv(lambda e: e.tensor_scalar(out=padrep.t[:], in0=onesF.t[0:32, :], scalar1=sm.t[0:32, 22:23], scalar2=0.0, op0=ALU.mult, op1=ALU.add), [onesF, sm], [padrep])
                pso, pso2 = PF[5], PF[4]
                dv(lambda e: e.matmul(pso.t[:, 0:32], padrep.t[:], ustr.t[0:32, 0:32], start=True, stop=True), [padrep, ustr], [pso], "pe")
                dv(lambda e: e.matmul(pso2.t[:, 0:32], padrep.t[:], ident_f.t[0:32, 0:32], start=True, stop=True), [padrep, ident_f], [pso2], "pe")
                dv(lambda e: e.tensor_copy(out=offs_sb.t[:], in_=pso.t[:, 0:32]), [pso], [offs_sb])
                dv(lambda e: e.tensor_tensor(out=pend_sb.t[:], in0=pso2.t[:, 0:32], in1=offs_sb.t[:], op=ALU.add), [pso2, offs_sb], [pend_sb])
                for t in range(32):
                    dv(lambda e, t=t: e.tensor_tensor(out=tmp32.t[:], in0=rank_all.t[:, t, :], in1=offs_sb.t[:], op=ALU.add), [rank_all.reg(t), offs_sb], [tmp32])
                    dv(lambda e, t=t: e.tensor_tensor(out=tmp32b.t[:], in0=tmp32.t[:], in1=M1all.t[:, t, :], op=ALU.mult), [tmp32, M1all.reg(t)], [tmp32b])
                    dv(lambda e, t=t: e.reduce_sum(out=posf.t[:, t:t + 1], in_=tmp32b.t[:], axis=AX.X), [tmp32b], [posf])
                    dv(lambda e, t=t: e.tensor_tensor(out=tmp32b.t[:], in0=tmp32.t[:], in1=M2all.t[:, t, :], op=ALU.mult), [tmp32, M2all.reg(t)], [tmp32b])
                    dv(lambda e, t=t: e.reduce_sum(out=posf.t[:, 32 + t:33 + t], in_=tmp32b.t[:], axis=AX.X), [tmp32b], [posf])
                dv(lambda e: e.tensor_copy(out=posi.t[:], in_=posf.t[:]), [posf], [posi])
                dv(lambda e: e.tensor_scalar(out=tmp32.t[:], in0=pend_sb.t[:], scalar1=cst_sb.t[:, 0:1], scalar2=1.0, op0=ALU.is_le, op1=ALU.mult), [pend_sb, cst_sb], [tmp32])
                dv(lambda e: e.reduce_sum(out=sm.t[:, 24:25], in_=tmp32.t[:], axis=AX.X), [tmp32], [sm])
                dv(lambda e: e.tensor_scalar(out=sm.t[:, 25:26], in0=sm.t[:, 24:25], scalar1=float(NEXP - 1), scalar2=0.0, op0=ALU.min, op1=ALU.add), [sm], [sm])
                beb = P.sb("beb", [128, 128], F32)
                widf = P.sb("widf", [128, 128], F32)
                dv(lambda e: e.tensor_scalar(out=beb.t[:], in0=onesF.t[:], scalar1=sm.t[:, 25:26], scalar2=0.0, op0=ALU.mult, op1=ALU.add), [onesF, sm], [beb])
                psb = PF[5]
                dv(lambda e: e.matmul(psb.t[:, 0:128], beb.t[:], ident_f.t[:], start=True, stop=True), [beb, ident_f], [psb], "pe")
                dv(lambda e: e.tensor_scalar(out=widf.t[:], in0=psb.t[:, 0:128], scalar1=128.0, scalar2=cst_sb.t[:, 1:2], op0=ALU.mult, op1=ALU.add),
                   [psb, cst_sb], [widf])
                berep = P.sb("berep", [128, 128], F32)
                eqs = P.sb("eqs", [128, 128], F32)
                dv(lambda e: e.tensor_copy(out=berep.t[:], in_=psb.t[:, 0:128]), [psb], [berep])
                dv(lambda e: e.tensor_tensor(out=eqs.t[:, 1:128], in0=berep.t[:, 1:128], in1=berep.t[:, 0:127], op=ALU.is_equal), [berep], [eqs])
                dv(lambda e: e.memset(eqs.t[:, 48:49], 0.0), [], [eqs])
                dv(lambda e: e.scalar_tensor_tensor(out=widf.t[:, 1:128], in0=eqs.t[:, 1:128], scalar=1.0e6, in1=widf.t[:, 1:128], op0=ALU.mult, op1=ALU.add),
                   [eqs, widf], [widf])
                dv(lambda e: e.tensor_copy(out=widx.t[:], in_=widf.t[:]), [widf], [widx])
                for t in range(32):
                    for k in range(2):
                        ci = k * 32 + t
                        sch.custom_dma("pool", ch_sc, lambda e, t=t, ci=ci: e.indirect_dma_start(
                            out=XBUF[:, :], out_offset=bass.IndirectOffsetOnAxis(ap=posi.t[:, ci:ci + 1], axis=0),
                            in_=H2.t[:, t, :], in_offset=None, bounds_check=bc_pos, oob_is_err=False), reads=[H2.reg(t), posi])
                sch.barrier()

        phaseE()
        if debug and "posi" in debug:
            d_posi = nc.dram_tensor("posi", [128, 64], I32, kind="ExternalOutput").ap()
            d_widx = nc.dram_tensor("widx", [128, 128], I32, kind="ExternalOutput").ap()
            d_wts = nc.dram_tensor("wts", [128, 64], F32, kind="ExternalOutput").ap()
            ch_d = sch.chan("dbg")
            sch.dma("sp", ch_d, d_posi[:, :], posi.t[:], reads=[posi])
            sch.dma("sp", ch_d, d_widx[:, :], widx.t[:], reads=[widx])
            sch.dma("sp", ch_d, d_wts[:, :], wts.t[:], reads=[wts])
            sch.barrier()
        if stop_after == "E":
            return nc

        def phaseF():
            pes = contextlib.ExitStack()
            with pes:
                P = Pool_(nc, pes)
                xbs = [P.sb("xb%d" % i, [128, D], BF16) for i in range(2)]
                xbT = [P.sb("xbT%d" % i, [128, D], BF16) for i in range(2)]
                w1s = [P.sb("w1_%d" % i, [128, 16, FF], BF16) for i in range(2)]
                w3s = [P.sb("w3_%d" % i, [128, 16, FF], BF16) for i in range(2)]
                w2s = [P.sb("w2_%d" % i, [128, 4, D], BF16) for i in range(2)]
                s1s = [P.sb("s1_%d" % i, [128, FF], F32) for i in range(2)]
                ubs = [P.sb("ub%d" % i, [128, FF], BF16) for i in range(2)]
                uTs = [P.sb("uT%d" % i, [128, FF], BF16) for i in range(2)]
                ybs = [P.sb("yb%d" % i, [128, D], F32) for i in range(2)]
                ch_xb = [sch.chan("f_xb%d" % i) for i in range(2)]
                ch_w = [sch.chan("f_w%d" % i, sw=True) for i in range(2)]
                ch_y = [sch.chan("f_y%d" % i) for i in range(2)]

                def dv(fn, reads, writes, eng="dve"):
                    sch.op(eng, fn, reads=reads, writes=writes)

                wg_rows = w_e_gate.rearrange("e (p c) n -> (e p) (c n)", c=16).rearrange("r (a m) -> (r a) m", m=2048)
                wu_rows = w_e_up.rearrange("e (p c) n -> (e p) (c n)", c=16).rearrange("r (a m) -> (r a) m", m=2048)
                wd_rows = w_e_down.rearrange("e (p c) n -> (e p) (c n)", c=4).rearrange("r (a m) -> (r a) m", m=2048)

                NSTR = 2
                PER = NBLK // NSTR

                def blk(it):
                    return it % NSTR, (it % NSTR) * PER + it // NSTR

                def load_x(it):
                    _, b = blk(it)
                    sch.dma("sp", ch_xb[it % 2], xbs[it % 2].t[:], XBUF[b * 128:(b + 1) * 128, :], writes=[xbs[it % 2]])

                def load_w(it):
                    sidx, b = blk(it)
                    w1, w3, w2 = w1s[sidx], w3s[sidx], w2s[sidx]
                    for (dst, srcv) in ((w1, WG16), (w3, WU16), (w2, WD16)):
                        sch.custom_dma("pool", ch_w[sidx], lambda e, dst=dst, srcv=srcv, b=b: e.indirect_dma_start(
                            out=dst.t[:].rearrange("p c n -> p (c n)"), out_offset=None, in_=srcv[:, :],
                            in_offset=bass.IndirectOffsetOnAxis(ap=widx.t[:, b:b + 1], axis=0),
                            bounds_check=bc_w, oob_is_err=False), reads=[widx], writes=[dst])
                    for bb in (w1, w3, w2):
                        sch.fence(ch_w[sidx], bb)

                def transpose_x(it):
                    xb, xT_ = xbs[it % 2], xbT[it % 2]
                    for half in range(2):
                        pb = PB[half]
                        for j in range(8):
                            c = half * 8 + j
                            dv(lambda e, pb=pb, j=j, c=c: e.transpose(out=pb.t[:, j * 128:(j + 1) * 128], in_=xb.t[:].rearrange("q (p c) -> q c p", c=16)[:, c, :], identity=ident_bf.t[:]),
                               [xb, ident_bf], [pb], "pe")
                        if half == 0:
                            dv(lambda e, pb=pb: e.activation(out=xT_.t[:, 0:1024], in_=pb.t[:], func=AF.Copy), [pb], [xT_.reg(0)], "act")
                        else:
                            dv(lambda e, pb=pb: e.tensor_copy(out=xT_.t[:, 1024:2048], in_=pb.t[:]), [pb], [xT_.reg(1)])

                load_x(0)
                for it0 in range(NSTR):
                    load_w(it0)
                transpose_x(0)
                for it in range(NBLK):
                    i = it % 2
                    sidx, b = blk(it)
                    if it + 1 < NBLK:
                        load_x(it + 1)
                    xb, xT_, w1, w3, w2 = xbs[i], xbT[i], w1s[sidx], w3s[sidx], w2s[sidx]
                    ps1, ps3 = PF[0], PF[1]
                    for c in range(16):
                        dv(lambda e, c=c: e.matmul(ps1.t[:], xT_.t[:, c * 128:(c + 1) * 128], w1.t[:, c, :], start=(c == 0), stop=(c == 15)),
                           [xT_.reg(c // 8), w1], [ps1], "pe")
                    for c in range(16):
                        dv(lambda e, c=c: e.matmul(ps3.t[:], xT_.t[:, c * 128:(c + 1) * 128], w3.t[:, c, :], start=(c == 0), stop=(c == 15)),
                           [xT_.reg(c // 8), w3], [ps3], "pe")
                    s1, ub, uT, yb = s1s[i], ubs[i], uTs[i], ybs[i]
                    dv(lambda e: e.activation(out=s1.t[:], in_=ps1.t[:], func=AF.Silu), [ps1], [s1], "act")
                    dv(lambda e: e.tensor_tensor(out=ub.t[:], in0=ps3.t[:], in1=s1.t[:], op=ALU.mult), [ps3, s1], [ub])
                    if it + 1 < NBLK:
                        transpose_x(it + 1)
                    pbu = PB[0]
                    for k in range(4):
                        dv(lambda e, k=k: e.transpose(out=pbu.t[:, k * 128:(k + 1) * 128], in_=ub.t[:].rearrange("q (p c) -> q c p", c=4)[:, k, :], identity=ident_bf.t[:]),
                           [ub, ident_bf], [pbu], "pe")
                    dv(lambda e: e.tensor_copy(out=uT.t[:], in_=pbu.t[:, 0:512]), [pbu], [uT])
                    for n in range(4):
                        py = PF[2 + n]
                        for k in range(4):
                            dv(lambda e, k=k, n=n, py=py: e.matmul(py.t[:], uT.t[:, k * 128:(k + 1) * 128], w2.t[:, k, n * 512:(n + 1) * 512], start=(k == 0), stop=(k == 3)),
                               [uT, w2], [py], "pe")
                        if n % 2 == 0:
                            dv(lambda e, n=n, py=py: e.activation(out=yb.t[:, n * 512:(n + 1) * 512], in_=py.t[:], func=AF.Copy), [py], [yb.reg(n)], "act")
                        else:
                            dv(lambda e, n=n, py=py: e.tensor_copy(out=yb.t[:, n * 512:(n + 1) * 512], in_=py.t[:]), [py], [yb.reg(n)])
                    sch.dma("sp", ch_y[i], OBUF[b * 128:(b + 1) * 128, :], yb.t[:], reads=[yb])
                    if it + NSTR < NBLK:
                        load_w(it + NSTR)
                sch.barrier()

        phaseF()
        if stop_after == "F":
            return nc

        def phaseG():
            pes = contextlib.ExitStack()
            with pes:
                P = Pool_(nc, pes)
                gfin_b = P.sb("gfin_b", [128, D], F32)
                x1s = [P.sb("gx1_%d" % i, [128, D], F32) for i in range(2)]
                o1s = [P.sb("go1_%d" % i, [128, D], F32) for i in range(2)]
                o2s = [P.sb("go2_%d" % i, [128, D], F32) for i in range(2)]
                sm = P.sb("gsm", [128, 8], F32)
                ch_g = sch.chan("g_c")
                ch_x = [sch.chan("g_x%d" % i) for i in range(2)]
                ch_o1 = [sch.chan("g_o1%d" % i, sw=True) for i in range(2)]
                ch_o2 = [sch.chan("g_o2%d" % i, sw=True) for i in range(2)]
                ch_y = [sch.chan("g_y%d" % i) for i in range(2)]
                sch.dma("sp", ch_g, gfin_b.t[:], g_final.rearrange("(o n) -> o n", o=1).partition_broadcast(128), writes=[gfin_b])

                def dv(fn, reads, writes, eng="dve"):
                    sch.op(eng, fn, reads=reads, writes=writes)

                def col(i):
                    return sm.t[:, i:i + 1]

                for t in range(32):
                    i = t % 2
                    rows = slice(t * 128, (t + 1) * 128)
                    x1b, o1, o2 = x1s[i], o1s[i], o2s[i]
                    sch.dma("sp", ch_x[i], x1b.t[:], X1[rows, :], writes=[x1b])
                    sch.custom_dma("pool", ch_o1[i], lambda e, o1=o1, t=t: e.indirect_dma_start(
                        out=o1.t[:, :], out_offset=None, in_=OBUF[:, :],
                        in_offset=bass.IndirectOffsetOnAxis(ap=posi.t[:, t:t + 1], axis=0), bounds_check=bc_pos, oob_is_err=False),
                        reads=[posi], writes=[o1])
                    sch.custom_dma("pool", ch_o2[i], lambda e, o2=o2, t=t: e.indirect_dma_start(
                        out=o2.t[:, :], out_offset=None, in_=OBUF[:, :],
                        in_offset=bass.IndirectOffsetOnAxis(ap=posi.t[:, 32 + t:33 + t], axis=0), bounds_check=bc_pos, oob_is_err=False),
                        reads=[posi], writes=[o2])
                    dv(lambda e, t=t, o1=o1, x1b=x1b: e.scalar_tensor_tensor(out=x1b.t[:], in0=o1.t[:], scalar=wts.t[:, t:t + 1], in1=x1b.t[:], op0=ALU.mult, op1=ALU.add),
                       [o1, wts, x1b], [x1b])
                    dv(lambda e, t=t, o2=o2, x1b=x1b: e.scalar_tensor_tensor(out=x1b.t[:], in0=o2.t[:], scalar=wts.t[:, 32 + t:33 + t], in1=x1b.t[:], op0=ALU.mult, op1=ALU.add),
                       [o2, wts, x1b], [x1b])
                    dv(lambda e: e.memset(sm.t[:, 0:4], 0.0), [], [sm])
                    dv(lambda e, o1=o1, x1b=x1b: e.activation(out=o1.t[:], in_=x1b.t[:], func=AF.Square, accum_out=col(0)), [x1b, sm], [o1, sm], "act")
                    dv(lambda e: e.activation(out=col(1), in_=col(0), func=AF.Ln, scale=1.0 / D, bias=eps_sb.t[:, 0:1]), [sm, eps_sb], [sm], "act")
                    dv(lambda e: e.activation(out=col(2), in_=col(1), func=AF.Exp, scale=-0.5), [sm], [sm], "act")
                    dv(lambda e, o2=o2, x1b=x1b: e.scalar_tensor_tensor(out=o2.t[:], in0=x1b.t[:], scalar=col(2), in1=gfin_b.t[:], op0=ALU.mult, op1=ALU.mult),
                       [x1b, sm, gfin_b], [o2])
                    sch.dma("sp", ch_y[i], y[rows, :], o2.t[:], reads=[o2])
                sch.barrier()

        phaseG()

    return nc


def host_inputs(inputs, cores=None):
    x = np.asarray(inputs["x"], dtype=np.float32)
    f32 = np.float32
    inv = (10000.0 ** (-np.arange(0, 64, 2, dtype=np.float32) / np.float32(64))).astype(f32)
    rmat = np.zeros((128, 128), f32)
    for blk in range(2):
        for i in range(32):
            rmat[blk * 64 + i + 32, blk * 64 + i] = -1.0
            rmat[blk * 64 + i, blk * 64 + i + 32] = 1.0
    ident = np.eye(128, dtype=f32)
    consts = np.zeros((128, 64), f32)
    consts[:, 0] = 128.0 * np.arange(128)
    consts[:, 1] = np.arange(128)
    thr = np.ascontiguousarray(np.broadcast_to(128.0 * np.arange(64, dtype=f32)[None, :], (128, 64)), f32)
    shared = {
        "g_mix": np.ascontiguousarray(inputs["g_mix"][0], f32),
        "w_in": np.ascontiguousarray(inputs["w_in"][0], f32),
        "q_norm_a": np.ascontiguousarray(inputs["q_norm_a"][0], f32),
        "k_norm_a": np.ascontiguousarray(inputs["k_norm_a"][0], f32),
        "lam4": np.ascontiguousarray(np.stack([inputs["lam_q1"][0], inputs["lam_k1"][0],
                                               inputs["lam_q2"][0], inputs["lam_k2"][0]]), f32),
        "subln_b": np.ascontiguousarray(inputs["subln_b"][0], f32),
        "w_branch_a": np.ascontiguousarray(inputs["w_branch_a"][0], f32),
        "w_branch_b": np.ascontiguousarray(inputs["w_branch_b"][0], f32),
        "w_gate": np.ascontiguousarray(inputs["w_gate"][0], f32),
        "b_gate": np.ascontiguousarray(inputs["b_gate"][0], f32),
        "w_out": np.ascontiguousarray(inputs["w_out"][0], f32),
        "g_ffn": np.ascontiguousarray(inputs["g_ffn"][0], f32),
        "w_router": np.ascontiguousarray(np.concatenate([inputs["w_router_group"][0],
                                                         inputs["w_router_expert"][0]], axis=1), f32),
        "w_e_gate": np.ascontiguousarray(inputs["w_e_gate"][0], f32),
        "w_e_up": np.ascontiguousarray(inputs["w_e_up"][0], f32),
        "w_e_down": np.ascontiguousarray(inputs["w_e_down"][0], f32),
        "g_final": np.ascontiguousarray(inputs["g_final"], f32),
        "rmat": rmat, "ident": ident, "consts": consts,
        "ustrict": np.triu(np.ones((128, 128), f32), 1), "thr": thr,
    }
    in_maps = []
    for core in (range(NCORES) if cores is None else cores):
        b, hf = core // 2, core % 2
        own = np.arange(hf * OWN, (hf + 1) * OWN)
        oth = np.arange((1 - hf) * OWN, (2 - hf) * OWN)
        perm = np.concatenate([own, oth])
        xb = x[b]
        m = dict(shared)
        m["xT"] = np.ascontiguousarray(xb[perm].T)
        m["x_own"] = np.ascontiguousarray(xb[own])
        pos = perm.astype(f32)
        rows = (perm // 64).astype(f32)
        cols = (perm % 64).astype(f32)
        angR = rows[None, :] * inv[:, None]
        angC = cols[None, :] * inv[:, None]
        angP = pos[None, :] * inv[:, None]
        m["cosA"] = np.ascontiguousarray(np.concatenate([np.cos(angR)] * 2 + [np.cos(angC)] * 2, 0), f32)
        m["sinA"] = np.ascontiguousarray(np.concatenate([np.sin(angR)] * 2 + [np.sin(angC)] * 2, 0), f32)
        m["cosB"] = np.ascontiguousarray(np.concatenate([np.cos(angP)] * 4, 0), f32)
        m["sinB"] = np.ascontiguousarray(np.concatenate([np.sin(angP)] * 4, 0), f32)
        in_maps.append(m)
    return in_maps


def kernel(**inputs):
    in_maps = host_inputs(inputs)
    nc = build()
    res = run_bass_kernel_spmd(nc, in_maps, core_ids=list(range(NCORES)))
    out = np.zeros((4, S, D), np.float32)
    for core in range(NCORES):
        b, hf = core // 2, core % 2
        out[b, hf * OWN:(hf + 1) * OWN] = res.results[core]["y"]
    return out
```

```python
import contextlib
import numpy as np
import concourse.bass as bass
import concourse.mybir as mybir
from concourse.bass_utils import run_bass_kernel_spmd

F32 = mybir.dt.float32
BF16 = mybir.dt.bfloat16
I32 = mybir.dt.int32
AF = mybir.ActivationFunctionType
ALU = mybir.AluOpType
AX = mybir.AxisListType

D = 2048
S = 8192
OWN = 4096
NCORES = 8
EPS = 1e-6
NEXP = 32
FF = 512
NBLK = 96
NPOS = NBLK * 128
IN_COLS = 4608


class Buf:
    def __init__(self, name, t=None, parent=None):
        self.name, self.t, self.parent = name, t, parent
        self.w, self.r, self.kids = {}, {}, {}

    def reg(self, key):
        if key not in self.kids:
            self.kids[key] = Buf("%s.%s" % (self.name, key), self.t, self)
        return self.kids[key]


def _merge(d, s):
    for k, v in s.items():
        if d.get(k, 0) < v:
            d[k] = v


class Sched:
    def __init__(self, nc, es):
        self.nc, self.es = nc, es
        self.engs = {"pe": nc.tensor, "act": nc.scalar, "dve": nc.vector, "pool": nc.gpsimd, "sp": nc.sync}
        self.sems, self.cnt, self.cur = {}, {}, {}
        self.known = {e: {} for e in self.engs}
        self.nsem = 0
        self.phase = 0
        for e in self.engs:
            self._newkey(e)

    def _newkey(self, e):
        key = "%s#%d" % (e, self.phase)
        self._newsem(key)
        self.cur[e] = key

    def _newsem(self, key):
        h = self.es.enter_context(self.nc.semaphore("s%d" % self.nsem))
        self.nsem += 1
        self.sems[key] = h
        self.cnt[key] = 0

    def chan(self, name, persistent=False, sw=False):
        self.nchan = getattr(self, "nchan", 0) + 1
        key = "dma_%s_%d" % (name, self.nchan)
        if not hasattr(self, "free_chans"):
            self.free_chans, self.live_chans = {True: [], False: []}, []
        free = self.free_chans[sw]
        if free and not persistent:
            h, base = free.pop()
            self.sems[key] = h
            self.cnt[key] = base
        else:
            self._newsem(key)
        if not persistent:
            self.live_chans.append((key, sw))
        return key

    def _deps(self, reads, writes):
        d = {}
        for b in reads:
            _merge(d, b.w)
            if b.parent is not None:
                _merge(d, b.parent.w)
            for k in b.kids.values():
                _merge(d, k.w)
        for b in writes:
            xs = [b] + ([b.parent] if b.parent is not None else []) + list(b.kids.values())
            for x in xs:
                _merge(d, x.w)
                _merge(d, x.r)
        return d

    def _wait(self, e, deps):
        kn = self.known[e]
        for k, v in deps.items():
            if e == "pe" and k == self.cur["pe"]:
                continue
            if k == self.cur[e] and v <= self.cnt[k] - 3:
                continue
            if kn.get(k, 0) < v:
                self.engs[e].wait_ge(self.sems[k], v)
                kn[k] = v

    def _commit(self, tok, reads, writes):
        k, v = tok
        for b in reads:
            if b.r.get(k, 0) < v:
                b.r[k] = v
        for b in writes:
            b.w = {k: v}
            b.r = {}
            for kid in b.kids.values():
                kid.w, kid.r = {}, {}

    def op(self, e, fn, reads=(), writes=()):
        self._wait(e, self._deps(reads, writes))
        ins = fn(self.engs[e])
        key = self.cur[e]
        self.cnt[key] += 1
        assert self.cnt[key] < 60000, key
        ins.then_inc(self.sems[key], 1)
        self._commit((key, self.cnt[key]), reads, writes)

    def dma(self, q, ch, out, in_, reads=(), writes=(), **kw):
        self._wait(q, self._deps(reads, writes))
        ins = self.engs[q].dma_start(out=out, in_=in_, **kw)
        self.cnt[ch] += 16
        assert self.cnt[ch] < 60000, ch
        ins.then_inc(self.sems[ch], 16)
        self._commit((ch, self.cnt[ch]), reads, writes)

    def custom_dma(self, q, ch, fn, reads=(), writes=()):
        self._wait(q, self._deps(reads, writes))
        ins = fn(self.engs[q])
        self.cnt[ch] += 16
        ins.then_inc(self.sems[ch], 16)
        self._commit((ch, self.cnt[ch]), reads, writes)

    def fence(self, ch, buf, readers=False):
        v = self.cnt[ch]
        for b in [buf] + list(buf.kids.values()):
            d = b.r if readers else b.w
            if ch in d or b is buf:
                d[ch] = v

    def wait_all(self, e, keys=None):
        tot = {k: v for k, v in self.cnt.items() if v > 0 and (keys is None or k in keys)}
        self._wait(e, tot)

    def barrier(self):
        tot = {k: v for k, v in self.cnt.items() if v > 0}
        for e in self.engs:
            kn = self.known[e]
            for k, v in tot.items():
                if k == self.cur[e]:
                    continue
                if kn.get(k, 0) < v:
                    self.engs[e].wait_ge(self.sems[k], v)
                    kn[k] = v
        for e in self.engs:
            for k, v in tot.items():
                self.known[e][k] = v
        self.phase += 1
        for e in self.engs:
            self._newkey(e)
        for (key, sw) in getattr(self, "live_chans", []):
            self.free_chans[sw].append((self.sems[key], self.cnt[key]))
        self.live_chans = []


class Pool_:
    N = [0]

    def __init__(self, nc, es):
        self.nc, self.es = nc, es

    def sb(self, name, shape, dt):
        Pool_.N[0] += 1
        t = self.es.enter_context(self.nc.sbuf_tensor("%s_%d" % (name, Pool_.N[0]), list(shape), dt))
        return Buf(name, t)


LCOL_FLAG = True


def build(stop_after=None, debug=False):
    nc = bass.Bass("TRN2", target_bir_lowering=False)

    def din(name, shape, dt=F32):
        return nc.dram_tensor(name, list(shape), dt, kind="ExternalInput").ap()

    def dscr(name, shape, dt):
        kind = "ExternalOutput" if (debug and name in debug) else "Internal"
        return nc.dram_tensor(name, list(shape), dt, kind=kind).ap()

    xT = din("xT", [D, S])
    x_own = din("x_own", [OWN, D])
    g_mix = din("g_mix", [D])
    w_in = din("w_in", [D, IN_COLS])
    q_norm_a = din("q_norm_a", [128])
    k_norm_a = din("k_norm_a", [128])
    lam4 = din("lam4", [4, 64])
    subln_b = din("subln_b", [128])
    w_branch_a = din("w_branch_a", [1024, D])
    w_branch_b = din("w_branch_b", [1024, D])
    w_gate = din("w_gate", [D, 2 * D])
    b_gate = din("b_gate", [2 * D])
    w_out = din("w_out", [D, D])
    g_ffn = din("g_ffn", [D])
    w_router = din("w_router", [D, 36])
    w_e_gate = din("w_e_gate", [NEXP, D, FF])
    w_e_up = din("w_e_up", [NEXP, D, FF])
    w_e_down = din("w_e_down", [NEXP, FF, D])
    g_final = din("g_final", [D])
    cosA = din("cosA", [128, S])
    sinA = din("sinA", [128, S])
    cosB = din("cosB", [128, S])
    sinB = din("sinB", [128, S])
    rmat = din("rmat", [128, 128])
    ident = din("ident", [128, 128])
    consts = din("consts", [128, 64])
    ustrict = din("ustrict", [128, 128])
    thr = din("thr", [128, 64])

    y = nc.dram_tensor("y", [OWN, D], F32, kind="ExternalOutput").ap()

    KAT = dscr("KAT", [2, 128, S], BF16)
    VA = dscr("VA", [S, 256], BF16)
    QAT = dscr("QAT", [8, 128, OWN], BF16)
    KBT = dscr("KBT", [8, 128, S], BF16)
    QBT = dscr("QBT", [8, 128, OWN], BF16)
    VB = dscr("VB", [S, 1024], BF16)
    HT = dscr("HT", [D, OWN], BF16)
    OT = dscr("OT", [D, OWN], BF16)
    MT = dscr("MT", [D, OWN], BF16)
    X1 = dscr("X1", [OWN, D], F32)
    XBUF = dscr("XBUF", [NPOS, D], BF16)
    OBUF = dscr("OBUF", [NPOS, D], F32)
    WD1 = dscr("WD1", [8, 128, 12288], BF16)
    WO16 = dscr("WO16", [4, 128, 8192], BF16)
    WG16 = dscr("WG16", [NEXP * 128, 8192], BF16)
    WU16 = dscr("WU16", [NEXP * 128, 8192], BF16)
    WD16 = dscr("WD16", [NEXP * 128, 8192], BF16)

    es = contextlib.ExitStack()
    with es:
        sch = Sched(nc, es)
        P0 = Pool_(nc, es)

        ps_es = contextlib.ExitStack()
        es.enter_context(ps_es)
        PS = []
        for i in range(8):
            t = ps_es.enter_context(nc.psum_tensor("ps%d" % i, [128, 512], F32))
            PS.append(Buf("ps%d" % i, t))

        ones_bf = P0.sb("ones_bf", [128, 128], BF16)
        rmat_bf = P0.sb("rmat_bf", [128, 128], BF16)
        ident_bf = P0.sb("ident_bf", [128, 128], BF16)
        ident_f = P0.sb("ident_f", [128, 128], F32)
        gmix_sb = P0.sb("gmix_sb", [128, 16], F32)
        qn_sb = P0.sb("qn_sb", [128, 1], F32)
        kn_sb = P0.sb("kn_sb", [128, 1], F32)
        ch_c = sch.chan("const", persistent=True)
        ch_cp = sch.chan("constp", persistent=True, sw=True)
        sch.op("dve", lambda e: e.memset(ones_bf.t[:], 1.0), writes=[ones_bf])
        eps_sb = P0.sb("eps_sb", [128, 1], F32)
        sch.op("dve", lambda e: e.memset(eps_sb.t[:], EPS), writes=[eps_sb])
        sch.dma("pool", ch_cp, rmat_bf.t[:], rmat[:, :], writes=[rmat_bf])
        sch.dma("pool", ch_cp, ident_bf.t[:], ident[:, :], writes=[ident_bf])
        sch.dma("sp", ch_c, ident_f.t[:], ident[:, :], writes=[ident_f])
        sch.dma("sp", ch_c, gmix_sb.t[:], g_mix.rearrange("(c p) -> p c", p=128), writes=[gmix_sb],
                allow_slow_non_contiguous=True)
        sch.dma("sp", ch_c, qn_sb.t[:], q_norm_a.rearrange("(p o) -> p o", o=1), writes=[qn_sb])
        sch.dma("sp", ch_c, kn_sb.t[:], k_norm_a.rearrange("(p o) -> p o", o=1), writes=[kn_sb])


        zero_bf = P0.sb("zero_bf", [128, D], BF16)
        ch_z = sch.chan("zfill", persistent=True)
        sch.op("pool", lambda e: e.memset(zero_bf.t[:], 0.0), writes=[zero_bf])
        for b_ in range(NBLK):
            sch.dma("sp", ch_z, XBUF[b_ * 128:(b_ + 1) * 128, :], zero_bf.t[:], reads=[zero_bf])

        xT_v = xT.rearrange("(c p) t -> p c t", p=128)
        w_in_v = w_in.rearrange("(c p) n -> p c n", p=128)

        def phaseA(passno):
            pes = contextlib.ExitStack()
            with pes:
                P = Pool_(nc, pes)
                if passno == 1:
                    segs = [(1024, 512), (2560, 2048)]
                    ntiles = 16
                else:
                    segs = [(0, 1024), (1536, 1024)]
                    ntiles = 8
                ncols = sum(n for _, n in segs)
                wsb = P.sb("wsb", [128, 16, ncols], BF16)
                ch_w = sch.chan("wA", sw=True)
                off = 0
                segoff = {}
                for (c0, n) in segs:
                    segoff[c0] = off
                    for c in range(16):
                        for p0 in range(0, n, 512):
                            sch.dma("pool", ch_w, wsb.t[:, c, off + p0:off + p0 + 512], w_in_v[:, c, c0 + p0:c0 + p0 + 512],
                                    writes=[wsb.reg((c0, c, p0))])
                    off += n
                sch.fence(ch_w, wsb)
                xt = P.sb("xt", [128, 16, 512], F32)
                hts = [P.sb("ht%d" % i, [128, 16, 512], BF16) for i in range(2)]
                sqs = [P.sb("sq%d" % i, [128, 512], BF16) for i in range(8)]
                rstd = P.sb("rstd", [128, 512], F32)
                cs = [[P.sb("cs%d_%d" % (i, j), [128, 512], F32) for j in range(4)] for i in range(2)]
                xgs = [P.sb("xg%d" % i, [128, 512], BF16) for i in range(2)]
                sq2s = [P.sb("sq2_%d" % i, [128, 512], BF16) for i in range(2)]
                rs2 = [P.sb("rs2_%d" % i, [128, 512], F32) for i in range(2)]
                t1s = [P.sb("t1_%d" % i, [128, 512], F32) for i in range(2)]
                t2s = [P.sb("t2_%d" % i, [128, 512], F32) for i in range(2)]
                outs = [P.sb("out%d" % i, [128, 512], BF16) for i in range(3)]
                vts = [P.sb("vt%d" % i, [128, 512], BF16) for i in range(3)]
                ch_x = sch.chan("xA")
                ch_cs = [sch.chan("csA%d" % i) for i in range(2)]
                ch_out = [sch.chan("outA%d" % i) for i in range(3)]
                ch_vt = [sch.chan("vtA%d" % i) for i in range(3)]
                ch_ht = [sch.chan("htA%d" % i) for i in range(2)]
                ps_main = [PS[0], PS[1], PS[2]]
                ps_ss, ps_rot, ps_ss2 = PS[3], PS[4], PS[5]
                cnt = {"main": 0, "ep": 0, "out": 0, "vt": 0}

                def prep_load(t):
                    tk = slice(t * 512, (t + 1) * 512)
                    csb = cs[t % 2]
                    for c4 in range(4):
                        sch.dma("sp", ch_x, xt.t[:, 4 * c4:4 * c4 + 4, :], xT_v[:, 4 * c4:4 * c4 + 4, tk],
                                writes=[xt.reg(c4)])
                    sch.fence(ch_x, xt)
                    for j, tab in enumerate((cosA, sinA, cosB, sinB)):
                        sch.dma("sp", ch_cs[t % 2], csb[j].t[:], tab[:, tk], writes=[csb[j]])
                    for j in range(4):
                        csb[j].w = {ch_cs[t % 2]: sch.cnt[ch_cs[t % 2]]}

                def prep_sq(t, q):
                    for c in range(4 * q, 4 * q + 4):
                        sq = sqs[c % 8]
                        sch.op("act", lambda e, c=c, sq=sq: e.activation(out=sq.t[:], in_=xt.t[:, c, :], func=AF.Square),
                               reads=[xt.reg(c // 4)], writes=[sq])

                def prep_mm(t, q):
                    for c in range(4 * q, 4 * q + 4):
                        sq = sqs[c % 8]
                        sch.op("pe", lambda e, c=c, sq=sq: e.matmul(ps_ss.t[:], ones_bf.t[:], sq.t[:], start=(c == 0), stop=(c == 15)),
                               reads=[ones_bf, sq], writes=[ps_ss])

                def prep_fin(t):
                    tk = slice(t * 512, (t + 1) * 512)
                    ht = hts[t % 2]
                    sch.op("act", lambda e: e.activation(out=rstd.t[:], in_=ps_ss.t[:], func=AF.Ln, scale=1.0 / D, bias=eps_sb.t[:, 0:1]),
                           reads=[ps_ss, eps_sb], writes=[rstd])
                    sch.op("act", lambda e: e.activation(out=rstd.t[:], in_=rstd.t[:], func=AF.Exp, scale=-0.5),
                           reads=[rstd], writes=[rstd])
                    for c in range(16):
                        sch.op("dve", lambda e, c=c: e.scalar_tensor_tensor(out=ht.t[:, c, :], in0=xt.t[:, c, :],
                                                                            scalar=gmix_sb.t[:, c:c + 1], in1=rstd.t[:],
                                                                            op0=ALU.mult, op1=ALU.mult),
                               reads=[xt.reg(c // 4), rstd, gmix_sb], writes=[ht.reg(c)])
                    if passno == 2:
                        for c4 in range(4):
                            sch.dma("sp", ch_ht[t % 2], HT.rearrange("(c p) t -> p c t", p=128)[:, 4 * c4:4 * c4 + 4, tk],
                                    ht.t[:, 4 * c4:4 * c4 + 4, :], reads=[ht.reg(4 * c4 + i) for i in range(4)])
                        sch.fence(ch_ht[t % 2], ht, readers=True)

                def prep_step(t, n):
                    if n == 1:
                        prep_load(t)
                    if 3 <= n <= 6:
                        prep_sq(t, n - 3)
                    if 4 <= n <= 7:
                        prep_mm(t, n - 4)
                    if n == 7:
                        prep_fin(t)

                def tile_main(t):
                    tk = slice(t * 512, (t + 1) * 512)
                    ht = hts[t % 2]
                    csb = cs[t % 2]
                    if passno == 1:
                        fm = [("A", 1024 + 128 * i, kn_sb, KAT[i, :, tk]) for i in range(2)] + \
                             [("B", 2560 + 128 * i, None, KBT[i, :, tk]) for i in range(8)]
                    else:
                        fm = [("A", 128 * i, qn_sb, QAT[i, :, tk]) for i in range(8)] + \
                             [("B", 1536 + 128 * i, None, QBT[i, :, tk]) for i in range(8)]
                    nfm = [0]
                    pend = [None]
                    for (kind, c0, gsb, dst) in fm:
                        seg0 = max(s0 for (s0, n) in segs if s0 <= c0)
                        wo = segoff[seg0] + (c0 - seg0)
                        ps = ps_main[cnt["main"] % 3]
                        cnt["main"] += 1
                        for c in range(16):
                            sch.op("pe", lambda e, c=c, ps=ps, wo=wo: e.matmul(ps.t[:], wsb.t[:, c, wo:wo + 128], ht.t[:, c, :],
                                                                               start=(c == 0), stop=(c == 15)),
                                   reads=[wsb, ht.reg(c)], writes=[ps])
                        def _ep(kind=kind, gsb=gsb, dst=dst, ps=ps):
                            i2 = cnt["ep"] % 2
                            cnt["ep"] += 1
                            xg, sq2, r2, t1, t2 = xgs[i2], sq2s[i2], rs2[i2], t1s[i2], t2s[i2]
                            ob = outs[cnt["out"] % 3]
                            cho = ch_out[cnt["out"] % 3]
                            cnt["out"] += 1
                            if kind == "A":
                                cosb, sinb = csb[0], csb[1]
                                sch.op("act", lambda e, ps=ps, sq2=sq2: e.activation(out=sq2.t[:], in_=ps.t[:], func=AF.Square),
                                       reads=[ps], writes=[sq2])
                                sch.op("act", lambda e, ps=ps, xg=xg, gsb=gsb: e.activation(out=xg.t[:], in_=ps.t[:], func=AF.Copy,
                                                                                           scale=gsb.t[:, 0:1]),
                                       reads=[ps, gsb], writes=[xg])
                                sch.op("pe", lambda e, sq2=sq2: e.matmul(ps_ss2.t[:], ones_bf.t[:], sq2.t[:], start=True, stop=True),
                                       reads=[ones_bf, sq2], writes=[ps_ss2])
                            else:
                                cosb, sinb = csb[2], csb[3]
                                sch.op("act", lambda e, ps=ps, xg=xg: e.activation(out=xg.t[:], in_=ps.t[:], func=AF.Copy),
                                       reads=[ps], writes=[xg])
                            sch.op("pe", lambda e, xg=xg: e.matmul(ps_rot.t[:], rmat_bf.t[:], xg.t[:], start=True, stop=True),
                                   reads=[rmat_bf, xg], writes=[ps_rot])
                            sch.op("pool", lambda e, xg=xg, t1=t1, cosb=cosb: e.tensor_tensor(out=t1.t[:], in0=xg.t[:], in1=cosb.t[:], op=ALU.mult),
                                   reads=[xg, cosb], writes=[t1])
                            sch.op("dve", lambda e, t2=t2, sinb=sinb: e.tensor_tensor(out=t2.t[:], in0=ps_rot.t[:], in1=sinb.t[:], op=ALU.mult),
                                   reads=[ps_rot, sinb], writes=[t2])
                            if kind == "A":
                                sch.op("act", lambda e, r2=r2: e.activation(out=r2.t[:], in_=ps_ss2.t[:], func=AF.Ln, scale=1.0 / 128, bias=eps_sb.t[:, 0:1]),
                                       reads=[ps_ss2, eps_sb], writes=[r2])
                                sch.op("act", lambda e, r2=r2: e.activation(out=r2.t[:], in_=r2.t[:], func=AF.Exp, scale=-0.5),
                                       reads=[r2], writes=[r2])
                                sch.op("pool", lambda e, t1=t1, t2=t2: e.tensor_tensor(out=t1.t[:], in0=t1.t[:], in1=t2.t[:], op=ALU.add),
                                       reads=[t1, t2], writes=[t1])
                                sch.op("pool", lambda e, t1=t1, r2=r2, ob=ob: e.tensor_tensor(out=ob.t[:], in0=t1.t[:], in1=r2.t[:], op=ALU.mult),
                                       reads=[t1, r2], writes=[ob])
                            else:
                                sch.op("pool", lambda e, t1=t1, t2=t2, ob=ob: e.tensor_tensor(out=ob.t[:], in0=t1.t[:], in1=t2.t[:], op=ALU.add),
                                       reads=[t1, t2], writes=[ob])
                            sch.dma("sp", cho, dst, ob.t[:], reads=[ob])
                            nfm[0] += 1
                            if t + 1 < ntiles:
                                prep_step(t + 1, nfm[0])
                        if pend[0] is not None:
                            pend[0]()
                        pend[0] = _ep

                    pend[0]()
                    pend[0] = None
                    if passno == 1:
                        for j in range(4):
                            rows = slice(t * 512 + j * 128, t * 512 + (j + 1) * 128)
                            for (c0, n, dstt, d0) in [(1280, 256, VA, 0), (3584, 512, VB, 0), (4096, 512, VB, 512)]:
                                seg0 = max(s0 for (s0, nn) in segs if s0 <= c0)
                                wo = segoff[seg0] + (c0 - seg0)
                                ps = ps_main[cnt["main"] % 3]
                                cnt["main"] += 1
                                for c in range(16):
                                    sch.op("pe", lambda e, c=c, ps=ps, wo=wo, n=n, j=j: e.matmul(ps.t[:, 0:n], ht.t[:, c, j * 128:(j + 1) * 128],
                                                                                                 wsb.t[:, c, wo:wo + n], start=(c == 0), stop=(c == 15)),
                                           reads=[wsb, ht.reg(c)], writes=[ps])
                                vt = vts[cnt["vt"] % 3]
                                chv = ch_vt[cnt["vt"] % 3]
                                cnt["vt"] += 1
                                sch.op("act", lambda e, ps=ps, vt=vt, n=n: e.activation(out=vt.t[:, 0:n], in_=ps.t[:, 0:n], func=AF.Copy),
                                       reads=[ps], writes=[vt])
                                sch.dma("sp", chv, dstt[rows, d0:d0 + n], vt.t[:, 0:n], reads=[vt])
                for n_ in range(1, 8):
                    prep_step(0, n_)
                for t in range(ntiles):
                    tile_main(t)
                sch.barrier()

        phaseA(1)
        if stop_after == "A1":
            return nc
        phaseA(2)
        if stop_after == "A2":
            return nc

        lam_init = 0.8 - 0.6 * float(np.exp(-0.3 * 0))
        neglam = P0.sb("neglam", [128, 1], F32)
        ones_f = P0.sb("ones_f", [1, 128], F32)
        lam_sb = P0.sb("lam_sb", [1, 256], F32)
        lam_t = P0.sb("lam_t", [1, 8], F32)
        sch.op("dve", lambda e: e.memset(ones_f.t[:], 1.0), writes=[ones_f])
        sch.dma("sp", ch_c, lam_sb.t[:], lam4.rearrange("(o a) d -> o (a d)", o=1), writes=[lam_sb])
        sch.op("dve", lambda e: e.tensor_tensor(out=lam_sb.t[0:1, 0:64], in0=lam_sb.t[0:1, 0:64], in1=lam_sb.t[0:1, 64:128], op=ALU.mult),
               reads=[lam_sb], writes=[lam_sb])
        sch.op("dve", lambda e: e.tensor_tensor(out=lam_sb.t[0:1, 128:192], in0=lam_sb.t[0:1, 128:192], in1=lam_sb.t[0:1, 192:256], op=ALU.mult),
               reads=[lam_sb], writes=[lam_sb])
        sch.op("dve", lambda e: e.reduce_sum(out=lam_t.t[0:1, 0:1], in_=lam_sb.t[0:1, 0:64], axis=AX.X), reads=[lam_sb], writes=[lam_t])
        sch.op("dve", lambda e: e.reduce_sum(out=lam_t.t[0:1, 1:2], in_=lam_sb.t[0:1, 128:192], axis=AX.X), reads=[lam_sb], writes=[lam_t])
        sch.op("act", lambda e: e.activation(out=lam_t.t[0:1, 2:4], in_=lam_t.t[0:1, 0:2], func=AF.Exp), reads=[lam_t], writes=[lam_t])
        sch.op("dve", lambda e: e.tensor_tensor(out=lam_t.t[0:1, 4:5], in0=lam_t.t[0:1, 3:4], in1=lam_t.t[0:1, 2:3], op=ALU.subtract),
               reads=[lam_t], writes=[lam_t])
        sch.op("dve", lambda e: e.tensor_scalar(out=lam_t.t[0:1, 5:6], in0=lam_t.t[0:1, 4:5], scalar1=-lam_init, scalar2=None, op0=ALU.add),
               reads=[lam_t], writes=[lam_t])
        sch.op("pe", lambda e: e.matmul(PS[7].t[:, 0:1], ones_f.t[0:1, :], lam_t.t[0:1, 5:6], start=True, stop=True),
               reads=[ones_f, lam_t], writes=[PS[7]])
        sch.op("dve", lambda e: e.tensor_copy(out=neglam.t[:], in_=PS[7].t[:, 0:1]), reads=[PS[7]], writes=[neglam])

        OT_v = OT.rearrange("(c p) t -> c p t", p=128)
        SCALE_A = 128.0 ** -0.5
        SCALE_B = 64.0 ** -0.5

        def attention(kind):
            pes = contextlib.ExitStack()
            with pes:
                P = Pool_(nc, pes)
                psS = []
                for i in range(2):
                    t = pes.enter_context(nc.psum_tensor("psS%s%d" % (kind, i), [128, 1024], F32))
                    psS.append(Buf("psS%d" % i, t))
                psO = []
                for i in range(2):
                    t = pes.enter_context(nc.psum_tensor("psO%s%d" % (kind, i), [128, 512], F32))
                    psO.append(Buf("psO%d" % i, t))
                LCOL = LCOL_FLAG
                t = pes.enter_context(nc.psum_tensor("psL%s" % kind, [128, 512], F32))
                psL = Buf("psL", t)
                t = pes.enter_context(nc.psum_tensor("psRr%s" % kind, [128, 512], F32))
                psR = Buf("psR", t)
                psL2 = [psL, psR]
                sel64 = P.sb("sel64", [64, 128], F32)
                sch.op("pool", lambda e: e.memset(sel64.t[:], 1.0 / 32.0), writes=[sel64])
                lsbs = [P.sb("lsb%d" % i, [64, 512], F32) for i in range(2)]
                KTs = [P.sb("KT%d" % i, [128, S], BF16) for i in range(2)]
                Vs = [P.sb("V%d" % i, [128, 64, 128], BF16) for i in range(2)]
                QTs = [P.sb("QT%d" % i, [128, OWN], BF16) for i in range(2)]
                pTs = [P.sb("pT%d" % i, [128, 1024], BF16) for i in range(3)]
                rls = [P.sb("rl%d" % i, [128, 512], F32) for i in range(2)]
                d0s = [P.sb("d0%d" % i, [128, 512], F32) for i in range(2)]
                obs = [P.sb("ob%d" % i, [128, 512], BF16) for i in range(2)]
                ch_k = [sch.chan("k%d" % i) for i in range(2)]
                ch_v = [sch.chan("v%d" % i) for i in range(2)]
                ch_q = [sch.chan("q%d" % i) for i in range(2)]
                ch_o = [sch.chan("o%d" % i) for i in range(2)]
                NST = 32 if kind == "A" else 64
                LAG = 2

                def load_kv(g):
                    kb, vb = KTs[g % 2], Vs[g % 2]
                    if kind == "A":
                        src_k = KAT[g, :, :]
                        src_v = VA.rearrange("(kc p) c -> p kc c", p=128)[:, :, g * 128:(g + 1) * 128]
                    else:
                        src_k = KBT[g, :, :]
                        src_v = VB.rearrange("(kc p) c -> p kc c", p=128)[:, :, g * 128:(g + 1) * 128]
                    sch.dma("sp", ch_k[g % 2], kb.t[:], src_k, writes=[kb])
                    sch.dma("sp", ch_v[g % 2], vb.t[:], src_v, writes=[vb])

                def load_q(h):
                    src = QAT[h, :, :] if kind == "A" else QBT[h, :, :]
                    sch.dma("sp", ch_q[h % 2], QTs[h % 2].t[:], src, writes=[QTs[h % 2]])

                def kvidx(h):
                    return h // 4 if kind == "A" else h

                steps = [(h, qt, st) for h in range(8) for qt in range(8) for st in range(NST)]
                N = len(steps)
                load_kv(0)
                load_q(0)
                if kind == "B":
                    emit_conversions()
                epi_n = [0]

                def emit_S(i):
                    h, qt, st = steps[i]
                    g = kvidx(h)
                    kb, qb = KTs[g % 2], QTs[h % 2]
                    ps = psS[i % 2]
                    pT = pTs[i % 3]
                    for m in range(2):
                        if kind == "A":
                            kc = 2 * st + m
                            lhsT = kb.t[:, kc * 128:(kc + 1) * 128]
                            rhs = qb.t[:, qt * 512:(qt + 1) * 512]
                        else:
                            kc = st
                            lhsT = kb.t[m * 64:(m + 1) * 64, kc * 128:(kc + 1) * 128]
                            rhs = qb.t[m * 64:(m + 1) * 64, qt * 512:(qt + 1) * 512]
                        sch.op("pe", lambda e, ps=ps, lhsT=lhsT, rhs=rhs, m=m: e.matmul(ps.t[:, m * 512:(m + 1) * 512], lhsT, rhs, start=True, stop=True),
                               reads=[kb, qb], writes=[ps])
                    sc = SCALE_A if kind == "A" else SCALE_B
                    sch.op("act", lambda e, ps=ps, pT=pT, sc=sc: e.activation(out=pT.t[:], in_=ps.t[:], func=AF.Exp, scale=sc),
                           reads=[ps], writes=[pT])

                def emit_PV(i):
                    h, qt, st = steps[i]
                    g = kvidx(h)
                    if qt == 0 and st == 0 and h + 1 < 8:
                        if kvidx(h + 1) != g:
                            load_kv(kvidx(h + 1))
                        load_q(h + 1)
                    vb = Vs[g % 2]
                    pT = pTs[i % 3]
                    for m in range(2):
                        if kind == "A":
                            kc = 2 * st + m
                            po = psO[qt % 2]
                            first, last = (st == 0 and m == 0), (st == NST - 1 and m == 1)
                        else:
                            kc = st
                            po = psO[m]
                            first, last = (st == 0), (st == NST - 1)
                        sch.op("pe", lambda e, po=po, pT=pT, kc=kc, m=m, first=first, last=last: e.matmul(po.t[:], vb.t[:, kc, :], pT.t[:, m * 512:(m + 1) * 512],
                                                                                                          start=first, stop=last),
                               reads=[vb, pT], writes=[po])
                    for m in range(2):
                        if LCOL:
                            sch.op("pe", lambda e, pT=pT, m=m: e.matmul(psL.t[32 * m:32 * m + 32, :], ones_bf.t[:, 0:32], pT.t[:, m * 512:(m + 1) * 512],
                                                                        start=(st == 0), stop=(st == NST - 1), tile_position=(0, 32 * m)),
                                   reads=[ones_bf, pT], writes=[psL])
                        else:
                            sch.op("pe", lambda e, pT=pT, m=m: e.matmul(psL2[m].t[:], ones_bf.t[:], pT.t[:, m * 512:(m + 1) * 512],
                                                                        start=(st == 0), stop=(st == NST - 1)),
                                   reads=[ones_bf, pT], writes=[psL2[m]])
                    if st == NST - 1:
                        k2 = epi_n[0] % 2
                        epi_n[0] += 1
                        ob, rl, d0, lsb = obs[k2], rls[k2], d0s[k2], lsbs[k2]
                        if LCOL:
                            sch.op("dve", lambda e, lsb=lsb: e.tensor_copy(out=lsb.t[0:64, :], in_=psL.t[0:64, :]), reads=[psL], writes=[lsb])
                        if kind == "A":
                            po = psO[qt % 2]
                            if LCOL:
                                sch.op("pe", lambda e, lsb=lsb: e.matmul(psR.t[:], sel64.t[0:64, :], lsb.t[0:64, :], start=True, stop=True),
                                       reads=[sel64, lsb], writes=[psR])
                                sch.op("dve", lambda e, rl=rl: e.reciprocal(out=rl.t[:], in_=psR.t[:]), reads=[psR], writes=[rl])
                            else:
                                sch.op("dve", lambda e, d0=d0: e.tensor_copy(out=d0.t[:], in_=psL2[0].t[:]), reads=[psL2[0]], writes=[d0])
                                sch.op("dve", lambda e, d0=d0, rl=rl: e.tensor_tensor(out=rl.t[:], in0=psL2[1].t[:], in1=d0.t[:], op=ALU.add), reads=[psL2[1], d0], writes=[rl])
                                sch.op("dve", lambda e, rl=rl: e.reciprocal(out=rl.t[:], in_=rl.t[:]), reads=[rl], writes=[rl])
                            sch.op("dve", lambda e, ob=ob, po=po, rl=rl: e.tensor_tensor(out=ob.t[:], in0=po.t[:], in1=rl.t[:], op=ALU.mult),
                                   reads=[po, rl], writes=[ob])
                            row = h
                        else:
                            for m in range(2):
                                tgt = d0 if m == 0 else rl
                                if LCOL:
                                    sch.op("pe", lambda e, lsb=lsb, m=m: e.matmul(psR.t[:], sel64.t[32 * m:32 * m + 32, :], lsb.t[32 * m:32 * m + 32, :], start=True, stop=True),
                                           reads=[sel64, lsb], writes=[psR])
                                    sch.op("dve", lambda e, rl=rl: e.reciprocal(out=rl.t[:], in_=psR.t[:]), reads=[psR], writes=[rl])
                                else:
                                    sch.op("dve", lambda e, rl=rl, m=m: e.reciprocal(out=rl.t[:], in_=psL2[m].t[:]), reads=[psL2[m]], writes=[rl])
                                sch.op("dve", lambda e, tgt=tgt, rl=rl, m=m: e.tensor_tensor(out=tgt.t[:], in0=psO[m].t[:], in1=rl.t[:], op=ALU.mult),
                                       reads=[psO[m], rl], writes=[tgt])
                            sch.op("dve", lambda e, ob=ob, rl=rl, d0=d0: e.scalar_tensor_tensor(out=ob.t[:], in0=rl.t[:], scalar=neglam.t[:, 0:1],
                                                                                                  in1=d0.t[:], op0=ALU.mult, op1=ALU.add),
                                   reads=[rl, d0, neglam], writes=[ob])
                            row = 8 + h
                        sch.dma("sp", ch_o[k2], OT_v[row, :, qt * 512:(qt + 1) * 512], ob.t[:], reads=[ob])

                for i in range(N + LAG):
                    if i < N:
                        emit_S(i)
                    if i >= LAG:
                        emit_PV(i - LAG)
                sch.barrier()


        def emit_conversions():
            ch = sch.chan("conv", sw=True)
            wa_v = w_branch_a.rearrange("(c p) n -> p c n", p=128)
            wb_v = w_branch_b.rearrange("(c p) n -> p c n", p=128)
            wg_v = w_gate.rearrange("(c p) n -> p c n", p=128)
            wo_v = w_out.rearrange("(c p) n -> p c n", p=128)
            for grp in range(8):
                cs_ = slice(grp * 256, (grp + 1) * 256)
                cs2 = slice(D + grp * 256, D + (grp + 1) * 256)
                for (off, nk, src) in ((0, 8, wa_v[:, :, cs_]), (2048, 8, wb_v[:, :, cs_]), (4096, 16, wg_v[:, :, cs_]), (8192, 16, wg_v[:, :, cs2])):
                    sch.dma("pool", ch, WD1[grp, :, off:off + nk * 256].rearrange("p (k n) -> p k n", n=256), src)
            for n in range(4):
                for c4 in range(4):
                    sch.dma("pool", ch, WO16[n, :, c4 * 2048:(c4 + 1) * 2048].rearrange("p (k n) -> p k n", n=512),
                            wo_v[:, 4 * c4:4 * c4 + 4, n * 512:(n + 1) * 512])
            for e_ in range(NEXP):
                rows = slice(e_ * 128, (e_ + 1) * 128)
                for (dstw, srcw, cc) in ((WG16, w_e_gate, 16), (WU16, w_e_up, 16), (WD16, w_e_down, 4)):
                    srcv = srcw[e_].rearrange("(p c) n -> p (c n)", c=cc)
                    for a_ in range(4):
                        sch.dma("pool", ch, dstw[rows, a_ * 2048:(a_ + 1) * 2048], srcv[:, a_ * 2048:(a_ + 1) * 2048])

        sch.barrier()
        ps_es.close()
        attention("A")
        if stop_after == "BA":
            return nc
        attention("B")
        if stop_after == "BB":
            return nc
        ps_es = contextlib.ExitStack()
        es.enter_context(ps_es)
        del PS[:]
        for i in range(8):
            t = ps_es.enter_context(nc.psum_tensor("psd%d" % i, [128, 512], F32))
            PS.append(Buf("psd%d" % i, t))

        sg_sb = P0.sb("sg_sb", [128, 1], F32)
        bg_sb = P0.sb("bg_sb", [128, 32], F32)
        sch.dma("sp", ch_c, sg_sb.t[:], subln_b.rearrange("(p o) -> p o", o=1), writes=[sg_sb])
        sch.dma("sp", ch_c, bg_sb.t[:], b_gate.rearrange("(c p) -> p c", p=128), writes=[bg_sb], allow_slow_non_contiguous=True)
        sch.op("dve", lambda e: e.tensor_scalar(out=sg_sb.t[:], in0=sg_sb.t[:], scalar1=(1.0 - lam_init), scalar2=0.0,
                                                op0=ALU.mult, op1=ALU.add), reads=[sg_sb], writes=[sg_sb])

        def phaseD1():
            pes = contextlib.ExitStack()
            with pes:
                P = Pool_(nc, pes)
                TT = 512
                NT = OWN // TT
                hts = [P.sb("d1ht%d" % i, [128, 16, TT], BF16) for i in range(2)]
                ots = [P.sb("d1ot%d" % i, [128, 16, TT], BF16) for i in range(2)]
                mts = [P.sb("d1mt%d" % i, [128, 16, TT], BF16) for i in range(2)]
                wgrp = []
                for i in range(2):
                    wgrp.append(dict(wa=P.sb("wa%d" % i, [128, 8, 256], BF16), wb=P.sb("wb%d" % i, [128, 8, 256], BF16),
                                     wga=P.sb("wga%d" % i, [128, 16, 256], BF16), wgb=P.sb("wgb%d" % i, [128, 16, 256], BF16)))
                ch_wg = [sch.chan("d1w%d" % i) for i in range(2)]
                ch_in = [sch.chan("d1in%d" % i) for i in range(2)]
                ch_mt = [sch.chan("d1mt%d" % i) for i in range(2)]
                sqs = [P.sb("d1sq%d" % i, [128, 512], BF16) for i in range(2)]
                rs = [P.sb("d1rs%d" % i, [128, 512], F32) for i in range(2)]
                sga = [P.sb("d1sga%d" % i, [128, 512], F32) for i in range(2)]
                sgb = [P.sb("d1sgb%d" % i, [128, 512], F32) for i in range(2)]
                m1s = [P.sb("d1m1%d" % i, [128, 512], F32) for i in range(2)]
                m2s = [P.sb("d1m2%d" % i, [128, 512], F32) for i in range(2)]
                HT_v = HT.rearrange("(c p) t -> p c t", p=128)
                OTv = OT.rearrange("(c p) t -> p c t", p=128)
                MT_v = MT.rearrange("(c p) t -> p c t", p=128)
                st_ = {"g": 0, "ep": 0, "sl": 0}

                def prep(tt):
                    tk = slice(tt * TT, (tt + 1) * TT)
                    ht, ot, ch = hts[tt % 2], ots[tt % 2], ch_in[tt % 2]
                    for c4 in range(4):
                        sch.dma("sp", ch, ot.t[:, 4 * c4:4 * c4 + 4, :], OTv[:, 4 * c4:4 * c4 + 4, tk], writes=[ot.reg(c4)])
                    for c4 in range(4):
                        sch.dma("sp", ch, ht.t[:, 4 * c4:4 * c4 + 4, :], HT_v[:, 4 * c4:4 * c4 + 4, tk], writes=[ht.reg(c4)])
                    sch.fence(ch, ht)
                    sch.fence(ch, ot)
                    for c in range(8, 16):
                        i2 = st_["sl"] % 2
                        st_["sl"] += 1
                        sq, r_ = sqs[i2], rs[i2]
                        pss = PS[i2]
                        sch.op("act", lambda e, sq=sq, c=c: e.activation(out=sq.t[:], in_=ot.t[:, c, :], func=AF.Square),
                               reads=[ot.reg(c // 4)], writes=[sq])
                        sch.op("pe", lambda e, pss=pss, sq=sq: e.matmul(pss.t[:], ones_bf.t[:], sq.t[:], start=True, stop=True),
                               reads=[ones_bf, sq], writes=[pss])
                        sch.op("act", lambda e, r_=r_, pss=pss: e.activation(out=r_.t[:], in_=pss.t[:], func=AF.Ln, scale=1.0 / 128, bias=eps_sb.t[:, 0:1]),
                               reads=[pss, eps_sb], writes=[r_])
                        sch.op("act", lambda e, r_=r_: e.activation(out=r_.t[:], in_=r_.t[:], func=AF.Exp, scale=-0.5), reads=[r_], writes=[r_])
                        sch.op("dve", lambda e, r_=r_, c=c: e.scalar_tensor_tensor(out=ot.t[:, c, :], in0=ot.t[:, c, :], scalar=sg_sb.t[:, 0:1],
                                                                                     in1=r_.t[:], op0=ALU.mult, op1=ALU.mult),
                               reads=[ot.reg(c // 4), sg_sb, r_], writes=[ot.reg(c // 4)])

                def main(tt):
                    tk = slice(tt * TT, (tt + 1) * TT)
                    ht, ot, mt = hts[tt % 2], ots[tt % 2], mts[tt % 2]
                    for grp in range(8):
                        wg = wgrp[st_["g"] % 2]
                        chw = ch_wg[st_["g"] % 2]
                        st_["g"] += 1
                        sch.dma("sp", chw, wg["wa"].t[:], WD1[grp, :, 0:2048].rearrange("p (k n) -> p k n", n=256), writes=[wg["wa"]])
                        sch.dma("sp", chw, wg["wb"].t[:], WD1[grp, :, 2048:4096].rearrange("p (k n) -> p k n", n=256), writes=[wg["wb"]])
                        sch.dma("sp", chw, wg["wga"].t[:], WD1[grp, :, 4096:8192].rearrange("p (k n) -> p k n", n=256), writes=[wg["wga"]])
                        sch.dma("sp", chw, wg["wgb"].t[:], WD1[grp, :, 8192:12288].rearrange("p (k n) -> p k n", n=256), writes=[wg["wgb"]])
                        for nm in ("wa", "wb", "wga", "wgb"):
                            wg[nm].w = {chw: sch.cnt[chw]}
                        for jj in range(2):
                            j = grp * 2 + jj
                            wsl = slice(jj * 128, (jj + 1) * 128)
                            i2 = st_["ep"] % 2
                            st_["ep"] += 1
                            pa, pb, ga, gb = PS[4 * i2], PS[1 + 4 * i2], PS[2 + 4 * i2], PS[3 + 4 * i2]
                            for k in range(16):
                                sch.op("pe", lambda e, k=k, ga=ga, wsl=wsl: e.matmul(ga.t[:], wg["wga"].t[:, k, wsl], ht.t[:, k, :], start=(k == 0), stop=(k == 15)),
                                       reads=[wg["wga"], ht.reg(k // 4)], writes=[ga])
                            for k in range(16):
                                sch.op("pe", lambda e, k=k, gb=gb, wsl=wsl: e.matmul(gb.t[:], wg["wgb"].t[:, k, wsl], ht.t[:, k, :], start=(k == 0), stop=(k == 15)),
                                       reads=[wg["wgb"], ht.reg(k // 4)], writes=[gb])
                            for k in range(8):
                                sch.op("pe", lambda e, k=k, pa=pa, wsl=wsl: e.matmul(pa.t[:], wg["wa"].t[:, k, wsl], ot.t[:, k, :], start=(k == 0), stop=(k == 7)),
                                       reads=[wg["wa"], ot.reg(k // 4)], writes=[pa])
                            for k in range(8):
                                sch.op("pe", lambda e, k=k, pb=pb, wsl=wsl: e.matmul(pb.t[:], wg["wb"].t[:, k, wsl], ot.t[:, 8 + k, :], start=(k == 0), stop=(k == 7)),
                                       reads=[wg["wb"], ot.reg(2 + k // 4)], writes=[pb])
                            sa, sb_, m1, m2 = sga[i2], sgb[i2], m1s[i2], m2s[i2]
                            sch.op("act", lambda e, sa=sa, ga=ga, j=j: e.activation(out=sa.t[:], in_=ga.t[:], func=AF.Sigmoid, bias=bg_sb.t[:, j:j + 1]),
                                   reads=[ga, bg_sb], writes=[sa])
                            sch.op("act", lambda e, sb_=sb_, gb=gb, j=j: e.activation(out=sb_.t[:], in_=gb.t[:], func=AF.Sigmoid, bias=bg_sb.t[:, 16 + j:17 + j]),
                                   reads=[gb, bg_sb], writes=[sb_])
                            sch.op("dve", lambda e, m1=m1, sa=sa, pa=pa: e.tensor_tensor(out=m1.t[:], in0=pa.t[:], in1=sa.t[:], op=ALU.mult),
                                   reads=[pa, sa], writes=[m1])
                            sch.op("dve", lambda e, m2=m2, sb_=sb_, pb=pb: e.tensor_tensor(out=m2.t[:], in0=pb.t[:], in1=sb_.t[:], op=ALU.mult),
                                   reads=[pb, sb_], writes=[m2])
                            sch.op("pool", lambda e, m1=m1, m2=m2, j=j: e.tensor_tensor(out=mt.t[:, j, :], in0=m1.t[:], in1=m2.t[:], op=ALU.add),
                                   reads=[m1, m2], writes=[mt.reg(j // 4)])
                        if grp == 1 and tt + 1 < NT:
                            prep(tt + 1)
                    for c4 in range(4):
                        sch.dma("sp", ch_mt[tt % 2], MT_v[:, 4 * c4:4 * c4 + 4, tk], mt.t[:, 4 * c4:4 * c4 + 4, :], reads=[mt.reg(c4)])
                    sch.fence(ch_mt[tt % 2], mt, readers=True)

                prep(0)
                for tt in range(NT):
                    main(tt)
                sch.barrier()

        phaseD1()
        if stop_after == "D1":
            return nc

        def phaseD2():
            pes = contextlib.ExitStack()
            with pes:
                P = Pool_(nc, pes)
                mt = P.sb("d2mt", [128, 16, OWN], BF16)
                wos = [P.sb("d2wo%d" % i, [128, 16, 512], BF16) for i in range(2)]
                xr = [P.sb("d2xr%d" % i, [128, 512], F32) for i in range(3)]
                ch_m = sch.chan("d2m")
                ch_w = [sch.chan("d2w%d" % i) for i in range(2)]
                ch_x = [sch.chan("d2x%d" % i) for i in range(3)]
                ch_s = [sch.chan("d2s%d" % i) for i in range(3)]
                MT_v = MT.rearrange("(c p) t -> p c t", p=128)
                wo_v = w_out.rearrange("(c p) n -> p c n", p=128)
                for c in range(16):
                    sch.dma("sp", ch_m, mt.t[:, c, :], MT_v[:, c, :], writes=[mt.reg(c)])
                sch.fence(ch_m, mt)
                it = 0
                for n in range(4):
                    wo = wos[n % 2]
                    ns = slice(n * 512, (n + 1) * 512)
                    sch.dma("sp", ch_w[n % 2], wo.t[:], WO16[n].rearrange("p (k n) -> p k n", n=512), writes=[wo])
                    for ts in range(32):
                        rows = slice(ts * 128, (ts + 1) * 128)
                        ps = PS[it % 4]
                        x_ = xr[it % 3]
                        chx, chs = ch_x[it % 3], ch_s[it % 3]
                        it += 1
                        sch.dma("sp", chx, x_.t[:], x_own[rows, ns], writes=[x_])
                        for k in range(16):
                            sch.op("pe", lambda e, k=k, ps=ps, rows=rows, wo=wo: e.matmul(ps.t[:], mt.t[:, k, rows], wo.t[:, k, :], start=(k == 0), stop=(k == 15)),
                                   reads=[mt, wo], writes=[ps])
                        sch.op("dve", lambda e, x_=x_, ps=ps: e.tensor_tensor(out=x_.t[:], in0=ps.t[:], in1=x_.t[:], op=ALU.add),
                               reads=[ps, x_], writes=[x_])
                        sch.dma("sp", chs, X1[rows, ns], x_.t[:], reads=[x_])
                sch.barrier()

        phaseD2()
        if stop_after == "D2":
            return nc

        ps_es.close()
        ps_es2 = contextlib.ExitStack()
        es.enter_context(ps_es2)
        PF = []
        for i in range(6):
            t = ps_es2.enter_context(nc.psum_tensor("pf%d" % i, [128, 512], F32))
            PF.append(Buf("pf%d" % i, t))
        PB = []
        for i in range(2):
            t = ps_es2.enter_context(nc.psum_tensor("pb%d" % i, [128, 1024], BF16))
            PB.append(Buf("pb%d" % i, t))

        bc_pos = nc.gpsimd.to_reg(NPOS - 1)
        bc_w = nc.gpsimd.to_reg(NEXP * 128 - 1)
        posi = P0.sb("posi", [128, 64], I32)
        wts = P0.sb("wts", [128, 64], F32)
        widx = P0.sb("widx", [128, 128], I32)
        XB_ROWS = XBUF

        def phaseE():
            pes = contextlib.ExitStack()
            with pes:
                P = Pool_(nc, pes)
                H2 = P.sb("H2", [128, 32, D], BF16)
                gffn_b = P.sb("gffn_b", [128, D], F32)
                wr_sb = P.sb("wr_sb", [128, 16, 36], F32)
                ustr = P.sb("ustr", [128, 128], F32)
                onesF = P.sb("onesF", [128, 128], F32)
                thr_sb = P.sb("thr_sb", [128, 64], F32)
                cst_sb = P.sb("cst_sb", [128, 64], F32)
                x1b = P.sb("x1b", [128, D], F32)
                h2f = P.sb("h2f", [128, D], F32)
                h2T = P.sb("h2T", [128, D], F32)
                M1all = P.sb("M1all", [128, 32, 32], F32)
                M2all = P.sb("M2all", [128, 32, 32], F32)
                rank_all = P.sb("rank_all", [128, 32, 32], F32)
                Msum = P.sb("Msum", [128, 32], F32)
                Mt = P.sb("Mt", [128, 32], F32)
                lgt = P.sb("lgt", [128, 36], F32)
                sm = P.sb("sm", [128, 64], F32)
                esel = P.sb("esel", [128, 8], F32)
                esel2 = P.sb("esel2", [128, 8], F32)
                oh1 = P.sb("oh1", [128, 8], F32)
                oh2 = P.sb("oh2", [128, 8], F32)
                goh = P.sb("goh", [128, 4], F32)
                junk4 = P.sb("junk4", [128, 4], F32)
                posf = P.sb("posf", [128, 64], F32)
                tmp32 = P.sb("tmp32", [128, 32], F32)
                tmp32b = P.sb("tmp32b", [128, 32], F32)
                offs_sb = P.sb("offs_sb", [128, 32], F32)
                pend_sb = P.sb("pend_sb", [128, 32], F32)
                padrep = P.sb("padrep", [32, 128], F32)
                cmp64 = P.sb("cmp64", [32, 64], F32)
                ch_e = sch.chan("e_c")
                ch_x1 = sch.chan("e_x1")
                ch_sc = sch.chan("e_sc", sw=True)
                sch.dma("sp", ch_e, gffn_b.t[:], g_ffn.rearrange("(o n) -> o n", o=1).partition_broadcast(128), writes=[gffn_b])
                sch.dma("sp", ch_e, wr_sb.t[:], w_router.rearrange("(c p) n -> p c n", p=128), writes=[wr_sb])
                sch.dma("sp", ch_e, ustr.t[:], ustrict[:, :], writes=[ustr])
                sch.dma("sp", ch_e, thr_sb.t[:], thr[:, :], writes=[thr_sb])
                sch.dma("sp", ch_e, cst_sb.t[:], consts[:, :], writes=[cst_sb])
                for b_ in (gffn_b, wr_sb, ustr, thr_sb, cst_sb):
                    b_.w = {ch_e: sch.cnt[ch_e]}
                sch.op("dve", lambda e: e.memset(onesF.t[:], 1.0), writes=[onesF])
                sch.op("dve", lambda e: e.memset(Msum.t[:], 0.0), writes=[Msum])

                def dv(fn, reads, writes, eng="dve"):
                    sch.op(eng, fn, reads=reads, writes=writes)

                def col(i):
                    return sm.t[:, i:i + 1]

                for t in range(32):
                    rows = slice(t * 128, (t + 1) * 128)
                    sch.dma("sp", ch_x1, x1b.t[:], X1[rows, :], writes=[x1b])
                    dv(lambda e: e.memset(sm.t[:, 0:16], 0.0), [], [sm])
                    dv(lambda e: e.activation(out=h2f.t[:], in_=x1b.t[:], func=AF.Square, accum_out=col(0)), [x1b, sm], [h2f, sm], "act")
                    dv(lambda e: e.activation(out=col(1), in_=col(0), func=AF.Ln, scale=1.0 / D, bias=eps_sb.t[:, 0:1]), [sm, eps_sb], [sm], "act")
                    dv(lambda e: e.activation(out=col(2), in_=col(1), func=AF.Exp, scale=-0.5), [sm], [sm], "act")
                    dv(lambda e: e.scalar_tensor_tensor(out=h2f.t[:], in0=x1b.t[:], scalar=col(2), in1=gffn_b.t[:], op0=ALU.mult, op1=ALU.mult),
                       [x1b, sm, gffn_b], [h2f])
                    dv(lambda e, t=t: e.tensor_copy(out=H2.t[:, t, :], in_=h2f.t[:]), [h2f], [H2.reg(t)], "pool")
                    for g4 in range(4):
                        pst = PF[g4 % 2]
                        for j in range(4):
                            c = g4 * 4 + j
                            dv(lambda e, pst=pst, j=j, c=c: e.transpose(out=pst.t[:, j * 128:(j + 1) * 128], in_=h2f.t[:, c * 128:(c + 1) * 128], identity=ident_f.t[:]),
                               [h2f, ident_f], [pst], "pe")
                        dv(lambda e, pst=pst, g4=g4: e.activation(out=h2T.t[:, g4 * 512:(g4 + 1) * 512], in_=pst.t[:], func=AF.Copy), [pst], [h2T.reg(g4)], "act")
                    psl = PF[2]
                    for c in range(16):
                        dv(lambda e, c=c: e.matmul(psl.t[:, 0:36], h2T.t[:, c * 128:(c + 1) * 128], wr_sb.t[:, c, :], start=(c == 0), stop=(c == 15)),
                           [h2T.reg(c // 4), wr_sb], [psl], "pe")
                    dv(lambda e: e.tensor_copy(out=lgt.t[:], in_=psl.t[:, 0:36]), [psl], [lgt])
                    dv(lambda e: e.reduce_max(out=col(3), in_=lgt.t[:, 0:4], axis=AX.X), [lgt], [sm])
                    dv(lambda e: e.tensor_scalar(out=col(4), in0=col(3), scalar1=-1.0, scalar2=0.0, op0=ALU.mult, op1=ALU.add), [sm], [sm])
                    dv(lambda e: e.tensor_scalar(out=goh.t[:], in0=lgt.t[:, 0:4], scalar1=col(3), scalar2=1.0, op0=ALU.is_equal, op1=ALU.mult), [lgt, sm], [goh])
                    dv(lambda e: e.activation(out=junk4.t[:], in_=lgt.t[:, 0:4], func=AF.Exp, bias=col(4), accum_out=col(5)), [lgt, sm], [junk4, sm], "act")
                    dv(lambda e: e.reciprocal(out=col(6), in_=col(5)), [sm], [sm])
                    dv(lambda e: e.tensor_scalar(out=esel.t[:], in0=lgt.t[:, 4:12], scalar1=goh.t[:, 0:1], scalar2=0.0, op0=ALU.mult, op1=ALU.add), [lgt, goh], [esel])
                    for g_ in range(1, 4):
                        dv(lambda e, g_=g_: e.scalar_tensor_tensor(out=esel.t[:], in0=lgt.t[:, 4 + 8 * g_:12 + 8 * g_], scalar=goh.t[:, g_:g_ + 1], in1=esel.t[:],
                                                                   op0=ALU.mult, op1=ALU.add), [lgt, goh, esel], [esel])
                    dv(lambda e: e.reduce_max(out=col(7), in_=esel.t[:], axis=AX.X), [esel], [sm])
                    dv(lambda e: e.tensor_scalar(out=oh1.t[:], in0=esel.t[:], scalar1=col(7), scalar2=1.0, op0=ALU.is_equal, op1=ALU.mult), [esel, sm], [oh1])
                    dv(lambda e: e.scalar_tensor_tensor(out=esel2.t[:], in0=oh1.t[:], scalar=-1.0e30, in1=esel.t[:], op0=ALU.mult, op1=ALU.add), [oh1, esel], [esel2])
                    dv(lambda e: e.reduce_max(out=col(8), in_=esel2.t[:], axis=AX.X), [esel2], [sm])
                    dv(lambda e: e.tensor_scalar(out=oh2.t[:], in0=esel2.t[:], scalar1=col(8), scalar2=1.0, op0=ALU.is_equal, op1=ALU.mult), [esel2, sm], [oh2])
                    dv(lambda e: e.tensor_tensor(out=col(9), in0=col(8), in1=col(7), op=ALU.subtract), [sm], [sm])
                    dv(lambda e: e.activation(out=col(10), in_=col(9), func=AF.Exp), [sm], [sm], "act")
                    dv(lambda e: e.tensor_scalar(out=col(11), in0=col(10), scalar1=1.0, scalar2=0.0, op0=ALU.add, op1=ALU.add), [sm], [sm])
                    dv(lambda e: e.reciprocal(out=col(12), in_=col(11)), [sm], [sm])
                    dv(lambda e, t=t: e.tensor_tensor(out=wts.t[:, t:t + 1], in0=col(12), in1=col(6), op=ALU.mult), [sm], [wts])
                    dv(lambda e, t=t: e.tensor_tensor(out=wts.t[:, 32 + t:33 + t], in0=col(6), in1=wts.t[:, t:t + 1], op=ALU.subtract), [sm, wts], [wts])
                    for g_ in range(4):
                        dv(lambda e, g_=g_, t=t: e.tensor_scalar(out=M1all.t[:, t, 8 * g_:8 * g_ + 8], in0=oh1.t[:], scalar1=goh.t[:, g_:g_ + 1], scalar2=0.0,
                                                                 op0=ALU.mult, op1=ALU.add), [oh1, goh], [M1all.reg(t)])
                        dv(lambda e, g_=g_, t=t: e.tensor_scalar(out=M2all.t[:, t, 8 * g_:8 * g_ + 8], in0=oh2.t[:], scalar1=goh.t[:, g_:g_ + 1], scalar2=0.0,
                                                                 op0=ALU.mult, op1=ALU.add), [oh2, goh], [M2all.reg(t)])
                    dv(lambda e, t=t: e.tensor_tensor(out=Mt.t[:], in0=M1all.t[:, t, :], in1=M2all.t[:, t, :], op=ALU.add), [M1all.reg(t), M2all.reg(t)], [Mt])
                    psr = PF[3]
                    dv(lambda e: e.matmul(psr.t[:, 0:32], ustr.t[:], Mt.t[:], start=True, stop=False), [ustr, Mt], [psr], "pe")
                    dv(lambda e: e.matmul(psr.t[:, 0:32], onesF.t[:], Msum.t[:], start=False, stop=True), [onesF, Msum], [psr], "pe")
                    dv(lambda e, t=t: e.tensor_copy(out=rank_all.t[:, t, :], in_=psr.t[:, 0:32]), [psr], [rank_all.reg(t)])
                    dv(lambda e: e.tensor_tensor(out=Msum.t[:], in0=Msum.t[:], in1=Mt.t[:], op=ALU.add), [Msum, Mt], [Msum])

                psc = PF[4]
                dv(lambda e: e.matmul(psc.t[0:32, 0:1], Msum.t[:], onesF.t[:, 0:1], start=True, stop=True), [Msum, onesF], [psc], "pe")
                dv(lambda e: e.tensor_copy(out=sm.t[0:32, 20:21], in_=psc.t[0:32, 0:1]), [psc], [sm])
                dv(lambda e: e.tensor_scalar(out=cmp64.t[:], in0=thr_sb.t[0:32, :], scalar1=sm.t[0:32, 20:21], scalar2=1.0, op0=ALU.is_lt, op1=ALU.mult), [thr_sb, sm], [cmp64])
                dv(lambda e: e.reduce_sum(out=sm.t[0:32, 21:22], in_=cmp64.t[:], axis=AX.X), [cmp64], [sm])
                dv(lambda e: e.tensor_scalar(out=sm.t[0:32, 22:23], in0=sm.t[0:32, 21:22], scalar1=128.0, scalar2=0.0, op0=ALU.mult, op1=ALU.add), [sm], [sm])
                dv(lambda e: e.tensor_scalar(out=padrep.t[:], in0=onesF.t[0:32, :], scalar1=sm.t[0:32, 22:23], scalar2=0.0, op0=ALU.mult, op1=ALU.add), [onesF, sm], [padrep])
                pso, pso2 = PF[5], PF[4]
                dv(lambda e: e.matmul(pso.t[:, 0:32], padrep.t[:], ustr.t[0:32, 0:32], start=True, stop=True), [padrep, ustr], [pso], "pe")
                dv(lambda e: e.matmul(pso2.t[:, 0:32], padrep.t[:], ident_f.t[0:32, 0:32], start=True, stop=True), [padrep, ident_f], [pso2], "pe")
                dv(lambda e: e.tensor_copy(out=offs_sb.t[:], in_=pso.t[:, 0:32]), [pso], [offs_sb])
                dv(lambda e: e.tensor_tensor(out=pend_sb.t[:], in0=pso2.t[:, 0:32], in1=offs_sb.t[:], op=ALU.add), [pso2, offs_sb], [pend_sb])
                for t in range(32):
                    dv(lambda e, t=t: e.tensor_tensor(out=tmp32.t[:], in0=rank_all.t[:, t, :], in1=offs_sb.t[:], op=ALU.add), [rank_all.reg(t), offs_sb], [tmp32])
                    dv(lambda e, t=t: e.tensor_tensor(out=tmp32b.t[:], in0=tmp32.t[:], in1=M1all.t[:, t, :], op=ALU.mult), [tmp32, M1all.reg(t)], [tmp32b])
                    dv(lambda e, t=t: e.reduce_sum(out=posf.t[:, t:t + 1], in_=tmp32b.t[:], axis=AX.X), [tmp32b], [posf])
                    dv(lambda e, t=t: e.tensor_tensor(out=tmp32b.t[:], in0=tmp32.t[:], in1=M2all.t[:, t, :], op=ALU.mult), [tmp32, M2all.reg(t)], [tmp32b])
                    dv(lambda e, t=t: e.reduce_sum(out=posf.t[:, 32 + t:33 + t], in_=tmp32b.t[:], axis=AX.X), [tmp32b], [posf])
                dv(lambda e: e.tensor_copy(out=posi.t[:], in_=posf.t[:]), [posf], [posi])
                dv(lambda e: e.tensor_scalar(out=tmp32.t[:], in0=pend_sb.t[:], scalar1=cst_sb.t[:, 0:1], scalar2=1.0, op0=ALU.is_le, op1=ALU.mult), [pend_sb, cst_sb], [tmp32])
                dv(lambda e: e.reduce_sum(out=sm.t[:, 24:25], in_=tmp32.t[:], axis=AX.X), [tmp32], [sm])
                dv(lambda e: e.tensor_scalar(out=sm.t[:, 25:26], in0=sm.t[:, 24:25], scalar1=float(NEXP - 1), scalar2=0.0, op0=ALU.min, op1=ALU.add), [sm], [sm])
                beb = P.sb("beb", [128, 128], F32)
                widf = P.sb("widf", [128, 128], F32)
                dv(lambda e: e.tensor_scalar(out=beb.t[:], in0=onesF.t[:], scalar1=sm.t[:, 25:26], scalar2=0.0, op0=ALU.mult, op1=ALU.add), [onesF, sm], [beb])
                psb = PF[5]
                dv(lambda e: e.matmul(psb.t[:, 0:128], beb.t[:], ident_f.t[:], start=True, stop=True), [beb, ident_f], [psb], "pe")
                dv(lambda e: e.tensor_scalar(out=widf.t[:], in0=psb.t[:, 0:128], scalar1=128.0, scalar2=cst_sb.t[:, 1:2], op0=ALU.mult, op1=ALU.add),
                   [psb, cst_sb], [widf])
                berep = P.sb("berep", [128, 128], F32)
                eqs = P.sb("eqs", [128, 128], F32)
                dv(lambda e: e.tensor_copy(out=berep.t[:], in_=psb.t[:, 0:128]), [psb], [berep])
                dv(lambda e: e.tensor_tensor(out=eqs.t[:, 1:128], in0=berep.t[:, 1:128], in1=berep.t[:, 0:127], op=ALU.is_equal), [berep], [eqs])
                dv(lambda e: e.memset(eqs.t[:, 48:49], 0.0), [], [eqs])
                dv(lambda e: e.scalar_tensor_tensor(out=widf.t[:, 1:128], in0=eqs.t[:, 1:128], scalar=1.0e6, in1=widf.t[:, 1:128], op0=ALU.mult, op1=ALU.add),
                   [eqs, widf], [widf])
                dv(lambda e: e.tensor_copy(out=widx.t[:], in_=widf.t[:]), [widf], [widx])
                for t in range(32):
                    for k in range(2):
                        ci = k * 32 + t
                        sch.custom_dma("pool", ch_sc, lambda e, t=t, ci=ci: e.indirect_dma_start(
                            out=XBUF[:, :], out_offset=bass.IndirectOffsetOnAxis(ap=posi.t[:, ci:ci + 1], axis=0),
                            in_=H2.t[:, t, :], in_offset=None, bounds_check=bc_pos, oob_is_err=False), reads=[H2.reg(t), posi])
                sch.barrier()

        phaseE()
        if debug and "posi" in debug:
            d_posi = nc.dram_tensor("posi", [128, 64], I32, kind="ExternalOutput").ap()
            d_widx = nc.dram_tensor("widx", [128, 128], I32, kind="ExternalOutput").ap()
            d_wts = nc.dram_tensor("wts", [128, 64], F32, kind="ExternalOutput").ap()
            ch_d = sch.chan("dbg")
            sch.dma("sp", ch_d, d_posi[:, :], posi.t[:], reads=[posi])
            sch.dma("sp", ch_d, d_widx[:, :], widx.t[:], reads=[widx])
            sch.dma("sp", ch_d, d_wts[:, :], wts.t[:], reads=[wts])
            sch.barrier()
        if stop_after == "E":
            return nc

        def phaseF():
            pes = contextlib.ExitStack()
            with pes:
                P = Pool_(nc, pes)
                xbs = [P.sb("xb%d" % i, [128, D], BF16) for i in range(2)]
                xbT = [P.sb("xbT%d" % i, [128, D], BF16) for i in range(2)]
                w1s = [P.sb("w1_%d" % i, [128, 16, FF], BF16) for i in range(2)]
                w3s = [P.sb("w3_%d" % i, [128, 16, FF], BF16) for i in range(2)]
                w2s = [P.sb("w2_%d" % i, [128, 4, D], BF16) for i in range(2)]
                s1s = [P.sb("s1_%d" % i, [128, FF], F32) for i in range(2)]
                ubs = [P.sb("ub%d" % i, [128, FF], BF16) for i in range(2)]
                uTs = [P.sb("uT%d" % i, [128, FF], BF16) for i in range(2)]
                ybs = [P.sb("yb%d" % i, [128, D], F32) for i in range(2)]
                ch_xb = [sch.chan("f_xb%d" % i) for i in range(2)]
                ch_w = [sch.chan("f_w%d" % i, sw=True) for i in range(2)]
                ch_y = [sch.chan("f_y%d" % i) for i in range(2)]

                def dv(fn, reads, writes, eng="dve"):
                    sch.op(eng, fn, reads=reads, writes=writes)

                wg_rows = w_e_gate.rearrange("e (p c) n -> (e p) (c n)", c=16).rearrange("r (a m) -> (r a) m", m=2048)
                wu_rows = w_e_up.rearrange("e (p c) n -> (e p) (c n)", c=16).rearrange("r (a m) -> (r a) m", m=2048)
                wd_rows = w_e_down.rearrange("e (p c) n -> (e p) (c n)", c=4).rearrange("r (a m) -> (r a) m", m=2048)

                NSTR = 2
                PER = NBLK // NSTR

                def blk(it):
                    return it % NSTR, (it % NSTR) * PER + it // NSTR

                def load_x(it):
                    _, b = blk(it)
                    sch.dma("sp", ch_xb[it % 2], xbs[it % 2].t[:], XBUF[b * 128:(b + 1) * 128, :], writes=[xbs[it % 2]])

                def load_w(it):
                    sidx, b = blk(it)
                    w1, w3, w2 = w1s[sidx], w3s[sidx], w2s[sidx]
                    for (dst, srcv) in ((w1, WG16), (w3, WU16), (w2, WD16)):
                        sch.custom_dma("pool", ch_w[sidx], lambda e, dst=dst, srcv=srcv, b=b: e.indirect_dma_start(
                            out=dst.t[:].rearrange("p c n -> p (c n)"), out_offset=None, in_=srcv[:, :],
                            in_offset=bass.IndirectOffsetOnAxis(ap=widx.t[:, b:b + 1], axis=0),
                            bounds_check=bc_w, oob_is_err=False), reads=[widx], writes=[dst])
                    for bb in (w1, w3, w2):
                        sch.fence(ch_w[sidx], bb)

                def transpose_x(it):
                    xb, xT_ = xbs[it % 2], xbT[it % 2]
                    for half in range(2):
                        pb = PB[half]
                        for j in range(8):
                            c = half * 8 + j
                            dv(lambda e, pb=pb, j=j, c=c: e.transpose(out=pb.t[:, j * 128:(j + 1) * 128], in_=xb.t[:].rearrange("q (p c) -> q c p", c=16)[:, c, :], identity=ident_bf.t[:]),
                               [xb, ident_bf], [pb], "pe")
                        if half == 0:
                            dv(lambda e, pb=pb: e.activation(out=xT_.t[:, 0:1024], in_=pb.t[:], func=AF.Copy), [pb], [xT_.reg(0)], "act")
                        else:
                            dv(lambda e, pb=pb: e.tensor_copy(out=xT_.t[:, 1024:2048], in_=pb.t[:]), [pb], [xT_.reg(1)])

                load_x(0)
                for it0 in range(NSTR):
                    load_w(it0)
                transpose_x(0)
                for it in range(NBLK):
                    i = it % 2
                    sidx, b = blk(it)
                    if it + 1 < NBLK:
                        load_x(it + 1)
                    xb, xT_, w1, w3, w2 = xbs[i], xbT[i], w1s[sidx], w3s[sidx], w2s[sidx]
                    ps1, ps3 = PF[0], PF[1]
                    for c in range(16):
                        dv(lambda e, c=c: e.matmul(ps1.t[:], xT_.t[:, c * 128:(c + 1) * 128], w1.t[:, c, :], start=(c == 0), stop=(c == 15)),
                           [xT_.reg(c // 8), w1], [ps1], "pe")
                    for c in range(16):
                        dv(lambda e, c=c: e.matmul(ps3.t[:], xT_.t[:, c * 128:(c + 1) * 128], w3.t[:, c, :], start=(c == 0), stop=(c == 15)),
                           [xT_.reg(c // 8), w3], [ps3], "pe")
                    s1, ub, uT, yb = s1s[i], ubs[i], uTs[i], ybs[i]
                    dv(lambda e: e.activation(out=s1.t[:], in_=ps1.t[:], func=AF.Silu), [ps1], [s1], "act")
                    dv(lambda e: e.tensor_tensor(out=ub.t[:], in0=ps3.t[:], in1=s1.t[:], op=ALU.mult), [ps3, s1], [ub])
                    if it + 1 < NBLK:
                        transpose_x(it + 1)
                    pbu = PB[0]
                    for k in range(4):
                        dv(lambda e, k=k: e.transpose(out=pbu.t[:, k * 128:(k + 1) * 128], in_=ub.t[:].rearrange("q (p c) -> q c p", c=4)[:, k, :], identity=ident_bf.t[:]),
                           [ub, ident_bf], [pbu], "pe")
                    dv(lambda e: e.tensor_copy(out=uT.t[:], in_=pbu.t[:, 0:512]), [pbu], [uT])
                    for n in range(4):
                        py = PF[2 + n]
                        for k in range(4):
                            dv(lambda e, k=k, n=n, py=py: e.matmul(py.t[:], uT.t[:, k * 128:(k + 1) * 128], w2.t[:, k, n * 512:(n + 1) * 512], start=(k == 0), stop=(k == 3)),
                               [uT, w2], [py], "pe")
                        if n % 2 == 0:
                            dv(lambda e, n=n, py=py: e.activation(out=yb.t[:, n * 512:(n + 1) * 512], in_=py.t[:], func=AF.Copy), [py], [yb.reg(n)], "act")
                        else:
                            dv(lambda e, n=n, py=py: e.tensor_copy(out=yb.t[:, n * 512:(n + 1) * 512], in_=py.t[:]), [py], [yb.reg(n)])
                    sch.dma("sp", ch_y[i], OBUF[b * 128:(b + 1) * 128, :], yb.t[:], reads=[yb])
                    if it + NSTR < NBLK:
                        load_w(it + NSTR)
                sch.barrier()

        phaseF()
        if stop_after == "F":
            return nc

        def phaseG():
            pes = contextlib.ExitStack()
            with pes:
                P = Pool_(nc, pes)
                gfin_b = P.sb("gfin_b", [128, D], F32)
                x1s = [P.sb("gx1_%d" % i, [128, D], F32) for i in range(2)]
                o1s = [P.sb("go1_%d" % i, [128, D], F32) for i in range(2)]
                o2s = [P.sb("go2_%d" % i, [128, D], F32) for i in range(2)]
                sm = P.sb("gsm", [128, 8], F32)
                ch_g = sch.chan("g_c")
                ch_x = [sch.chan("g_x%d" % i) for i in range(2)]
                ch_o1 = [sch.chan("g_o1%d" % i, sw=True) for i in range(2)]
                ch_o2 = [sch.chan("g_o2%d" % i, sw=True) for i in range(2)]
                ch_y = [sch.chan("g_y%d" % i) for i in range(2)]
                sch.dma("sp", ch_g, gfin_b.t[:], g_final.rearrange("(o n) -> o n", o=1).partition_broadcast(128), writes=[gfin_b])

                def dv(fn, reads, writes, eng="dve"):
                    sch.op(eng, fn, reads=reads, writes=writes)

                def col(i):
                    return sm.t[:, i:i + 1]

                for t in range(32):
                    i = t % 2
                    rows = slice(t * 128, (t + 1) * 128)
                    x1b, o1, o2 = x1s[i], o1s[i], o2s[i]
                    sch.dma("sp", ch_x[i], x1b.t[:], X1[rows, :], writes=[x1b])
                    sch.custom_dma("pool", ch_o1[i], lambda e, o1=o1, t=t: e.indirect_dma_start(
                        out=o1.t[:, :], out_offset=None, in_=OBUF[:, :],
                        in_offset=bass.IndirectOffsetOnAxis(ap=posi.t[:, t:t + 1], axis=0), bounds_check=bc_pos, oob_is_err=False),
                        reads=[posi], writes=[o1])
                    sch.custom_dma("pool", ch_o2[i], lambda e, o2=o2, t=t: e.indirect_dma_start(
                        out=o2.t[:, :], out_offset=None, in_=OBUF[:, :],
                        in_offset=bass.IndirectOffsetOnAxis(ap=posi.t[:, 32 + t:33 + t], axis=0), bounds_check=bc_pos, oob_is_err=False),
                        reads=[posi], writes=[o2])
                    dv(lambda e, t=t, o1=o1, x1b=x1b: e.scalar_tensor_tensor(out=x1b.t[:], in0=o1.t[:], scalar=wts.t[:, t:t + 1], in1=x1b.t[:], op0=ALU.mult, op1=ALU.add),
                       [o1, wts, x1b], [x1b])
                    dv(lambda e, t=t, o2=o2, x1b=x1b: e.scalar_tensor_tensor(out=x1b.t[:], in0=o2.t[:], scalar=wts.t[:, 32 + t:33 + t], in1=x1b.t[:], op0=ALU.mult, op1=ALU.add),
                       [o2, wts, x1b], [x1b])
                    dv(lambda e: e.memset(sm.t[:, 0:4], 0.0), [], [sm])
                    dv(lambda e, o1=o1, x1b=x1b: e.activation(out=o1.t[:], in_=x1b.t[:], func=AF.Square, accum_out=col(0)), [x1b, sm], [o1, sm], "act")
                    dv(lambda e: e.activation(out=col(1), in_=col(0), func=AF.Ln, scale=1.0 / D, bias=eps_sb.t[:, 0:1]), [sm, eps_sb], [sm], "act")
                    dv(lambda e: e.activation(out=col(2), in_=col(1), func=AF.Exp, scale=-0.5), [sm], [sm], "act")
                    dv(lambda e, o2=o2, x1b=x1b: e.scalar_tensor_tensor(out=o2.t[:], in0=x1b.t[:], scalar=col(2), in1=gfin_b.t[:], op0=ALU.mult, op1=ALU.mult),
                       [x1b, sm, gfin_b], [o2])
                    sch.dma("sp", ch_y[i], y[rows, :], o2.t[:], reads=[o2])
                sch.barrier()

        phaseG()

    return nc


def host_inputs(inputs, cores=None):
    x = np.asarray(inputs["x"], dtype=np.float32)
    f32 = np.float32
    inv = (10000.0 ** (-np.arange(0, 64, 2, dtype=np.float32) / np.float32(64))).astype(f32)
    rmat = np.zeros((128, 128), f32)
    for blk in range(2):
        for i in range(32):
            rmat[blk * 64 + i + 32, blk * 64 + i] = -1.0
            rmat[blk * 64 + i, blk * 64 + i + 32] = 1.0
    ident = np.eye(128, dtype=f32)
    consts = np.zeros((128, 64), f32)
    consts[:, 0] = 128.0 * np.arange(128)
    consts[:, 1] = np.arange(128)
    thr = np.ascontiguousarray(np.broadcast_to(128.0 * np.arange(64, dtype=f32)[None, :], (128, 64)), f32)
    shared = {
        "g_mix": np.ascontiguousarray(inputs["g_mix"][0], f32),
        "w_in": np.ascontiguousarray(inputs["w_in"][0], f32),
        "q_norm_a": np.ascontiguousarray(inputs["q_norm_a"][0], f32),
        "k_norm_a": np.ascontiguousarray(inputs["k_norm_a"][0], f32),
        "lam4": np.ascontiguousarray(np.stack([inputs["lam_q1"][0], inputs["lam_k1"][0],
                                               inputs["lam_q2"][0], inputs["lam_k2"][0]]), f32),
        "subln_b": np.ascontiguousarray(inputs["subln_b"][0], f32),
        "w_branch_a": np.ascontiguousarray(inputs["w_branch_a"][0], f32),
        "w_branch_b": np.ascontiguousarray(inputs["w_branch_b"][0], f32),
        "w_gate": np.ascontiguousarray(inputs["w_gate"][0], f32),
        "b_gate": np.ascontiguousarray(inputs["b_gate"][0], f32),
        "w_out": np.ascontiguousarray(inputs["w_out"][0], f32),
        "g_ffn": np.ascontiguousarray(inputs["g_ffn"][0], f32),
        "w_router": np.ascontiguousarray(np.concatenate([inputs["w_router_group"][0],
                                                         inputs["w_router_expert"][0]], axis=1), f32),
        "w_e_gate": np.ascontiguousarray(inputs["w_e_gate"][0], f32),
        "w_e_up": np.ascontiguousarray(inputs["w_e_up"][0], f32),
        "w_e_down": np.ascontiguousarray(inputs["w_e_down"][0], f32),
        "g_final": np.ascontiguousarray(inputs["g_final"], f32),
        "rmat": rmat, "ident": ident, "consts": consts,
        "ustrict": np.triu(np.ones((128, 128), f32), 1), "thr": thr,
    }
    in_maps = []
    for core in (range(NCORES) if cores is None else cores):
        b, hf = core // 2, core % 2
        own = np.arange(hf * OWN, (hf + 1) * OWN)
        oth = np.arange((1 - hf) * OWN, (2 - hf) * OWN)
        perm = np.concatenate([own, oth])
        xb = x[b]
        m = dict(shared)
        m["xT"] = np.ascontiguousarray(xb[perm].T)
        m["x_own"] = np.ascontiguousarray(xb[own])
        pos = perm.astype(f32)
        rows = (perm // 64).astype(f32)
        cols = (perm % 64).astype(f32)
        angR = rows[None, :] * inv[:, None]
        angC = cols[None, :] * inv[:, None]
        angP = pos[None, :] * inv[:, None]
        m["cosA"] = np.ascontiguousarray(np.concatenate([np.cos(angR)] * 2 + [np.cos(angC)] * 2, 0), f32)
        m["sinA"] = np.ascontiguousarray(np.concatenate([np.sin(angR)] * 2 + [np.sin(angC)] * 2, 0), f32)
        m["cosB"] = np.ascontiguousarray(np.concatenate([np.cos(angP)] * 4, 0), f32)
        m["sinB"] = np.ascontiguousarray(np.concatenate([np.sin(angP)] * 4, 0), f32)
        in_maps.append(m)
    return in_maps


def kernel(**inputs):
    in_maps = host_inputs(inputs)
    nc = build()
    res = run_bass_kernel_spmd(nc, in_maps, core_ids=list(range(NCORES)))
    out = np.zeros((4, S, D), np.float32)
    for core in range(NCORES):
        b, hf = core // 2, core % 2
        out[b, hf * OWN:(hf + 1) * OWN] = res.results[core]["y"]
    return out
```
